# Optimizing a Trainium2 kernel written in Bass

```python
import math
import jax, jax.numpy as jnp
from jax import lax
import numpy as np

D_MODEL = 1024
BATCH = 16
SEQ = 4096
DEPTH = 4

CHUNK = 64
HEAD_DIM = 64
H_A = 8
LEFT_CHUNKS = 8
REL_MAX = 256
H_B = 4
H_C = 8
H_IDX = 4
D_IDX = 64
TOPK_MAX = 256
H_X = 4
MEM_LEN = 256
D_FF = -(-8 * D_MODEL // (3 * 256)) * 256
QB = 128
ROPE_THETA = 10000.0
EPS = 1e-6

W_A = H_A * HEAD_DIM
W_B = H_B * 2 * HEAD_DIM
W_C = H_C * HEAD_DIM
IN_SPLITS = (W_A, W_A, W_A, W_B, W_B, W_B, W_C, W_C, W_C, H_IDX * D_IDX, D_IDX, H_IDX, 3 * D_MODEL)
N_IN = 3 * W_A + 3 * W_B + 3 * W_C + H_IDX * D_IDX + D_IDX + H_IDX + 3 * D_MODEL
REL_TABLE = CHUNK + REL_MAX

kernel_name = 'hybrid_chunk_causal_encoder'


def rmsnorm(x, g):
    x32 = x.astype(jnp.float32)
    y = x32 * lax.rsqrt(jnp.mean(x32 * x32, axis=-1, keepdims=True) + EPS)
    return y.astype(x.dtype) * g


def rope_tables(seq):
    pos = jnp.arange(seq, dtype=jnp.float32)
    inv = ROPE_THETA ** (-jnp.arange(0, HEAD_DIM, 2, dtype=jnp.float32) / HEAD_DIM)
    ang = pos[:, None] * inv[None, :]
    return jnp.cos(ang), jnp.sin(ang)


def apply_rope(x, cos, sin):
    extra = x.ndim - 3
    shp = (cos.shape[0],) + (1,) * extra + (cos.shape[1],)
    c = cos.reshape(shp).astype(x.dtype)
    s = sin.reshape(shp).astype(x.dtype)
    x1, x2 = jnp.split(x, 2, axis=-1)
    return jnp.concatenate([x1 * c - x2 * s, x2 * c + x1 * s], axis=-1)


def masked_softmax(scores, mask):
    return jax.nn.softmax(jnp.where(mask, scores.astype(jnp.float32), -jnp.inf), axis=-1)


def chunk_band_attention(q, k, v, rel_bias):
    b, s, h, d = q.shape
    nc = s // CHUNK
    band = LEFT_CHUNKS + 1
    pad = ((0, 0), (LEFT_CHUNKS * CHUNK, 0), (0, 0), (0, 0))
    kp = jnp.pad(k, pad)
    vp = jnp.pad(v, pad)
    i = jnp.arange(CHUNK)[:, None]
    j = jnp.arange(band * CHUNK)[None, :]
    rel = jnp.clip(LEFT_CHUNKS * CHUNK + i - j, -(CHUNK - 1), REL_MAX) + (CHUNK - 1)
    bias = rel_bias[:, rel].astype(jnp.float32)
    key_off = jnp.arange(band * CHUNK) // CHUNK - LEFT_CHUNKS
    scale = d ** -0.5

    def one_chunk(n):
        start = n * CHUNK
        qn = lax.dynamic_slice_in_dim(q, start, CHUNK, axis=1)
        kn = lax.dynamic_slice_in_dim(kp, start, band * CHUNK, axis=1)
        vn = lax.dynamic_slice_in_dim(vp, start, band * CHUNK, axis=1)
        sc = jnp.einsum('bqhd,bkhd->bhqk', qn, kn).astype(jnp.float32) * scale + bias[None]
        valid = (n + key_off >= 0)[None, None, None, :]
        p = masked_softmax(sc, valid).astype(v.dtype)
        return jnp.einsum('bhqk,bkhd->bqhd', p, vn)

    o = lax.map(one_chunk, jnp.arange(nc))
    return o.transpose(1, 0, 2, 3, 4).reshape(b, s, h * d)


def diff_attention(q, k, v, lam, lambda_init, subln):
    b, s, h, _, d = q.shape
    key_chunk = jnp.arange(s) // CHUNK
    scale = d ** -0.5

    def one_block(n):
        qn = lax.dynamic_slice_in_dim(q, n * QB, QB, axis=1)
        sc = jnp.einsum('bqhcd,bkhcd->bhcqk', qn, k).astype(jnp.float32) * scale
        q_chunk = (n * QB + jnp.arange(QB)) // CHUNK
        valid = key_chunk[None, :] <= q_chunk[:, None]
        p = masked_softmax(sc, valid)
        a = p[:, :, 0] - lam * p[:, :, 1]
        return jnp.einsum('bhqk,bkhe->bqhe', a.astype(v.dtype), v)

    o = lax.map(one_block, jnp.arange(s // QB))
    o = o.transpose(1, 0, 2, 3, 4).reshape(b, s, h, 2 * d)
    o = rmsnorm(o, subln) * (1.0 - lambda_init)
    return o.reshape(b, s, h * 2 * d)


def dsa_attention(q, k, v, q_idx, k_idx, w_idx, topk):
    b, s, h, d = q.shape
    key_pos = jnp.arange(s)
    gather = jax.vmap(lambda t, idx: t[idx])
    scale = d ** -0.5

    def one_chunk(n):
        start = n * CHUNK
        qn = lax.dynamic_slice_in_dim(q, start, CHUNK, axis=1)
        qi = lax.dynamic_slice_in_dim(q_idx, start, CHUNK, axis=1)
        wi = lax.dynamic_slice_in_dim(w_idx, start, CHUNK, axis=1).astype(jnp.float32)
        logits = jnp.einsum('bqhd,bsd->bqhs', qi, k_idx).astype(jnp.float32) * D_IDX ** -0.5
        score = jnp.einsum('bqhs,bqh->bqs', jax.nn.relu(logits), wi)
        admissible = key_pos < start + CHUNK
        score = jnp.where(admissible[None, None, :], score, -jnp.inf)
        vals, idx = lax.top_k(score, topk)
        valid = jnp.isfinite(vals)
        kg = gather(k, idx)
        vg = gather(v, idx)
        sc = jnp.einsum('bqhd,bqkhd->bhqk', qn, kg).astype(jnp.float32) * scale
        p = masked_softmax(sc, valid[:, None]).astype(v.dtype)
        return jnp.einsum('bhqk,bqkhd->bqhd', p, vg)

    o = lax.map(one_chunk, jnp.arange(s // CHUNK))
    return o.transpose(1, 0, 2, 3, 4).reshape(b, s, h * d)


def memory_cross_attention(hn, mem_n, w_q, w_kv, w_o):
    b, s, _ = hn.shape
    m = mem_n.shape[1]
    q = (hn @ w_q).reshape(b, s, H_X, HEAD_DIM)
    kv = (mem_n @ w_kv).reshape(b, m, 2, H_X, HEAD_DIM)
    sc = jnp.einsum('bqhd,bmhd->bhqm', q, kv[:, :, 0]).astype(jnp.float32) * HEAD_DIM ** -0.5
    p = jax.nn.softmax(sc, axis=-1).astype(hn.dtype)
    o = jnp.einsum('bhqm,bmhd->bqhd', p, kv[:, :, 1]).reshape(b, s, H_X * HEAD_DIM)
    return o @ w_o


def swiglu(hn, w_gu, w_down):
    g, u = jnp.split(hn @ w_gu, 2, axis=-1)
    return (jax.nn.silu(g) * u) @ w_down


def setup_inputs(seed: int = 0) -> dict:
    key = jax.random.key(seed)
    ks = jax.random.split(key, 20)

    def normal(k, shape, scale):
        return jax.random.normal(k, shape, jnp.float32) * scale

    def gain(k, shape):
        return 1.0 + normal(k, shape, 0.05)

    return {
        'x': normal(ks[0], (BATCH, SEQ, D_MODEL), 1.0),
        'mem': normal(ks[1], (BATCH, MEM_LEN, D_MODEL), 1.0),
        'norm_mix': gain(ks[2], (DEPTH, D_MODEL)),
        'w_in': normal(ks[3], (DEPTH, D_MODEL, N_IN), D_MODEL ** -0.5),
        'rel_bias_a': normal(ks[4], (DEPTH, H_A, REL_TABLE), 0.5),
        'lambda_vecs': normal(ks[5], (DEPTH, 4, HEAD_DIM), 0.1),
        'subln_b': gain(ks[6], (DEPTH, 2 * HEAD_DIM)),
        'w_up_a': normal(ks[7], (DEPTH, W_A, D_MODEL), W_A ** -0.5),
        'w_up_b': normal(ks[8], (DEPTH, W_B, D_MODEL), W_B ** -0.5),
        'w_up_c': normal(ks[9], (DEPTH, W_C, D_MODEL), W_C ** -0.5),
        'w_out': normal(ks[10], (DEPTH, D_MODEL, D_MODEL), D_MODEL ** -0.5),
        'norm_cross': gain(ks[11], (DEPTH, D_MODEL)),
        'w_q_x': normal(ks[12], (DEPTH, D_MODEL, H_X * HEAD_DIM), D_MODEL ** -0.5),
        'w_kv_x': normal(ks[13], (DEPTH, D_MODEL, 2 * H_X * HEAD_DIM), D_MODEL ** -0.5),
        'w_o_x': normal(ks[14], (DEPTH, H_X * HEAD_DIM, D_MODEL), (H_X * HEAD_DIM) ** -0.5),
        'norm_ffn': gain(ks[15], (DEPTH, D_MODEL)),
        'w_gu': normal(ks[16], (DEPTH, D_MODEL, 2 * D_FF), D_MODEL ** -0.5),
        'w_down': normal(ks[17], (DEPTH, D_FF, D_MODEL), D_FF ** -0.5),
        'mem_norm': gain(ks[18], (D_MODEL,)),
        'final_norm': gain(ks[19], (D_MODEL,)),
    }


def reference(x, mem, norm_mix, w_in, rel_bias_a, lambda_vecs, subln_b, w_up_a, w_up_b, w_up_c,
              w_out, norm_cross, w_q_x, w_kv_x, w_o_x, norm_ffn, w_gu, w_down, mem_norm, final_norm):
    b, s, _ = x.shape
    topk = min(TOPK_MAX, s // 4)
    cos, sin = rope_tables(s)
    offsets = [int(o) for o in np.cumsum(IN_SPLITS)[:-1]]
    mem_n = rmsnorm(mem, mem_norm)
    shp_a = (b, s, H_A, HEAD_DIM)
    shp_b = (b, s, H_B, 2, HEAD_DIM)
    shp_c = (b, s, H_C, HEAD_DIM)
    for l in range(DEPTH):
        xn = rmsnorm(x, norm_mix[l])
        (qa, ka, va, qb, kb, vb, qc, kc, vc, qi, ki, wi, gates) = jnp.split(xn @ w_in[l], offsets, axis=-1)
        o_a = chunk_band_attention(qa.reshape(shp_a), ka.reshape(shp_a), va.reshape(shp_a), rel_bias_a[l])
        lam_init = 0.8 - 0.6 * math.exp(-0.3 * l)
        lv = lambda_vecs[l].astype(jnp.float32)
        lam = jnp.exp(jnp.sum(lv[0] * lv[1])) - jnp.exp(jnp.sum(lv[2] * lv[3])) + lam_init
        o_b = diff_attention(apply_rope(qb.reshape(shp_b), cos, sin), apply_rope(kb.reshape(shp_b), cos, sin),
                             vb.reshape(b, s, H_B, 2 * HEAD_DIM), lam, lam_init, subln_b[l])
        o_c = dsa_attention(apply_rope(qc.reshape(shp_c), cos, sin), apply_rope(kc.reshape(shp_c), cos, sin),
                            vc.reshape(shp_c),
                            apply_rope(qi.reshape(b, s, H_IDX, D_IDX), cos, sin),
                            apply_rope(ki[:, :, None, :], cos, sin)[:, :, 0],
                            wi * H_IDX ** -0.5, topk)
        g = jax.nn.sigmoid(gates).reshape(b, s, 3, D_MODEL)
        y = (g[:, :, 0] * (o_a @ w_up_a[l]) + g[:, :, 1] * (o_b @ w_up_b[l])
             + g[:, :, 2] * (o_c @ w_up_c[l]))
        x = x + y @ w_out[l]
        x = x + memory_cross_attention(rmsnorm(x, norm_cross[l]), mem_n, w_q_x[l], w_kv_x[l], w_o_x[l])
        x = x + swiglu(rmsnorm(x, norm_ffn[l]), w_gu[l], w_down[l])
    return rmsnorm(x, final_norm)
```

```python
import math
from contextlib import ExitStack

import numpy as np
import ml_dtypes

import concourse.bass as bass
import concourse.mybir as mybir
from concourse.bass_utils import run_bass_kernel_spmd

F32 = mybir.dt.float32
BF16 = mybir.dt.bfloat16
AF = mybir.ActivationFunctionType
ALU = mybir.AluOpType
AX = mybir.AxisListType

LIMIT = 30000
ENGS = ['pe', 'act', 'dve', 'pool', 'sp']

D = 1024
NCH = 8
MEM = 256
DFF = 2816
NFF = 22
N_IN = 8004
EPS = 1e-6
NEG = -240000.0
NIT = 18
TOPK = 256


class Res:
    __slots__ = ('name', 'w', 'r')

    def __init__(self, name=''):
        self.name = name
        self.w = None
        self.r = []


class Buf:
    __slots__ = ('t', 'r')

    def __init__(self, t, name=''):
        self.t = t
        self.r = Res(name)


class Prog:
    def __init__(self, nc, stack, dma_pool=None):
        self.nc = nc
        self.stack = stack
        self.streams = {e: [] for e in ENGS}
        self.idx = {e: 0 for e in ENGS}
        self.clock = {e: {} for e in ENGS}
        self.esems = {e: [] for e in ENGS}
        dma_pool = dma_pool or {'sp': 40, 'act': 4, 'pool': 24}
        self.dpool = {}
        self.dnext = {q: 0 for q in dma_pool}
        self.dcount = {}
        self.dsem = {}
        for q, n in dma_pool.items():
            self.dpool[q] = []
            for i in range(n):
                s = stack.enter_context(nc.semaphore(f"dq_{q}_{i}"))
                self.dpool[q].append(s)
                self.dcount[(q, i)] = 0
                self.dsem[(q, i)] = s
        self.nwaits = 0

    def _esem(self, eng, epoch):
        while len(self.esems[eng]) <= epoch:
            s = self.stack.enter_context(self.nc.semaphore(f"e_{eng}_{len(self.esems[eng])}"))
            self.esems[eng].append(s)
        return self.esems[eng][epoch]

    def _need(self, eng, tok, kind):
        key, val, snap = tok
        if key == eng:
            if eng == 'pe' or kind != 'raw':
                return
        ck = self.clock[eng]
        if ck.get(key, 0) >= val:
            return
        self.streams[eng].append(('wait', key, val))
        self.nwaits += 1
        new = dict(ck)
        for k, v in snap.items():
            if new.get(k, 0) < v:
                new[k] = v
        if new.get(key, 0) < val:
            new[key] = val
        self.clock[eng] = new

    def _deps(self, eng, reads, writes):
        for res in reads:
            if res.w is not None:
                self._need(eng, res.w, 'raw')
        for res in writes:
            if res.w is not None:
                self._need(eng, res.w, 'waw')
            for t in res.r:
                self._need(eng, t, 'war')

    def _commit(self, tok, reads, writes):
        for res in writes:
            res.w = tok
            res.r = []
        key = tok[0]
        for res in reads:
            if res in writes:
                continue
            if isinstance(key, str):
                res.r = [t for t in res.r if t[0] != key]
            res.r.append(tok)

    def op(self, eng, fn, reads=(), writes=()):
        self._deps(eng, reads, writes)
        self.idx[eng] += 1
        i = self.idx[eng]
        self.streams[eng].append(('op', fn, i))
        tok = (eng, i, self.clock[eng])
        self._commit(tok, reads, writes)
        return tok

    def dma(self, q, fn, reads=(), writes=()):
        self._deps(q, reads, writes)
        slot = self.dnext[q]
        self.dnext[q] = (slot + 1) % len(self.dpool[q])
        key = (q, slot)
        prev = self.dcount[key]
        if prev > 0:
            self._need(q, (key, prev, {}), 'raw')
        val = prev + 16
        assert val < 2 * LIMIT, "dma sem overflow"
        self.dcount[key] = val
        self.streams[q].append(('dma', fn, key))
        tok = (key, val, self.clock[q])
        self._commit(tok, reads, writes)
        return tok

    def barrier(self):
        for key, val in self.dcount.items():
            if val > 0:
                self._need('sp', (key, val, {}), 'raw')
        for e in ENGS:
            if e != 'sp' and self.idx[e] > 0:
                self._need('sp', (e, self.idx[e], self.clock[e]), 'raw')
        tok = self.op('sp', lambda e: e.nop())
        for e in ENGS:
            if e != 'sp':
                self._need(e, tok, 'raw')

    def finish(self):
        self.barrier()

    def emit(self):
        nc = self.nc
        handles = {'pe': 'tensor', 'act': 'scalar', 'dve': 'vector', 'pool': 'gpsimd', 'sp': 'sync'}
        for eng in ENGS:
            for ep in range((self.idx[eng] + LIMIT - 1) // LIMIT + 1):
                self._esem(eng, ep)
        with nc.Block() as block:
            for eng in ENGS:
                stream = self.streams[eng]

                def body(e, eng=eng, stream=stream):
                    for item in stream:
                        if item[0] == 'wait':
                            _, key, val = item
                            if isinstance(key, str):
                                e.wait_ge(self.esems[key][(val - 1) // LIMIT], (val - 1) % LIMIT + 1)
                            else:
                                e.wait_ge(self.dsem[key], val)
                        elif item[0] == 'op':
                            _, fn, i = item
                            fn(e).then_inc(self.esems[eng][(i - 1) // LIMIT], 1)
                        else:
                            _, fn, key = item
                            fn(e).then_inc(self.dsem[key], 16)

                getattr(block, handles[eng])(body)


class Rot:
    def __init__(self, bufs):
        self.bufs = bufs
        self.i = 0

    def next(self):
        b = self.bufs[self.i]
        self.i = (self.i + 1) % len(self.bufs)
        return b


class Builder:
    def __init__(self, S, NSEQ, DEPTH, debug=False, phases=None):
        self.S, self.NSEQ, self.DEPTH, self.debug = S, NSEQ, DEPTH, debug
        self.phases = phases
        self.NT = S // 128
        self.NB = S // 512
        self.nc = bass.Bass("TRN2", target_bir_lowering=False)
        self.uid = 0

    def dram_in(self, name, shape, dt=F32):
        return self.nc.dram_tensor(name, list(shape), dt, kind="ExternalInput").ap()

    def dram_scr(self, name, shape, dt):
        kind = "ExternalOutput" if self.debug else "Internal"
        return self.nc.dram_tensor(name, list(shape), dt, kind=kind).ap()

    def sb(self, st, name, shape, dt):
        self.uid += 1
        return st.enter_context(self.nc.sbuf_tensor(f"{name}_{self.uid}", list(shape), dt))

    def sbuf(self, st, name, shape, dt):
        return Buf(self.sb(st, name, shape, dt), name)

    def sbufs(self, st, name, shape, dt, n):
        return Rot([self.sbuf(st, f"{name}{i}", shape, dt) for i in range(n)])

    def ps(self, st, name, shape, dt):
        self.uid += 1
        return Buf(st.enter_context(self.nc.psum_tensor(f"{name}_{self.uid}", list(shape), dt)), name)

    def pss(self, st, name, shape, dt, n):
        return Rot([self.ps(st, f"{name}{i}", shape, dt) for i in range(n)])

    def build(self):
        nc = self.nc
        S, NSEQ, DEPTH = self.S, self.NSEQ, self.DEPTH
        L = DEPTH
        I = {}
        I['x'] = self.dram_in('x', [NSEQ, S, D])
        I['mem'] = self.dram_in('mem', [NSEQ, MEM, D])
        I['norm_mix'] = self.dram_in('norm_mix', [L, D])
        I['w_in'] = self.dram_in('w_in', [L, D, N_IN])
        I['rel_bias_a'] = self.dram_in('rel_bias_a', [L, 8, 320])
        I['lambda_vecs'] = self.dram_in('lambda_vecs', [L, 256])
        I['subln_b'] = self.dram_in('subln_b', [L, 128])
        I['w_up_a'] = self.dram_in('w_up_a', [L, 512, D])
        I['w_up_b'] = self.dram_in('w_up_b', [L, 512, D])
        I['w_up_c'] = self.dram_in('w_up_c', [L, 512, D])
        I['w_out'] = self.dram_in('w_out', [L, D, D])
        I['norm_cross'] = self.dram_in('norm_cross', [L, D])
        I['w_q_x'] = self.dram_in('w_q_x', [L, D, 256])
        I['w_kv_x'] = self.dram_in('w_kv_x', [L, D, 512])
        I['w_o_x'] = self.dram_in('w_o_x', [L, 256, D])
        I['norm_ffn'] = self.dram_in('norm_ffn', [L, D])
        I['w_gu'] = self.dram_in('w_gu', [L, D, 2 * DFF])
        I['w_down'] = self.dram_in('w_down', [L, DFF, D])
        I['mem_norm'] = self.dram_in('mem_norm', [D])
        I['final_norm'] = self.dram_in('final_norm', [D])
        I['c_ident'] = self.dram_in('c_ident', [128, 128], BF16)
        I['c_rot'] = self.dram_in('c_rot', [128, 128], BF16)
        I['c_flip'] = self.dram_in('c_flip', [128, 128], BF16)
        I['c_cos'] = self.dram_in('c_cos', [128, S], BF16)
        I['c_sin'] = self.dram_in('c_sin', [128, S], BF16)
        I['c_cmask'] = self.dram_in('c_cmask', [128, 4, 512], BF16)
        self.I = I
        self.out = nc.dram_tensor('out', [NSEQ, S, D], F32, kind="ExternalOutput").ap()
        X = {}
        X['xres'] = self.dram_scr('xres', [NSEQ, S, D], F32)
        for nm in ['qaT', 'kaT', 'qbT', 'kbT', 'qcT', 'kcT']:
            X[nm] = self.dram_scr(nm, [NSEQ, 512, S], BF16)
        X['qiT'] = self.dram_scr('qiT', [NSEQ, 256, S], BF16)
        X['kiT'] = self.dram_scr('kiT', [NSEQ, 128, S], BF16)
        X['va1'] = self.dram_scr('va1', [NSEQ, S, 8 * 65], BF16)
        X['vb1'] = self.dram_scr('vb1', [NSEQ, S, 4 * 129], BF16)
        X['vc1'] = self.dram_scr('vc1', [NSEQ, S, 8 * 65], BF16)
        X['wi'] = self.dram_scr('wi', [NSEQ, S, 4], F32)
        X['oT'] = self.dram_scr('oT', [NSEQ, 1536, S], BF16)
        X['ebias'] = self.dram_scr('ebias', [8, 1536], BF16)
        if self.debug:
            X['dbgI'] = self.dram_scr('dbgI', [self.NT, 128, S], F32)
            X['dbgM'] = self.dram_scr('dbgM', [self.NT, 128, S], BF16)
            X['dbgthr'] = self.dram_scr('dbgthr', [self.NT, 128, 4], F32)
        self.X = X

        with ExitStack() as st:
            self.P = P = Prog(nc, st)
            self.ident = self.sbuf(st, 'ident', [128, 128], BF16)
            self.rot = self.sbuf(st, 'rot', [128, 128], BF16)
            self.flip = self.sbuf(st, 'flip', [128, 128], BF16)
            self.gains = self.sbuf(st, 'gains', [128, 3, L, 8, 1], F32)
            self.gmem = self.sbuf(st, 'gmem', [128, 8, 1], F32)
            self.subln = self.sbuf(st, 'subln', [128, L, 1], F32)
            self.neglam = self.sbuf(st, 'neglam', [128, L], F32)
            self.neghalf = self.sbuf(st, 'neghalf', [128, 4], F32)
            self.memT = [self.sbuf(st, f'memT{s}', [128, NCH, MEM], BF16) for s in range(NSEQ)]
            self.setup()
            for l in range(DEPTH):
                if self.want('p1'):
                    self.phase1(l)
                if self.want('p2'):
                    for s in range(NSEQ):
                        self.phase2(l, s)
                if self.want('p3a'):
                    self.phase3a(l)
                if self.want('p3b'):
                    self.phase3b(l)
            P.finish()
            P.emit()
        return nc

    def want(self, ph):
        return self.phases is None or ph in self.phases

    def lam_init(self, l):
        return 0.8 - 0.6 * math.exp(-0.3 * l)

    def setup(self):
        P, I, L = self.P, self.I, self.DEPTH
        ident, rot, gains = self.ident, self.rot, self.gains
        P.dma('sp', lambda e: e.dma_start(out=ident.t[:], in_=I['c_ident'][:, :]), writes=[ident.r])
        P.dma('sp', lambda e: e.dma_start(out=rot.t[:], in_=I['c_rot'][:, :]), writes=[rot.r])
        P.dma('sp', lambda e: e.dma_start(out=self.flip.t[:], in_=I['c_flip'][:, :]), writes=[self.flip.r])
        for i, nm in enumerate(['norm_mix', 'norm_cross', 'norm_ffn']):
            src = I[nm].rearrange("l (c p o) -> p l c o", p=128, o=1)
            P.dma('sp', lambda e, i=i, src=src: e.dma_start(out=gains.t[:, i], in_=src, allow_slow_non_contiguous=True), writes=[gains.r])
        P.dma('sp', lambda e: e.dma_start(out=self.gmem.t[:], in_=I['mem_norm'].rearrange("(c p o) -> p c o", p=128, o=1), allow_slow_non_contiguous=True),
              writes=[self.gmem.r])
        P.dma('sp', lambda e: e.dma_start(out=self.subln.t[:], in_=I['subln_b'].rearrange("l (p o) -> p l o", o=1), allow_slow_non_contiguous=True),
              writes=[self.subln.r])
        P.op('dve', lambda e: e.memset(self.neghalf.t[:], -0.5), writes=[self.neghalf.r])
        with ExitStack() as st:
            lv = self.sbuf(st, 'lv', [128, L * 256], F32)
            tmp = self.sbuf(st, 'lvtmp', [128, L, 2, 64], F32)
            sm = self.sbuf(st, 'lvs', [128, L, 2], F32)
            ex = self.sbuf(st, 'lve', [128, L, 2], F32)
            src = I['lambda_vecs'].rearrange("l k -> (l k)").partition_broadcast(128)
            P.dma('sp', lambda e: e.dma_start(out=lv.t[:], in_=src), writes=[lv.r])
            lv4 = lv.t[:].rearrange("p (l a b k) -> p l a b k", l=L, a=2, b=2)
            P.op('dve', lambda e: e.tensor_tensor(out=tmp.t[:], in0=lv4[:, :, :, 0, :], in1=lv4[:, :, :, 1, :], op=ALU.mult),
                 reads=[lv.r], writes=[tmp.r])
            P.op('dve', lambda e: e.tensor_reduce(out=sm.t[:], in_=tmp.t[:], axis=AX.X, op=ALU.add),
                 reads=[tmp.r], writes=[sm.r])
            P.op('act', lambda e: e.activation(out=ex.t[:], in_=sm.t[:], func=AF.Exp), reads=[sm.r], writes=[ex.r])
            for l in range(L):
                P.op('dve', lambda e, l=l: e.tensor_scalar(out=self.neglam.t[:, l:l + 1], in0=ex.t[:, l, 1:2],
                                                          scalar1=ex.t[:, l, 0:1], scalar2=-self.lam_init(l),
                                                          op0=ALU.subtract, op1=ALU.add),
                     reads=[ex.r], writes=[self.neglam.r])
            mt = self.sbufs(st, 'memt', [128, D], F32, 2)
            junk = self.sbuf(st, 'memjunk', [128, D], F32)
            mb = self.sbufs(st, 'memb', [128, D], BF16, 2)
            ssq = self.sbufs(st, 'memss', [128, 2], F32, 2)
            tp = self.pss(st, 'memtp', [128, D], BF16, 2)
            for s in range(self.NSEQ):
                for j in range(2):
                    x_t, x_b, ss, pt = mt.next(), mb.next(), ssq.next(), tp.next()
                    P.dma('sp', lambda e, s=s, j=j, x_t=x_t: e.dma_start(out=x_t.t[:], in_=I['mem'][s, j * 128:(j + 1) * 128, :]),
                          writes=[x_t.r])
                    self.rms_to_bf16(x_t, junk, ss, x_b)
                    self.transpose_to(x_b, pt, self.memT[s], j * 128, 128, 'dve')
        P.barrier()

    def rms_to_bf16(self, x_t, junk, ss, x_b):
        P = self.P
        P.op('act', lambda e: e.activation(out=junk.t[:], in_=x_t.t[:], func=AF.Square, accum_out=ss.t[:, 0:1]),
             reads=[x_t.r], writes=[junk.r, ss.r])
        P.op('dve', lambda e: e.tensor_scalar(out=ss.t[:, 1:2], in0=ss.t[:, 0:1], scalar1=1.0 / D, scalar2=EPS,
                                              op0=ALU.mult, op1=ALU.add), reads=[ss.r], writes=[ss.r])
        P.op('pool', lambda e: e.tensor_tensor(out=ss.t[:, 0:1], in0=ss.t[:, 1:2], in1=self.neghalf.t[:, 0:1], op=ALU.pow),
             reads=[ss.r, self.neghalf.r], writes=[ss.r])
        P.op('dve', lambda e: e.tensor_scalar(out=x_b.t[:], in0=x_t.t[:], scalar1=ss.t[:, 0:1], scalar2=None, op0=ALU.mult),
             reads=[x_t.r, ss.r], writes=[x_b.r])

    def transpose_to(self, x_b, pt, dstT, col0, ncols, eng, dst_res=None):
        P = self.P
        for c in range(NCH):
            P.op('pe', lambda e, c=c: e.transpose(out=pt.t[:, c * 128:(c + 1) * 128], in_=x_b.t[:, c * 128:(c + 1) * 128],
                                                  identity=self.ident.t[:]),
                 reads=[x_b.r, self.ident.r], writes=[pt.r])
        src = pt.t[:].rearrange("p (c t) -> p c t", c=NCH)
        dres = dst_res if dst_res is not None else dstT.r
        if eng == 'act':
            P.op('act', lambda e: e.activation(out=dstT.t[:, :, col0:col0 + ncols], in_=src, func=AF.Copy),
                 reads=[pt.r], writes=[dres])
        else:
            P.op('dve', lambda e: e.tensor_copy(out=dstT.t[:, :, col0:col0 + ncols], in_=src), reads=[pt.r], writes=[dres])

    def load_weight(self, st_stage, dst, dst_sl, src_ap, nrows_chunks, ncols, scale_fn, const=1.0, rowchunk0=0):
        P = self.P
        stage = st_stage
        CW = stage.bufs[0].t.shape[1]
        for c in range(nrows_chunks):
            for c0 in range(0, ncols, CW):
                c1 = min(ncols, c0 + CW)
                sg = stage.next()
                P.dma('sp', lambda e, c=c, c0=c0, c1=c1, sg=sg: e.dma_start(
                    out=sg.t[:, 0:c1 - c0], in_=src_ap[(rowchunk0 + c) * 128:(rowchunk0 + c + 1) * 128, c0:c1]), writes=[sg.r])
                sc = scale_fn(c)
                rd = [sg.r] + ([sc[1]] if sc is not None else [])
                if sc is not None:
                    P.op('pool', lambda e, c=c, c0=c0, c1=c1, sg=sg, sc=sc: e.tensor_scalar(
                        out=dst_sl(c, c0, c1), in0=sg.t[:, 0:c1 - c0], scalar1=sc[0], scalar2=const,
                        op0=ALU.mult, op1=ALU.mult), reads=rd, writes=[dst.r])
                else:
                    P.op('pool', lambda e, c=c, c0=c0, c1=c1, sg=sg: e.tensor_scalar(
                        out=dst_sl(c, c0, c1), in0=sg.t[:, 0:c1 - c0], scalar1=const, scalar2=1.0,
                        op0=ALU.mult, op1=ALU.mult), reads=rd, writes=[dst.r])

    def x_src(self, l):
        return self.I['x'] if l == 0 else self.X['xres']

    def phase1(self, l):
        P, I, X, S = self.P, self.I, self.X, self.S
        with ExitStack() as st:
            w1 = self.sbuf(st, 'w1', [128, NCH, 4932], BF16)
            wki = self.sbuf(st, 'wki', [128, NCH, 128], BF16)
            stage = self.sbufs(st, 'stage', [128, 1644], F32, 2)
            cosT = self.sbuf(st, 'cosT', [128, S], BF16)
            sinT = self.sbuf(st, 'sinT', [128, S], BF16)
            P.dma('sp', lambda e: e.dma_start(out=cosT.t[:], in_=I['c_cos'][:, :]), writes=[cosT.r])
            P.dma('sp', lambda e: e.dma_start(out=sinT.t[:], in_=I['c_sin'][:, :]), writes=[sinT.r])
            gm = self.gains
            self.load_weight(stage, w1, lambda c, c0, c1: w1.t[:, c, c0:c1], I['w_in'][l], NCH, 4932,
                             lambda c: (gm.t[:, 0, l, c, :], gm.r))
            for c in range(NCH):
                for h in range(2):
                    P.op('pool', lambda e, c=c, h=h: e.tensor_copy(out=wki.t[:, c, h * 64:(h + 1) * 64], in_=w1.t[:, c, 4864:4928]),
                         reads=[w1.r], writes=[wki.r])
            xt = self.sbufs(st, 'xt', [128, D], F32, 3)
            junk = self.sbuf(st, 'junk', [128, D], F32)
            xb = self.sbufs(st, 'xb', [128, D], BF16, 2)
            ssq = self.sbufs(st, 'ss', [128, 2], F32, 4)
            xnT = [self.sbuf(st, f'xnT{i}', [128, NCH, 512], BF16) for i in range(2)]
            xnT_res = [[Res() for _ in range(4)] for _ in range(2)]
            tp = self.pss(st, 'tp', [128, D], BF16, 2)
            pm = self.pss(st, 'pm', [128, 512], F32, 4)
            pr = self.pss(st, 'pr', [128, 512], F32, 2)
            qsb = self.sbufs(st, 'qsb', [128, 512], BF16, 3)
            t1 = self.sbufs(st, 't1', [128, 512], F32, 2)
            t2 = self.sbufs(st, 't2', [128, 512], F32, 2)
            osb = self.sbufs(st, 'osb', [128, 512], BF16, 4)
            v65 = self.sbufs(st, 'v65', [128, 8, 65], BF16, 4)
            v129 = self.sbufs(st, 'v129', [128, 4, 129], BF16, 2)
            wsb = self.sbufs(st, 'wsb', [128, 4], F32, 2)
            for b_ in v65.bufs + v129.bufs:
                P.op('dve', lambda e, b_=b_: e.memset(b_.t[:], 1.0), writes=[b_.r])
            ftiles = []
            for i in range(4):
                ftiles.append(('qaT', i * 128, (w1, 0 + i * 128), False))
                ftiles.append(('kaT', i * 128, (w1, 512 + i * 128), False))
            for i in range(4):
                ftiles.append(('qbT', i * 128, (w1, 1536 + i * 128), True))
                ftiles.append(('kbT', i * 128, (w1, 2048 + i * 128), True))
                ftiles.append(('qcT', i * 128, (w1, 3072 + i * 128), True))
                ftiles.append(('kcT', i * 128, (w1, 3584 + i * 128), True))
            for i in range(2):
                ftiles.append(('qiT', i * 128, (w1, 4608 + i * 128), True))
            ftiles.append(('kiT', 0, (wki, 0), True))
            blk = 0
            for s in range(self.NSEQ):
                for tb in range(self.NB):
                    T0 = tb * 512
                    xn = xnT[blk % 2]
                    xr = xnT_res[blk % 2]
                    blk += 1
                    for j in range(4):
                        x_t, x_b, ss, pt = xt.next(), xb.next(), ssq.next(), tp.next()
                        src = self.x_src(l)[s, T0 + j * 128:T0 + (j + 1) * 128, :]
                        P.dma('sp', lambda e, x_t=x_t, src=src: e.dma_start(out=x_t.t[:], in_=src), writes=[x_t.r])
                        self.rms_to_bf16(x_t, junk, ss, x_b)
                        self.transpose_to(x_b, pt, xn, j * 128, 128, 'dve', dst_res=xr[j])
                    for (dst, row0, (wt, col0), rope) in ftiles:
                        pmm = pm.next()
                        for c in range(NCH):
                            P.op('pe', lambda e, c=c, wt=wt, col0=col0, pmm=pmm, xn=xn: e.matmul(
                                pmm.t[:], lhsT=wt.t[:, c, col0:col0 + 128], rhs=xn.t[:, c, :], start=(c == 0), stop=(c == NCH - 1)),
                                reads=[wt.r] + xr, writes=[pmm.r])
                        dview = X[dst][s, row0:row0 + 128, T0:T0 + 512]
                        if not rope:
                            o_ = osb.next()
                            P.op('act', lambda e, o_=o_, pmm=pmm: e.activation(out=o_.t[:], in_=pmm.t[:], func=AF.Copy),
                                 reads=[pmm.r], writes=[o_.r])
                        else:
                            q_ = qsb.next()
                            prr = pr.next()
                            a1, a2, o_ = t1.next(), t2.next(), osb.next()
                            P.op('act', lambda e, q_=q_, pmm=pmm: e.activation(out=q_.t[:], in_=pmm.t[:], func=AF.Copy),
                                 reads=[pmm.r], writes=[q_.r])
                            P.op('pe', lambda e, q_=q_, prr=prr: e.matmul(prr.t[:], lhsT=self.rot.t[:], rhs=q_.t[:], start=True, stop=True),
                                 reads=[self.rot.r, q_.r], writes=[prr.r])
                            P.op('pool', lambda e, q_=q_, a1=a1, T0=T0: e.tensor_tensor(out=a1.t[:], in0=q_.t[:], in1=cosT.t[:, T0:T0 + 512], op=ALU.mult),
                                 reads=[q_.r, cosT.r], writes=[a1.r])
                            P.op('dve', lambda e, prr=prr, a2=a2, T0=T0: e.tensor_tensor(out=a2.t[:], in0=prr.t[:], in1=sinT.t[:, T0:T0 + 512], op=ALU.mult),
                                 reads=[prr.r, sinT.r], writes=[a2.r])
                            P.op('dve', lambda e, a1=a1, a2=a2, o_=o_: e.tensor_tensor(out=o_.t[:], in0=a1.t[:], in1=a2.t[:], op=ALU.add),
                                 reads=[a1.r, a2.r], writes=[o_.r])
                        P.dma('pool', lambda e, o_=o_, dview=dview: e.dma_start(out=dview, in_=o_.t[:]), reads=[o_.r])
                    for j in range(4):
                        tok0 = T0 + j * 128
                        for (dst, col0, nh, hd, vpool) in (('va1', 1024, 8, 64, v65), ('vb1', 2560, 4, 128, v129), ('vc1', 4096, 8, 64, v65)):
                            pmm = pm.next()
                            for c in range(NCH):
                                P.op('pe', lambda e, c=c, j=j, col0=col0, pmm=pmm, xn=xn: e.matmul(
                                    pmm.t[:], lhsT=xn.t[:, c, j * 128:(j + 1) * 128], rhs=w1.t[:, c, col0:col0 + 512],
                                    start=(c == 0), stop=(c == NCH - 1)), reads=[w1.r, xr[j]], writes=[pmm.r])
                            v_ = vpool.next()
                            P.op('act', lambda e, v_=v_, pmm=pmm, nh=nh, hd=hd: e.activation(
                                out=v_.t[:, :, 0:hd], in_=pmm.t[:].rearrange("p (h d) -> p h d", h=nh), func=AF.Copy),
                                reads=[pmm.r], writes=[v_.r])
                            dview = X[dst][s, tok0:tok0 + 128, :]
                            P.dma('pool', lambda e, v_=v_, dview=dview: e.dma_start(out=dview, in_=v_.t[:].rearrange("p h d -> p (h d)")), reads=[v_.r])
                        pmm = pm.next()
                        for c in range(NCH):
                            P.op('pe', lambda e, c=c, j=j, pmm=pmm, xn=xn: e.matmul(
                                pmm.t[:, 0:4], lhsT=xn.t[:, c, j * 128:(j + 1) * 128], rhs=w1.t[:, c, 4928:4932],
                                start=(c == 0), stop=(c == NCH - 1)), reads=[w1.r, xr[j]], writes=[pmm.r])
                        w_ = wsb.next()
                        P.op('dve', lambda e, w_=w_, pmm=pmm: e.tensor_scalar(out=w_.t[:], in0=pmm.t[:, 0:4], scalar1=1.0 / 16.0, scalar2=None, op0=ALU.mult),
                             reads=[pmm.r], writes=[w_.r])
                        dview = X['wi'][s, tok0:tok0 + 128, :]
                        P.dma('pool', lambda e, w_=w_, dview=dview: e.dma_start(out=dview, in_=w_.t[:]), reads=[w_.r])
            P.barrier()

    def phase2(self, l, s):
        self.mixerA(l, s)
        self.mixerB(l, s)
        self.mixerC(l, s)

    def _p2_common(self, st):
        c = {}
        c['sc'] = self.pss(st, 'sc', [128, 512], F32, 3)
        c['acc'] = [self.ps(st, f'acc{i}', [128, 512], F32) for i in range(4)]
        c['tp'] = self.ps(st, 'tpo', [128, 1024], BF16)
        c['E'] = self.sbufs(st, 'E', [128, 512], BF16, 4)
        c['rec'] = self.sbufs(st, 'rec', [128, 1], F32, 8)
        c['oblk'] = self.sbufs(st, 'oblk', [128, 4, 512], BF16, 2)
        c['oT'] = self.sbufs(st, 'oTsb', [128, 4, 512], BF16, 2)
        return c

    def _load_kv(self, st, s, kname, vname, vw):
        P, X, S, NT = self.P, self.X, self.S, self.NT
        kT = self.sbuf(st, 'kT', [128, 4, S], BF16)
        v1 = self.sbuf(st, 'v1', [128, NT, vw], BF16)
        P.dma('sp', lambda e: e.dma_start(out=kT.t[:], in_=X[kname][s].rearrange("(c p) t -> p c t", p=128)), writes=[kT.r])
        half = NT // 2
        for hh in range(2):
            P.dma('sp', lambda e, hh=hh: e.dma_start(
                out=v1.t[:, hh * half:(hh + 1) * half, :],
                in_=X[vname][s, hh * half * 128:(hh + 1) * half * 128, :].rearrange("(t p) f -> p t f", p=128)), writes=[v1.r])
        return kT, v1

    def _store_o(self, c, ob, s, row0, Q0):
        P, X = self.P, self.X
        oT = c['oT'].next()
        tp = c['tp']
        for half in range(2):
            for cc in range(2):
                ch = half * 2 + cc
                for j in range(4):
                    P.op('pe', lambda e, ch=ch, j=j, cc=cc: e.transpose(
                        out=tp.t[:, cc * 512 + j * 128: cc * 512 + (j + 1) * 128],
                        in_=ob.t[:, j, ch * 128:(ch + 1) * 128], identity=self.ident.t[:]),
                        reads=[ob.r, self.ident.r], writes=[tp.r])
            P.op('dve', lambda e, half=half: e.tensor_copy(
                out=oT.t[:, half * 2:half * 2 + 2, :], in_=tp.t[:].rearrange("p (c q) -> p c q", c=2)),
                reads=[tp.r], writes=[oT.r])
        dview = X['oT'][s, row0:row0 + 512, Q0:Q0 + 512].rearrange("(c p) q -> p c q", p=128)
        P.dma('pool', lambda e: e.dma_start(out=dview, in_=oT.t[:]), reads=[oT.r])

    def _finalize(self, c, acc, j, hd, out_ap, out_res):
        P = self.P
        rec = c['rec'].next()
        P.op('dve', lambda e: e.reciprocal(out=rec.t[:], in_=acc.t[:, hd:hd + 1]), reads=[acc.r], writes=[rec.r])
        P.op('dve', lambda e: e.tensor_scalar(out=out_ap, in0=acc.t[:, 0:hd], scalar1=rec.t[:, 0:1], scalar2=None, op0=ALU.mult),
             reads=[acc.r, rec.r], writes=[out_res])

    def mixerA(self, l, s):
        P, I, X, S, NB = self.P, self.I, self.X, self.S, self.NB
        with ExitStack() as st:
            c = self._p2_common(st)
            kT, v1 = self._load_kv(st, s, 'kaT', 'va1', 520)
            bm = self.sbuf(st, 'bm', [128, 8, 8, 512], BF16)
            qb = self.sbufs(st, 'qblk', [128, 4, 512], BF16, 2)
            e_f = self.sbuf(st, 'e_f', [8, 1536], F32)
            e_b = self.sbuf(st, 'e_b', [8, 1536], BF16)
            r_eb = Res()
            P.dma('sp', lambda e: e.dma_start(out=e_f.t[:, 449:769], in_=I['rel_bias_a'][l]), writes=[e_f.r])
            P.op('dve', lambda e: e.tensor_copy(out=e_f.t[:, 0:449], in_=e_f.t[:, 449:450].to_broadcast([8, 449])),
                 reads=[e_f.r], writes=[e_f.r])
            P.op('dve', lambda e: e.tensor_copy(out=e_f.t[:, 769:1536], in_=e_f.t[:, 768:769].to_broadcast([8, 767])),
                 reads=[e_f.r], writes=[e_f.r])
            P.op('dve', lambda e: e.tensor_scalar(out=e_b.t[:], in0=e_f.t[:], scalar1=8.0, scalar2=None, op0=ALU.mult),
                 reads=[e_f.r], writes=[e_b.r])
            P.dma('sp', lambda e: e.dma_start(out=X['ebias'][:, :], in_=e_b.t[:]), reads=[e_b.r], writes=[r_eb])
            for h in range(8):
                src = bass.AP(tensor=X['ebias'].tensor, offset=h * 1536 + 1, ap=[[1, 128], [128, 8], [1, 512]])
                P.dma('sp', lambda e, h=h, src=src: e.dma_start(out=bm.t[:, h], in_=src), reads=[r_eb], writes=[bm.r])
            for t in range(8):
                for ph in range(2):
                    lo_fb = max(0, 2 * t + ph - 8)
                    hi_fb = min(7, 2 * t + ph)
                    if lo_fb > 0:
                        P.op('pool', lambda e, t=t, ph=ph, lo_fb=lo_fb: e.memset(bm.t[(1 - ph) * 64:(2 - ph) * 64, :, 7 - t, 0:64 * lo_fb], NEG),
                             writes=[bm.r])
                    if hi_fb < 7:
                        P.op('pool', lambda e, t=t, ph=ph, hi_fb=hi_fb: e.memset(bm.t[(1 - ph) * 64:(2 - ph) * 64, :, 7 - t, 64 * (hi_fb + 1):512], NEG),
                             writes=[bm.r])
            for b in range(NB):
                Q0 = 512 * b
                q_ = qb.next()
                P.dma('sp', lambda e, q_=q_, Q0=Q0: e.dma_start(
                    out=q_.t[:], in_=X['qaT'][s, :, Q0:Q0 + 512].rearrange("(c p) t -> p c t", p=128)), writes=[q_.r])
                ob = c['oblk'].next()
                tmin = 4 if b == 0 else 0
                for h in range(8):
                    pr = slice((h % 2) * 64, (h % 2) * 64 + 64)
                    for t in range(tmin, 8):
                        K0 = Q0 - 512 + 128 * t
                        sc = c['sc'].next()
                        P.op('pe', lambda e, sc=sc, pr=pr, h=h, K0=K0, q_=q_: e.matmul(
                            sc.t[:], lhsT=kT.t[pr, h // 2, K0:K0 + 128], rhs=q_.t[pr, h // 2, :], start=True, stop=False),
                            reads=[kT.r, q_.r], writes=[sc.r])
                        P.op('pe', lambda e, sc=sc, h=h, t=t: e.matmul(
                            sc.t[:], lhsT=self.flip.t[:], rhs=bm.t[:, h, 7 - t, :], start=False, stop=True),
                            reads=[bm.r, self.flip.r], writes=[sc.r])
                        E = c['E'].next()
                        P.op('act', lambda e, sc=sc, E=E: e.activation(out=E.t[:], in_=sc.t[:], func=AF.Exp, scale=0.125),
                             reads=[sc.r], writes=[E.r])
                        for j in range(4):
                            if j <= t <= j + 4:
                                first = (t == max(j, tmin))
                                last = (t == j + 4)
                                acc = c['acc'][j]
                                P.op('pe', lambda e, acc=acc, E=E, j=j, K0=K0, h=h, first=first, last=last: e.matmul(
                                    acc.t[:, 0:65], lhsT=E.t[:, j * 128:(j + 1) * 128], rhs=v1.t[:, K0 // 128, h * 65:(h + 1) * 65],
                                    start=first, stop=last), reads=[E.r, v1.r], writes=[acc.r])
                    for j in range(4):
                        self._finalize(c, c['acc'][j], j, 64, ob.t[:, j, h * 64:(h + 1) * 64], ob.r)
                self._store_o(c, ob, s, 0, Q0)
            P.barrier()

    def mixerB(self, l, s):
        P, I, X, S, NB = self.P, self.I, self.X, self.S, self.NB
        with ExitStack() as st:
            c = self._p2_common(st)
            kT, v1 = self._load_kv(st, s, 'kbT', 'vb1', 516)
            cm = self.sbuf(st, 'cm', [128, 4, 512], BF16)
            P.dma('sp', lambda e: e.dma_start(out=cm.t[:], in_=I['c_cmask'][:, :, :]), writes=[cm.r])
            qb = self.sbufs(st, 'qblk', [128, 4, 512], BF16, 2)
            obm = [self.sbufs(st, f'obm{i}', [128, 4, 128], F32, 2) for i in range(2)]
            df = self.sbufs(st, 'df', [128, 4, 128], F32, 2)
            sq = self.sbuf(st, 'sq', [128, 4, 128], F32)
            st4 = self.sbufs(st, 'st4', [128, 3, 4], F32, 2)
            for b in range(NB):
                Q0 = 512 * b
                q_ = qb.next()
                P.dma('sp', lambda e, q_=q_, Q0=Q0: e.dma_start(
                    out=q_.t[:], in_=X['qbT'][s, :, Q0:Q0 + 512].rearrange("(c p) t -> p c t", p=128)), writes=[q_.r])
                ob = c['oblk'].next()
                nkt = 4 * b + 4
                for h in range(4):
                    oms = [obm[0].next(), obm[1].next()]
                    for mp in range(2):
                        pr = slice(mp * 64, mp * 64 + 64)
                        for t in range(nkt):
                            K0 = 128 * t
                            rel = t - 4 * b
                            sc = c['sc'].next()
                            P.op('pe', lambda e, sc=sc, pr=pr, h=h, K0=K0, q_=q_, rel=rel: e.matmul(
                                sc.t[:], lhsT=kT.t[pr, h, K0:K0 + 128], rhs=q_.t[pr, h, :], start=True, stop=(rel < 0)),
                                reads=[kT.r, q_.r], writes=[sc.r])
                            if rel >= 0:
                                P.op('pe', lambda e, sc=sc, rel=rel: e.matmul(
                                    sc.t[:], lhsT=self.ident.t[:], rhs=cm.t[:, rel, :], start=False, stop=True),
                                    reads=[cm.r, self.ident.r], writes=[sc.r])
                            E = c['E'].next()
                            P.op('act', lambda e, sc=sc, E=E: e.activation(out=E.t[:], in_=sc.t[:], func=AF.Exp, scale=0.125),
                                 reads=[sc.r], writes=[E.r])
                            for j in range(4):
                                if rel <= j:
                                    acc = c['acc'][j]
                                    P.op('pe', lambda e, acc=acc, E=E, j=j, t=t, h=h, b=b: e.matmul(
                                        acc.t[:, 0:129], lhsT=E.t[:, j * 128:(j + 1) * 128], rhs=v1.t[:, t, h * 129:(h + 1) * 129],
                                        start=(t == 0), stop=(t == 4 * b + j)), reads=[E.r, v1.r], writes=[acc.r])
                        for j in range(4):
                            self._finalize(c, c['acc'][j], j, 128, oms[mp].t[:, j, :], oms[mp].r)
                    d_ = df.next()
                    s4 = st4.next()
                    P.op('dve', lambda e, d_=d_, oms=oms: e.scalar_tensor_tensor(
                        out=d_.t[:], in0=oms[1].t[:], scalar=self.neglam.t[:, l:l + 1], in1=oms[0].t[:], op0=ALU.mult, op1=ALU.add),
                        reads=[oms[0].r, oms[1].r, self.neglam.r], writes=[d_.r])
                    P.op('pool', lambda e, d_=d_: e.tensor_tensor(out=sq.t[:], in0=d_.t[:], in1=d_.t[:], op=ALU.mult),
                         reads=[d_.r], writes=[sq.r])
                    P.op('dve', lambda e, s4=s4: e.tensor_reduce(out=s4.t[:, 0, :], in_=sq.t[:], axis=AX.X, op=ALU.add),
                         reads=[sq.r], writes=[s4.r])
                    P.op('dve', lambda e, s4=s4: e.tensor_scalar(out=s4.t[:, 1, :], in0=s4.t[:, 0, :], scalar1=1.0 / 128.0, scalar2=EPS,
                                                                op0=ALU.mult, op1=ALU.add), reads=[s4.r], writes=[s4.r])
                    P.op('pool', lambda e, s4=s4: e.tensor_tensor(out=s4.t[:, 2, :], in0=s4.t[:, 1, :], in1=self.neghalf.t[:, 0:4], op=ALU.pow),
                         reads=[s4.r, self.neghalf.r], writes=[s4.r])
                    P.op('dve', lambda e, d_=d_, s4=s4, ob=ob, h=h: e.tensor_tensor(
                        out=ob.t[:, :, h * 128:(h + 1) * 128], in0=d_.t[:], in1=s4.t[:, 2, :].unsqueeze(2).to_broadcast([128, 4, 128]), op=ALU.mult),
                        reads=[d_.r, s4.r], writes=[ob.r])
                self._store_o(c, ob, s, 512, Q0)
            P.barrier()

    def mixerC(self, l, s):
        P, I, X, S, NB, NT = self.P, self.I, self.X, self.S, self.NB, self.NT
        with ExitStack() as st:
            c = self._p2_common(st)
            kT, v1 = self._load_kv(st, s, 'kcT', 'vc1', 520)
            kiT = self.sbuf(st, 'kiT', [128, S], BF16)
            P.dma('sp', lambda e: e.dma_start(out=kiT.t[:], in_=X['kiT'][s]), writes=[kiT.r])
            qb = self.sbufs(st, 'qblk', [128, 4, 512], BF16, 2)
            qib = self.sbufs(st, 'qiblk', [128, 2, 512], BF16, 2)
            wib = self.sbufs(st, 'wiblk', [128, 4, 4], F32, 2)
            I_sb = self.sbuf(st, 'I_sb', [128, S], F32)
            junkI = self.sbuf(st, 'junkI', [128, S], BF16)
            Mq = self.sbuf(st, 'Mq', [128, S], BF16)
            negm = self.sbuf(st, 'negm', [128, NT, 512], BF16)
            rl = self.sbufs(st, 'rl', [128, 512], BF16, 8)
            dg = self.sbufs(st, 'dg', [128, 128], BF16, 8)
            pw = self.sbuf(st, 'pw', [128, NIT + 1], F32)
            thr0 = self.sbuf(st, 'thr0', [128, 1], F32)
            for i in range(NIT + 1):
                P.op('pool', lambda e, i=i: e.memset(pw.t[:, i:i + 1], 2.0 ** -(i + 1)), writes=[pw.r])
            P.op('pool', lambda e: e.memset(thr0.t[:], -1e29), writes=[thr0.r])
            sm = self.sbufs(st, 'bsm', [128, 4], F32, 2)
            halfs = self.sbufs(st, 'halfs', [128, NIT + 1], F32, 2)
            mid = self.sbufs(st, 'mid', [128, 1], F32, 4)
            cnt = self.sbufs(st, 'cnt', [128, 1], F32, 4)
            tsel = self.sbufs(st, 'tsel', [128, 1], F32, 4)
            for b in range(NB):
                Q0 = 512 * b
                nkt = 4 * b + 4
                q_, qi_, wi_ = qb.next(), qib.next(), wib.next()
                P.dma('sp', lambda e, q_=q_, Q0=Q0: e.dma_start(
                    out=q_.t[:], in_=X['qcT'][s, :, Q0:Q0 + 512].rearrange("(c p) t -> p c t", p=128)), writes=[q_.r])
                P.dma('sp', lambda e, qi_=qi_, Q0=Q0: e.dma_start(
                    out=qi_.t[:], in_=X['qiT'][s, :, Q0:Q0 + 512].rearrange("(c p) t -> p c t", p=128)), writes=[qi_.r])
                P.dma('sp', lambda e, wi_=wi_, Q0=Q0: e.dma_start(
                    out=wi_.t[:], in_=X['wi'][s, Q0:Q0 + 512, :].rearrange("(j p) h -> p j h", p=128)), writes=[wi_.r])
                for jj in range(4):
                    m = 4 * b + jj
                    n_k = 128 * (m + 1)
                    dgs = []
                    for h in range(4):
                        d_ = dg.next()
                        P.op('dve', lambda e, d_=d_, wi_=wi_, jj=jj, h=h: e.tensor_scalar(
                            out=d_.t[:], in0=self.ident.t[:], scalar1=wi_.t[:, jj, h:h + 1], scalar2=None, op0=ALU.mult),
                            reads=[self.ident.r, wi_.r], writes=[d_.r])
                        dgs.append(d_)
                    for k0 in range(0, n_k, 512):
                        w = min(512, n_k - k0)
                        rls = []
                        for h in range(4):
                            pr = slice((h % 2) * 64, (h % 2) * 64 + 64)
                            sc = c['sc'].next()
                            P.op('pe', lambda e, sc=sc, pr=pr, h=h, jj=jj, k0=k0, w=w, qi_=qi_: e.matmul(
                                sc.t[:, 0:w], lhsT=qi_.t[pr, h // 2, jj * 128:(jj + 1) * 128], rhs=kiT.t[pr, k0:k0 + w], start=True, stop=True),
                                reads=[qi_.r, kiT.r], writes=[sc.r])
                            r_ = rl.next()
                            if h % 2 == 0:
                                P.op('act', lambda e, sc=sc, r_=r_, w=w: e.activation(out=r_.t[:, 0:w], in_=sc.t[:, 0:w], func=AF.Relu),
                                     reads=[sc.r], writes=[r_.r])
                            else:
                                P.op('dve', lambda e, sc=sc, r_=r_, w=w: e.tensor_scalar(out=r_.t[:, 0:w], in0=sc.t[:, 0:w], scalar1=0.0, scalar2=None, op0=ALU.max),
                                     reads=[sc.r], writes=[r_.r])
                            rls.append(r_)
                        accI = c['acc'][(k0 // 512) % 4]
                        for h in range(4):
                            P.op('pe', lambda e, accI=accI, h=h, w=w, dgs=dgs, rls=rls: e.matmul(
                                accI.t[:, 0:w], lhsT=dgs[h].t[:], rhs=rls[h].t[:, 0:w], start=(h == 0), stop=(h == 3)),
                                reads=[dgs[h].r, rls[h].r], writes=[accI.r])
                        P.op('act', lambda e, accI=accI, k0=k0, w=w: e.activation(out=I_sb.t[:, k0:k0 + w], in_=accI.t[:, 0:w], func=AF.Copy),
                             reads=[accI.r], writes=[I_sb.r])
                    if m >= 2:
                        sm_ = sm.next()
                        hf = halfs.next()
                        P.op('dve', lambda e, sm_=sm_, n_k=n_k: e.tensor_reduce(out=sm_.t[:, 0:1], in_=I_sb.t[:, 0:n_k], axis=AX.X, op=ALU.max),
                             reads=[I_sb.r], writes=[sm_.r])
                        P.op('dve', lambda e, sm_=sm_, n_k=n_k: e.tensor_reduce(out=sm_.t[:, 1:2], in_=I_sb.t[:, 0:n_k], axis=AX.X, op=ALU.min),
                             reads=[I_sb.r], writes=[sm_.r])
                    P.op('dve', lambda e, n_k=n_k: e.memset(I_sb.t[0:64, n_k - 64:n_k], -1e30), writes=[I_sb.r])
                    if m >= 2:
                        P.op('dve', lambda e, sm_=sm_: e.tensor_tensor(out=sm_.t[:, 2:3], in0=sm_.t[:, 0:1], in1=sm_.t[:, 1:2], op=ALU.subtract),
                             reads=[sm_.r], writes=[sm_.r])
                        P.op('dve', lambda e, sm_=sm_, hf=hf: e.tensor_scalar(out=hf.t[:], in0=pw.t[:], scalar1=sm_.t[:, 2:3], scalar2=None, op0=ALU.mult),
                             reads=[sm_.r, pw.r], writes=[hf.r])
                        md = mid.next()
                        P.op('dve', lambda e, md=md, sm_=sm_, hf=hf: e.tensor_tensor(out=md.t[:], in0=sm_.t[:, 1:2], in1=hf.t[:, 0:1], op=ALU.add),
                             reads=[sm_.r, hf.r], writes=[md.r])
                        for it in range(NIT):
                            cn, ts, md2 = cnt.next(), tsel.next(), mid.next()
                            P.op('dve', lambda e, cn=cn, md=md, n_k=n_k: e.tensor_scalar(
                                out=junkI.t[:, 0:n_k], in0=I_sb.t[:, 0:n_k], scalar1=md.t[:, 0:1], scalar2=None,
                                op0=ALU.is_ge, op1=ALU.add, accum_out=cn.t[:, 0:1]),
                                reads=[I_sb.r, md.r], writes=[junkI.r, cn.r])
                            P.op('dve', lambda e, cn=cn, ts=ts, hf=hf, it=it: e.tensor_scalar(
                                out=ts.t[:], in0=cn.t[:], scalar1=float(TOPK), scalar2=hf.t[:, it:it + 1], op0=ALU.is_ge, op1=ALU.mult),
                                reads=[cn.r, hf.r], writes=[ts.r])
                            P.op('dve', lambda e, md=md, md2=md2, ts=ts, hf=hf, it=it: e.scalar_tensor_tensor(
                                out=md2.t[:], in0=md.t[:], scalar=hf.t[:, it + 1:it + 2], in1=ts.t[:], op0=ALU.subtract, op1=ALU.add),
                                reads=[md.r, ts.r, hf.r], writes=[md2.r])
                            md = md2
                        P.op('dve', lambda e, md=md, sm_=sm_, hf=hf: e.tensor_tensor(out=sm_.t[:, 3:4], in0=md.t[:], in1=hf.t[:, NIT:NIT + 1], op=ALU.subtract),
                             reads=[md.r, hf.r], writes=[sm_.r])
                        thr_ap, thr_res = sm_.t[:, 3:4], sm_.r
                    else:
                        thr_ap, thr_res = thr0.t[:, 0:1], thr0.r
                    P.op('dve', lambda e, n_k=n_k, thr_ap=thr_ap: e.tensor_scalar(
                        out=Mq.t[:, 0:n_k], in0=I_sb.t[:, 0:n_k], scalar1=thr_ap, scalar2=None, op0=ALU.is_ge),
                        reads=[I_sb.r, thr_res], writes=[Mq.r])
                    if self.debug and s == 0 and l == 0:
                        P.dma('sp', lambda e, m=m: e.dma_start(out=X['dbgI'][m], in_=I_sb.t[:]), reads=[I_sb.r])
                        P.dma('sp', lambda e, m=m: e.dma_start(out=X['dbgM'][m], in_=Mq.t[:]), reads=[Mq.r])
                        if m >= 2:
                            P.dma('sp', lambda e, m=m, sm_=sm_: e.dma_start(out=X['dbgthr'][m], in_=sm_.t[:]), reads=[sm_.r])
                    tp = c['tp']
                    for t0 in range(0, m + 1, 8):
                        nt_ = min(8, m + 1 - t0)
                        for tt in range(nt_):
                            P.op('pe', lambda e, t0=t0, tt=tt: e.transpose(
                                out=tp.t[:, tt * 128:(tt + 1) * 128], in_=Mq.t[:, (t0 + tt) * 128:(t0 + tt + 1) * 128], identity=self.ident.t[:]),
                                reads=[Mq.r, self.ident.r], writes=[tp.r])
                        P.op('dve', lambda e, t0=t0, nt_=nt_, jj=jj: e.tensor_scalar(
                            out=negm.t[:, t0:t0 + nt_, jj * 128:(jj + 1) * 128],
                            in0=tp.t[:, 0:nt_ * 128].rearrange("p (t q) -> p t q", t=nt_),
                            scalar1=-1.0, scalar2=-NEG, op0=ALU.add, op1=ALU.mult), reads=[tp.r], writes=[negm.r])
                    if m + 1 < nkt:
                        P.op('pool', lambda e, m=m, nkt=nkt, jj=jj: e.memset(negm.t[:, m + 1:nkt, jj * 128:(jj + 1) * 128], NEG), writes=[negm.r])
                ob = c['oblk'].next()
                for h in range(8):
                    pr = slice((h % 2) * 64, (h % 2) * 64 + 64)
                    for t in range(nkt):
                        K0 = 128 * t
                        sc = c['sc'].next()
                        P.op('pe', lambda e, sc=sc, pr=pr, h=h, K0=K0, q_=q_: e.matmul(
                            sc.t[:], lhsT=kT.t[pr, h // 2, K0:K0 + 128], rhs=q_.t[pr, h // 2, :], start=True, stop=False),
                            reads=[kT.r, q_.r], writes=[sc.r])
                        P.op('pe', lambda e, sc=sc, t=t: e.matmul(
                            sc.t[:], lhsT=self.ident.t[:], rhs=negm.t[:, t, :], start=False, stop=True),
                            reads=[negm.r, self.ident.r], writes=[sc.r])
                        E = c['E'].next()
                        P.op('act', lambda e, sc=sc, E=E: e.activation(out=E.t[:], in_=sc.t[:], func=AF.Exp, scale=0.125),
                             reads=[sc.r], writes=[E.r])
                        for j in range(4):
                            if t <= 4 * b + j:
                                acc = c['acc'][j]
                                P.op('pe', lambda e, acc=acc, E=E, j=j, t=t, h=h, b=b: e.matmul(
                                    acc.t[:, 0:65], lhsT=E.t[:, j * 128:(j + 1) * 128], rhs=v1.t[:, t, h * 65:(h + 1) * 65],
                                    start=(t == 0), stop=(t == 4 * b + j)), reads=[E.r, v1.r], writes=[acc.r])
                    for j in range(4):
                        self._finalize(c, c['acc'][j], j, 64, ob.t[:, j, h * 64:(h + 1) * 64], ob.r)
                self._store_o(c, ob, s, 1024, Q0)
            P.barrier()

    def load_x_block(self, l, s, T0, ntile, xts, xbp, ssp, junk, tpp, dstT, dst_res, load=True):
        P = self.P
        for j in range(ntile):
            x_t = xts[j]
            if load:
                src = self.x_src(l)[s, T0 + j * 128:T0 + (j + 1) * 128, :]
                P.dma('sp', lambda e, x_t=x_t, src=src: e.dma_start(out=x_t.t[:], in_=src), writes=[x_t.r])
            x_b, ss, pt = xbp.next(), ssp.next(), tpp.next()
            self.rms_to_bf16(x_t, junk, ss, x_b)
            self.transpose_to(x_b, pt, dstT, j * 128, 128, 'dve', dst_res=dst_res[j])

    def phase3a(self, l):
        P, I, X, S = self.P, self.I, self.X, self.S
        with ExitStack() as st:
            wg = self.sbuf(st, 'wg', [128, NCH, 3072], BF16)
            wu = self.sbuf(st, 'wu', [128, 12, D], BF16)
            wo = self.sbuf(st, 'wo', [128, NCH, D], BF16)
            wq = self.sbuf(st, 'wq', [128, NCH, 256], BF16)
            wox = self.sbuf(st, 'wox', [128, 2, D], BF16)
            wkv = self.sbuf(st, 'wkv', [128, NCH, 512], BF16)
            gn = self.gains
            with ExitStack() as st2:
                stage = self.sbufs(st2, 'stage', [128, 1024], F32, 2)
                self.load_weight(stage, wg, lambda c, c0, c1: wg.t[:, c, c0:c1], I['w_in'][l][:, 4932:8004], NCH, 3072,
                                 lambda c: (gn.t[:, 0, l, c, :], gn.r))
                self.load_weight(stage, wu, lambda c, c0, c1: wu.t[:, c, c0:c1], I['w_up_a'][l], 4, D, lambda c: None)
                self.load_weight(stage, wu, lambda c, c0, c1: wu.t[:, 4 + c, c0:c1], I['w_up_b'][l], 4, D,
                                 lambda c: (self.subln.t[:, l, :], self.subln.r), const=1.0 - self.lam_init(l))
                self.load_weight(stage, wu, lambda c, c0, c1: wu.t[:, 8 + c, c0:c1], I['w_up_c'][l], 4, D, lambda c: None)
                self.load_weight(stage, wo, lambda c, c0, c1: wo.t[:, c, c0:c1], I['w_out'][l], NCH, D, lambda c: None, const=0.5)
                self.load_weight(stage, wq, lambda c, c0, c1: wq.t[:, c, c0:c1], I['w_q_x'][l], NCH, 256,
                                 lambda c: (gn.t[:, 1, l, c, :], gn.r))
                self.load_weight(stage, wox, lambda c, c0, c1: wox.t[:, c, c0:c1], I['w_o_x'][l], 2, D, lambda c: None)
                self.load_weight(stage, wkv, lambda c, c0, c1: wkv.t[:, c, c0:c1], I['w_kv_x'][l], NCH, 512,
                                 lambda c: (self.gmem.t[:, c, :], self.gmem.r))
                P.barrier()
            tp = self.pss(st, 'tp', [128, D], BF16, 1)
            pm = self.pss(st, 'pm', [128, 512], F32, 3)
            acc = [self.ps(st, f'acc{i}', [128, 512], F32) for i in range(4)]
            kmT = [self.sbuf(st, f'kmT{s}', [128, 2, MEM], BF16) for s in range(self.NSEQ)]
            vm1 = [self.sbuf(st, f'vm1{s}', [128, 2, 4, 65], BF16) for s in range(self.NSEQ)]
            for s in range(self.NSEQ):
                P.op('dve', lambda e, s=s: e.memset(vm1[s].t[:], 1.0), writes=[vm1[s].r])
                for c2 in range(2):
                    pk = pm.next()
                    for c in range(NCH):
                        P.op('pe', lambda e, c=c, c2=c2, pk=pk, s=s: e.matmul(
                            pk.t[:, 0:MEM], lhsT=wkv.t[:, c, c2 * 128:(c2 + 1) * 128], rhs=self.memT[s].t[:, c, :],
                            start=(c == 0), stop=(c == NCH - 1)), reads=[wkv.r, self.memT[s].r], writes=[pk.r])
                    P.op('act', lambda e, c2=c2, pk=pk, s=s: e.activation(out=kmT[s].t[:, c2, :], in_=pk.t[:, 0:MEM], func=AF.Copy),
                         reads=[pk.r], writes=[kmT[s].r])
                for t in range(2):
                    pv = pm.next()
                    for c in range(NCH):
                        P.op('pe', lambda e, c=c, t=t, pv=pv, s=s: e.matmul(
                            pv.t[:, 0:256], lhsT=self.memT[s].t[:, c, t * 128:(t + 1) * 128], rhs=wkv.t[:, c, 256:512],
                            start=(c == 0), stop=(c == NCH - 1)), reads=[wkv.r, self.memT[s].r], writes=[pv.r])
                    P.op('act', lambda e, t=t, pv=pv, s=s: e.activation(
                        out=vm1[s].t[:, t, :, 0:64], in_=pv.t[:, 0:256].rearrange("p (h d) -> p h d", h=4), func=AF.Copy),
                        reads=[pv.r], writes=[vm1[s].r])
            xts = [self.sbuf(st, f'xt{j}', [128, D], F32) for j in range(4)]
            junk = self.sbuf(st, 'junk', [128, D], BF16)
            xbp = self.sbufs(st, 'xb', [128, D], BF16, 2)
            ssp = self.sbufs(st, 'ss', [128, 2], F32, 4)
            xnT = self.sbuf(st, 'xnT', [128, NCH, 512], BF16)
            xnT_res = [Res() for _ in range(4)]
            oTb = self.sbuf(st, 'oTb', [128, 12, 512], BF16)
            sgp = self.sbufs(st, 'sg', [128, 512], F32, 3)
            ttp = self.sbufs(st, 'tt', [128, 512], F32, 3)
            ysum = self.sbufs(st, 'ysum', [128, 512], F32, 2)
            yT = self.sbuf(st, 'yT', [128, NCH, 512], BF16)
            yT_res = [Res() for _ in range(NCH)]
            qxT = self.sbuf(st, 'qxT', [128, 2, 512], BF16)
            Ep = self.sbufs(st, 'E', [128, 512], BF16, 3)
            recp = self.sbufs(st, 'rec', [128, 1], F32, 8)
            ox = self.sbuf(st, 'ox', [128, 4, 256], BF16)
            oxT = self.sbuf(st, 'oxT', [128, 2, 512], BF16)
            for s in range(self.NSEQ):
                for tb in range(self.NB):
                    T0 = tb * 512
                    self.load_x_block(l, s, T0, 4, xts, xbp, ssp, junk, tp, xnT, xnT_res)
                    P.dma('sp', lambda e, s=s, T0=T0: e.dma_start(
                        out=oTb.t[:], in_=X['oT'][s, :, T0:T0 + 512].rearrange("(c p) q -> p c q", p=128)), writes=[oTb.r])
                    for f in range(NCH):
                        tts = []
                        for i in range(3):
                            pg = pm.next()
                            for c in range(NCH):
                                P.op('pe', lambda e, c=c, i=i, f=f, pg=pg: e.matmul(
                                    pg.t[:], lhsT=wg.t[:, c, i * 1024 + f * 128: i * 1024 + (f + 1) * 128], rhs=xnT.t[:, c, :],
                                    start=(c == 0), stop=(c == NCH - 1)), reads=[wg.r] + xnT_res, writes=[pg.r])
                            sg = sgp.next()
                            P.op('act', lambda e, sg=sg, pg=pg: e.activation(out=sg.t[:], in_=pg.t[:], func=AF.Tanh, scale=0.5),
                                 reads=[pg.r], writes=[sg.r])
                            pu = pm.next()
                            for c in range(4):
                                P.op('pe', lambda e, c=c, i=i, f=f, pu=pu: e.matmul(
                                    pu.t[:], lhsT=wu.t[:, 4 * i + c, f * 128:(f + 1) * 128], rhs=oTb.t[:, 4 * i + c, :],
                                    start=(c == 0), stop=(c == 3)), reads=[wu.r, oTb.r], writes=[pu.r])
                            tt = ttp.next()
                            P.op('dve', lambda e, sg=sg, pu=pu, tt=tt: e.scalar_tensor_tensor(
                                out=tt.t[:], in0=sg.t[:], scalar=1.0, in1=pu.t[:], op0=ALU.add, op1=ALU.mult),
                                reads=[sg.r, pu.r], writes=[tt.r])
                            tts.append(tt)
                        ys = ysum.next()
                        P.op('pool', lambda e, ys=ys, tts=tts: e.tensor_tensor(out=ys.t[:], in0=tts[0].t[:], in1=tts[1].t[:], op=ALU.add),
                             reads=[tts[0].r, tts[1].r], writes=[ys.r])
                        P.op('pool', lambda e, ys=ys, tts=tts, f=f: e.tensor_tensor(out=yT.t[:, f, :], in0=ys.t[:], in1=tts[2].t[:], op=ALU.add),
                             reads=[ys.r, tts[2].r], writes=[yT_res[f]])
                    for j in range(4):
                        for hf in range(2):
                            po = pm.next()
                            for f in range(NCH):
                                P.op('pe', lambda e, f=f, j=j, hf=hf, po=po: e.matmul(
                                    po.t[:], lhsT=yT.t[:, f, j * 128:(j + 1) * 128], rhs=wo.t[:, f, hf * 512:(hf + 1) * 512],
                                    start=(f == 0), stop=(f == NCH - 1)), reads=[wo.r] + yT_res, writes=[po.r])
                            P.op('dve', lambda e, j=j, hf=hf, po=po: e.tensor_tensor(
                                out=xts[j].t[:, hf * 512:(hf + 1) * 512], in0=po.t[:], in1=xts[j].t[:, hf * 512:(hf + 1) * 512], op=ALU.add),
                                reads=[po.r, xts[j].r], writes=[xts[j].r])
                    self.load_x_block(l, s, T0, 4, xts, xbp, ssp, junk, tp, xnT, xnT_res, load=False)
                    for c2 in range(2):
                        pq = pm.next()
                        for c in range(NCH):
                            P.op('pe', lambda e, c=c, c2=c2, pq=pq: e.matmul(
                                pq.t[:], lhsT=wq.t[:, c, c2 * 128:(c2 + 1) * 128], rhs=xnT.t[:, c, :],
                                start=(c == 0), stop=(c == NCH - 1)), reads=[wq.r] + xnT_res, writes=[pq.r])
                        P.op('act', lambda e, c2=c2, pq=pq: e.activation(out=qxT.t[:, c2, :], in_=pq.t[:], func=AF.Copy),
                             reads=[pq.r], writes=[qxT.r])
                    for h in range(4):
                        pr = slice((h % 2) * 64, (h % 2) * 64 + 64)
                        for t in range(2):
                            sc = pm.next()
                            P.op('pe', lambda e, sc=sc, pr=pr, h=h, t=t, s=s: e.matmul(
                                sc.t[:], lhsT=kmT[s].t[pr, h // 2, t * 128:(t + 1) * 128], rhs=qxT.t[pr, h // 2, :], start=True, stop=True),
                                reads=[kmT[s].r, qxT.r], writes=[sc.r])
                            E = Ep.next()
                            P.op('act', lambda e, sc=sc, E=E: e.activation(out=E.t[:], in_=sc.t[:], func=AF.Exp, scale=0.125),
                                 reads=[sc.r], writes=[E.r])
                            for j in range(4):
                                P.op('pe', lambda e, E=E, j=j, t=t, h=h, s=s: e.matmul(
                                    acc[j].t[:, 0:65], lhsT=E.t[:, j * 128:(j + 1) * 128], rhs=vm1[s].t[:, t, h, :],
                                    start=(t == 0), stop=(t == 1)), reads=[E.r, vm1[s].r], writes=[acc[j].r])
                        for j in range(4):
                            rec = recp.next()
                            P.op('dve', lambda e, rec=rec, j=j: e.reciprocal(out=rec.t[:], in_=acc[j].t[:, 64:65]), reads=[acc[j].r], writes=[rec.r])
                            P.op('dve', lambda e, rec=rec, j=j, h=h: e.tensor_scalar(
                                out=ox.t[:, j, h * 64:(h + 1) * 64], in0=acc[j].t[:, 0:64], scalar1=rec.t[:, 0:1], scalar2=None, op0=ALU.mult),
                                reads=[acc[j].r, rec.r], writes=[ox.r])
                    pt = tp.next()
                    for c2 in range(2):
                        for j in range(4):
                            P.op('pe', lambda e, c2=c2, j=j, pt=pt: e.transpose(
                                out=pt.t[:, (c2 * 4 + j) * 128:(c2 * 4 + j + 1) * 128], in_=ox.t[:, j, c2 * 128:(c2 + 1) * 128], identity=self.ident.t[:]),
                                reads=[ox.r, self.ident.r], writes=[pt.r])
                    P.op('dve', lambda e, pt=pt: e.tensor_copy(out=oxT.t[:], in_=pt.t[:].rearrange("p (c q) -> p c q", c=2)),
                         reads=[pt.r], writes=[oxT.r])
                    for j in range(4):
                        for hf in range(2):
                            po = pm.next()
                            for c2 in range(2):
                                P.op('pe', lambda e, c2=c2, j=j, hf=hf, po=po: e.matmul(
                                    po.t[:], lhsT=oxT.t[:, c2, j * 128:(j + 1) * 128], rhs=wox.t[:, c2, hf * 512:(hf + 1) * 512],
                                    start=(c2 == 0), stop=(c2 == 1)), reads=[wox.r, oxT.r], writes=[po.r])
                            P.op('dve', lambda e, j=j, hf=hf, po=po: e.tensor_tensor(
                                out=xts[j].t[:, hf * 512:(hf + 1) * 512], in0=po.t[:], in1=xts[j].t[:, hf * 512:(hf + 1) * 512], op=ALU.add),
                                reads=[po.r, xts[j].r], writes=[xts[j].r])
                        dview = X['xres'][s, T0 + j * 128:T0 + (j + 1) * 128, :]
                        P.dma('pool', lambda e, j=j, dview=dview: e.dma_start(out=dview, in_=xts[j].t[:]), reads=[xts[j].r])
            P.barrier()

    def phase3b(self, l):
        P, I, X, S = self.P, self.I, self.X, self.S
        last = (l == self.DEPTH - 1)
        TB = 256
        with ExitStack() as st:
            wgu = self.sbuf(st, 'wgu', [128, NCH, 2 * DFF], BF16)
            wd = self.sbuf(st, 'wd', [128, NFF, D], BF16)
            gn = self.gains
            with ExitStack() as st2:
                stage = self.sbufs(st2, 'stage', [128, 1408], F32, 2)
                self.load_weight(stage, wgu, lambda c, c0, c1: wgu.t[:, c, c0:c1], I['w_gu'][l], NCH, 2 * DFF,
                                 lambda c: (gn.t[:, 2, l, c, :], gn.r))
                self.load_weight(stage, wd, lambda c, c0, c1: wd.t[:, c, c0:c1], I['w_down'][l], NFF, D, lambda c: None)
                P.barrier()
            tp = self.pss(st, 'tp', [128, D], BF16, 2)
            pm = self.pss(st, 'pm', [128, 512], F32, 6)
            xts = [self.sbuf(st, f'xt{j}', [128, D], F32) for j in range(4)]
            junk = self.sbuf(st, 'junk', [128, D], BF16)
            xbp = self.sbufs(st, 'xb', [128, D], BF16, 2)
            ssp = self.sbufs(st, 'ss', [128, 2], F32, 4)
            hnT = [self.sbuf(st, f'hnT{i}', [128, NCH, TB], BF16) for i in range(2)]
            hnT_res = [[Res() for _ in range(2)] for _ in range(2)]
            hT = self.sbuf(st, 'hT', [128, NFF, TB], BF16)
            hT_res = [Res() for _ in range(NFF)]
            slp = self.sbufs(st, 'sl', [128, TB], F32, 3)
            if last:
                fin_g = self.sbuf(st, 'fin_g', [128, D], F32)
                P.dma('sp', lambda e: e.dma_start(out=fin_g.t[:], in_=I['final_norm'].partition_broadcast(128)), writes=[fin_g.r])
                outp = self.sbufs(st, 'outp', [128, D], F32, 2)
            blk = 0
            for s in range(self.NSEQ):
                for tb in range(S // TB):
                    T0 = tb * TB
                    xs = xts[(blk % 2) * 2:(blk % 2) * 2 + 2]
                    hn, hr = hnT[blk % 2], hnT_res[blk % 2]
                    blk += 1
                    self.load_x_block(l + 1, s, T0, 2, xs, xbp, ssp, junk, tp, hn, hr)
                    for f in range(NFF):
                        pg, pu = pm.next(), pm.next()
                        for (pp, off) in ((pg, 0), (pu, DFF)):
                            for c in range(NCH):
                                P.op('pe', lambda e, c=c, f=f, pp=pp, off=off, hn=hn: e.matmul(
                                    pp.t[:, 0:TB], lhsT=wgu.t[:, c, off + f * 128: off + (f + 1) * 128], rhs=hn.t[:, c, :],
                                    start=(c == 0), stop=(c == NCH - 1)), reads=[wgu.r] + hr, writes=[pp.r])
                        sl = slp.next()
                        P.op('act', lambda e, sl=sl, pg=pg: e.activation(out=sl.t[:], in_=pg.t[:, 0:TB], func=AF.Silu),
                             reads=[pg.r], writes=[sl.r])
                        P.op('dve', lambda e, sl=sl, pu=pu, f=f: e.tensor_tensor(out=hT.t[:, f, :], in0=pu.t[:, 0:TB], in1=sl.t[:], op=ALU.mult),
                             reads=[pu.r, sl.r], writes=[hT_res[f]])
                    for j in range(2):
                        for hf in range(2):
                            po = pm.next()
                            for f in range(NFF):
                                P.op('pe', lambda e, f=f, j=j, hf=hf, po=po: e.matmul(
                                    po.t[:], lhsT=hT.t[:, f, j * 128:(j + 1) * 128], rhs=wd.t[:, f, hf * 512:(hf + 1) * 512],
                                    start=(f == 0), stop=(f == NFF - 1)), reads=[wd.r] + hT_res, writes=[po.r])
                            P.op('dve', lambda e, j=j, hf=hf, po=po, xs=xs: e.tensor_tensor(
                                out=xs[j].t[:, hf * 512:(hf + 1) * 512], in0=po.t[:], in1=xs[j].t[:, hf * 512:(hf + 1) * 512], op=ALU.add),
                                reads=[po.r, xs[j].r], writes=[xs[j].r])
                        row0 = T0 + j * 128
                        if not last:
                            dview = X['xres'][s, row0:row0 + 128, :]
                            P.dma('pool', lambda e, j=j, dview=dview, xs=xs: e.dma_start(out=dview, in_=xs[j].t[:]), reads=[xs[j].r])
                        else:
                            ss = ssp.next()
                            o_ = outp.next()
                            x_t = xs[j]
                            P.op('act', lambda e, x_t=x_t, ss=ss: e.activation(out=junk.t[:], in_=x_t.t[:], func=AF.Square, accum_out=ss.t[:, 0:1]),
                                 reads=[x_t.r], writes=[junk.r, ss.r])
                            P.op('dve', lambda e, ss=ss: e.tensor_scalar(out=ss.t[:, 1:2], in0=ss.t[:, 0:1], scalar1=1.0 / D, scalar2=EPS,
                                                                        op0=ALU.mult, op1=ALU.add), reads=[ss.r], writes=[ss.r])
                            P.op('pool', lambda e, ss=ss: e.tensor_tensor(out=ss.t[:, 0:1], in0=ss.t[:, 1:2], in1=self.neghalf.t[:, 0:1], op=ALU.pow),
                                 reads=[ss.r, self.neghalf.r], writes=[ss.r])
                            P.op('dve', lambda e, x_t=x_t, ss=ss, o_=o_: e.scalar_tensor_tensor(
                                out=o_.t[:], in0=x_t.t[:], scalar=ss.t[:, 0:1], in1=fin_g.t[:], op0=ALU.mult, op1=ALU.mult),
                                reads=[x_t.r, ss.r, fin_g.r], writes=[o_.r])
                            dview = self.out[s, row0:row0 + 128, :]
                            P.dma('pool', lambda e, dview=dview, o_=o_: e.dma_start(out=dview, in_=o_.t[:]), reads=[o_.r])
            P.barrier()


def make_consts(S):
    bf = ml_dtypes.bfloat16
    ident = np.eye(128, dtype=np.float32).astype(bf)
    rot = np.zeros((128, 128), np.float32)
    for blk in range(2):
        for j in range(32):
            rot[blk * 64 + j + 32, blk * 64 + j] = -1.0
            rot[blk * 64 + j, blk * 64 + j + 32] = 1.0
    pos = np.arange(S, dtype=np.float32)
    inv = (10000.0 ** (-np.arange(0, 64, 2, dtype=np.float32) / 64)).astype(np.float32)
    ang = pos[None, :] * inv[:, None]
    cos = np.tile(np.cos(ang), (4, 1)).astype(bf)
    sin = np.tile(np.sin(ang), (4, 1)).astype(bf)
    cm = np.zeros((128, 4, 512), np.float32)
    for t in range(4):
        for p in range(128):
            kc = 2 * t + p // 64
            for fb in range(8):
                if kc > fb:
                    cm[p, t, fb * 64:(fb + 1) * 64] = NEG
    flip = np.eye(128, dtype=np.float32)[::-1].copy().astype(bf)
    return {'c_ident': ident, 'c_flip': flip, 'c_rot': rot.astype(bf), 'c_cos': cos, 'c_sin': sin, 'c_cmask': cm.astype(bf)}


_CACHE = {}


def kernel(**inputs):
    x = np.asarray(inputs['x'], np.float32)
    B, S, _ = x.shape
    NCORES = 8
    NSEQ = B // NCORES
    DEPTH = inputs['w_in'].shape[0]
    key = (S, NSEQ, DEPTH)
    if key not in _CACHE:
        _CACHE[key] = Builder(S, NSEQ, DEPTH).build()
    nc = _CACHE[key]
    consts = make_consts(S)
    shared = {k: np.ascontiguousarray(np.asarray(v, np.float32)) for k, v in inputs.items() if k not in ('x', 'mem')}
    shared['lambda_vecs'] = shared['lambda_vecs'].reshape(DEPTH, 256)
    shared.update(consts)
    mem = np.asarray(inputs['mem'], np.float32)
    in_maps = []
    for c in range(NCORES):
        m = dict(shared)
        m['x'] = np.ascontiguousarray(x[c * NSEQ:(c + 1) * NSEQ])
        m['mem'] = np.ascontiguousarray(mem[c * NSEQ:(c + 1) * NSEQ])
        in_maps.append(m)
    res = run_bass_kernel_spmd(nc, in_maps, core_ids=list(range(NCORES)))
    return np.concatenate([r['out'] for r in res.results], axis=0).astype(np.float32)
```

```python
import math
import os
KSKIP = os.environ.get('KSKIP', '')
from contextlib import ExitStack

import numpy as np
import ml_dtypes

import concourse.bass as bass
import concourse.mybir as mybir
from concourse.bass_utils import run_bass_kernel_spmd

F32 = mybir.dt.float32
BF16 = mybir.dt.bfloat16
AF = mybir.ActivationFunctionType
ALU = mybir.AluOpType
AX = mybir.AxisListType

LIMIT = 30000
ENGS = ['pe', 'act', 'dve', 'pool', 'sp']

D = 1024
NCH = 8
MEM = 256
DFF = 2816
NFF = 22
N_IN = 8004
EPS = 1e-6
NEG = -240000.0
NIT = 18
TOPK = 256


class Res:
    __slots__ = ('name', 'w', 'r')

    def __init__(self, name=''):
        self.name = name
        self.w = None
        self.r = []


class Buf:
    __slots__ = ('t', 'r')

    def __init__(self, t, name=''):
        self.t = t
        self.r = Res(name)


class Prog:
    def __init__(self, nc, stack, dma_pool=None):
        self.nc = nc
        self.stack = stack
        self.streams = {e: [] for e in ENGS}
        self.idx = {e: 0 for e in ENGS}
        self.clock = {e: {} for e in ENGS}
        self.esems = {e: [] for e in ENGS}
        dma_pool = dma_pool or {'sp': 40, 'act': 4, 'pool': 24}
        self.dpool = {}
        self.dnext = {q: 0 for q in dma_pool}
        self.dcount = {}
        self.dsem = {}
        for q, n in dma_pool.items():
            self.dpool[q] = []
            for i in range(n):
                s = stack.enter_context(nc.semaphore(f"dq_{q}_{i}"))
                self.dpool[q].append(s)
                self.dcount[(q, i)] = 0
                self.dsem[(q, i)] = s
        self.nwaits = 0

    def _esem(self, eng, epoch):
        while len(self.esems[eng]) <= epoch:
            s = self.stack.enter_context(self.nc.semaphore(f"e_{eng}_{len(self.esems[eng])}"))
            self.esems[eng].append(s)
        return self.esems[eng][epoch]

    def _need(self, eng, tok, kind):
        key, val, snap = tok
        if key == eng:
            if eng == 'pe' or kind != 'raw':
                return
        ck = self.clock[eng]
        if ck.get(key, 0) >= val:
            return
        self.streams[eng].append(('wait', key, val))
        self.nwaits += 1
        new = dict(ck)
        for k, v in snap.items():
            if new.get(k, 0) < v:
                new[k] = v
        if new.get(key, 0) < val:
            new[key] = val
        self.clock[eng] = new

    def _deps(self, eng, reads, writes):
        for res in reads:
            if res.w is not None:
                self._need(eng, res.w, 'raw')
        for res in writes:
            if res.w is not None:
                self._need(eng, res.w, 'waw')
            for t in res.r:
                self._need(eng, t, 'war')

    def _commit(self, tok, reads, writes):
        for res in writes:
            res.w = tok
            res.r = []
        key = tok[0]
        for res in reads:
            if res in writes:
                continue
            if isinstance(key, str):
                res.r = [t for t in res.r if t[0] != key]
            res.r.append(tok)

    def op(self, eng, fn, reads=(), writes=()):
        self._deps(eng, reads, writes)
        self.idx[eng] += 1
        i = self.idx[eng]
        self.streams[eng].append(('op', fn, i))
        tok = (eng, i, self.clock[eng])
        self._commit(tok, reads, writes)
        return tok

    def dma(self, q, fn, reads=(), writes=()):
        self._deps(q, reads, writes)
        slot = self.dnext[q]
        self.dnext[q] = (slot + 1) % len(self.dpool[q])
        key = (q, slot)
        prev = self.dcount[key]
        if prev > 0:
            self._need(q, (key, prev, {}), 'raw')
        val = prev + 16
        assert val < 2 * LIMIT, "dma sem overflow"
        self.dcount[key] = val
        self.streams[q].append(('dma', fn, key))
        tok = (key, val, self.clock[q])
        self._commit(tok, reads, writes)
        return tok

    def barrier(self):
        for key, val in self.dcount.items():
            if val > 0:
                self._need('sp', (key, val, {}), 'raw')
        for e in ENGS:
            if e != 'sp' and self.idx[e] > 0:
                self._need('sp', (e, self.idx[e], self.clock[e]), 'raw')
        tok = self.op('sp', lambda e: e.nop())
        for e in ENGS:
            if e != 'sp':
                self._need(e, tok, 'raw')

    def finish(self):
        self.barrier()

    def emit(self):
        nc = self.nc
        handles = {'pe': 'tensor', 'act': 'scalar', 'dve': 'vector', 'pool': 'gpsimd', 'sp': 'sync'}
        for eng in ENGS:
            for ep in range((self.idx[eng] + LIMIT - 1) // LIMIT + 1):
                self._esem(eng, ep)
        with nc.Block() as block:
            for eng in ENGS:
                stream = self.streams[eng]

                def body(e, eng=eng, stream=stream):
                    for item in stream:
                        if item[0] == 'wait':
                            _, key, val = item
                            if isinstance(key, str):
                                e.wait_ge(self.esems[key][(val - 1) // LIMIT], (val - 1) % LIMIT + 1)
                            else:
                                e.wait_ge(self.dsem[key], val)
                        elif item[0] == 'op':
                            _, fn, i = item
                            fn(e).then_inc(self.esems[eng][(i - 1) // LIMIT], 1)
                        else:
                            _, fn, key = item
                            fn(e).then_inc(self.dsem[key], 16)

                getattr(block, handles[eng])(body)


class Rot:
    def __init__(self, bufs):
        self.bufs = bufs
        self.i = 0

    def next(self):
        b = self.bufs[self.i]
        self.i = (self.i + 1) % len(self.bufs)
        return b


class Builder:
    def __init__(self, S, NSEQ, DEPTH, debug=False, phases=None):
        self.S, self.NSEQ, self.DEPTH, self.debug = S, NSEQ, DEPTH, debug
        self.phases = phases
        self.NT = S // 128
        self.NB = S // 512
        self.nc = bass.Bass("TRN2", target_bir_lowering=False)
        self.uid = 0

    def dram_in(self, name, shape, dt=F32):
        return self.nc.dram_tensor(name, list(shape), dt, kind="ExternalInput").ap()

    def dram_scr(self, name, shape, dt):
        kind = "ExternalOutput" if self.debug else "Internal"
        return self.nc.dram_tensor(name, list(shape), dt, kind=kind).ap()

    def sb(self, st, name, shape, dt):
        self.uid += 1
        return st.enter_context(self.nc.sbuf_tensor(f"{name}_{self.uid}", list(shape), dt))

    def sbuf(self, st, name, shape, dt):
        return Buf(self.sb(st, name, shape, dt), name)

    def sbufs(self, st, name, shape, dt, n):
        return Rot([self.sbuf(st, f"{name}{i}", shape, dt) for i in range(n)])

    def ps(self, st, name, shape, dt):
        self.uid += 1
        return Buf(st.enter_context(self.nc.psum_tensor(f"{name}_{self.uid}", list(shape), dt)), name)

    def pss(self, st, name, shape, dt, n):
        return Rot([self.ps(st, f"{name}{i}", shape, dt) for i in range(n)])

    def build(self):
        nc = self.nc
        S, NSEQ, DEPTH = self.S, self.NSEQ, self.DEPTH
        L = DEPTH
        I = {}
        I['x'] = self.dram_in('x', [NSEQ, S, D])
        I['mem'] = self.dram_in('mem', [NSEQ, MEM, D])
        I['norm_mix'] = self.dram_in('norm_mix', [L, D])
        I['w_in'] = self.dram_in('w_in', [L, D, N_IN])
        I['rel_bias_a'] = self.dram_in('rel_bias_a', [L, 8, 320])
        I['lambda_vecs'] = self.dram_in('lambda_vecs', [L, 256])
        I['subln_b'] = self.dram_in('subln_b', [L, 128])
        I['w_up_a'] = self.dram_in('w_up_a', [L, 512, D])
        I['w_up_b'] = self.dram_in('w_up_b', [L, 512, D])
        I['w_up_c'] = self.dram_in('w_up_c', [L, 512, D])
        I['w_out'] = self.dram_in('w_out', [L, D, D])
        I['norm_cross'] = self.dram_in('norm_cross', [L, D])
        I['w_q_x'] = self.dram_in('w_q_x', [L, D, 256])
        I['w_kv_x'] = self.dram_in('w_kv_x', [L, D, 512])
        I['w_o_x'] = self.dram_in('w_o_x', [L, 256, D])
        I['norm_ffn'] = self.dram_in('norm_ffn', [L, D])
        I['w_gu'] = self.dram_in('w_gu', [L, D, 2 * DFF])
        I['w_down'] = self.dram_in('w_down', [L, DFF, D])
        I['mem_norm'] = self.dram_in('mem_norm', [D])
        I['final_norm'] = self.dram_in('final_norm', [D])
        I['c_ident'] = self.dram_in('c_ident', [128, 128], BF16)
        I['c_rot'] = self.dram_in('c_rot', [128, 128], BF16)
        I['c_flip'] = self.dram_in('c_flip', [128, 128], BF16)
        I['c_cos'] = self.dram_in('c_cos', [128, S], BF16)
        I['c_sin'] = self.dram_in('c_sin', [128, S], BF16)
        I['c_cmask'] = self.dram_in('c_cmask', [128, 4, 512], BF16)
        self.I = I
        self.out = nc.dram_tensor('out', [NSEQ, S, D], F32, kind="ExternalOutput").ap()
        X = {}
        X['xres'] = self.dram_scr('xres', [NSEQ, S, D], F32)
        for nm in ['qaT', 'kaT', 'qbT', 'kbT', 'qcT', 'kcT']:
            X[nm] = self.dram_scr(nm, [NSEQ, 512, S], BF16)
        X['qiT'] = self.dram_scr('qiT', [NSEQ, 256, S], BF16)
        X['kiT'] = self.dram_scr('kiT', [NSEQ, 128, S], BF16)
        X['va1'] = self.dram_scr('va1', [NSEQ, S, 8 * 65], BF16)
        X['vb1'] = self.dram_scr('vb1', [NSEQ, S, 4 * 129], BF16)
        X['vc1'] = self.dram_scr('vc1', [NSEQ, S, 8 * 65], BF16)
        X['wi'] = self.dram_scr('wi', [NSEQ, S, 4], F32)
        X['oT'] = self.dram_scr('oT', [NSEQ, 1536, S], BF16)
        X['ebias'] = self.dram_scr('ebias', [8, 1536], BF16)
        self.X = X

        with ExitStack() as st:
            self.P = P = Prog(nc, st)
            self.ident = self.sbuf(st, 'ident', [128, 128], BF16)
            self.rot = self.sbuf(st, 'rot', [128, 128], BF16)
            self.flip = self.sbuf(st, 'flip', [128, 128], BF16)
            self.gains = self.sbuf(st, 'gains', [128, 3, L, 8, 1], F32)
            self.gmem = self.sbuf(st, 'gmem', [128, 8, 1], F32)
            self.subln = self.sbuf(st, 'subln', [128, L, 1], F32)
            self.neglam = self.sbuf(st, 'neglam', [128, L], F32)
            self.neghalf = self.sbuf(st, 'neghalf', [128, 4], F32)
            self.memT = [self.sbuf(st, f'memT{s}', [128, NCH, MEM], BF16) for s in range(NSEQ)]
            self.setup()
            for l in range(DEPTH):
                if self.want('p1'):
                    self.phase1(l)
                if self.want('p2'):
                    for s in range(NSEQ):
                        self.phase2(l, s)
                if self.want('p3a'):
                    self.phase3a(l)
                if self.want('p3b'):
                    self.phase3b(l)
            P.finish()
            P.emit()
        return nc

    def want(self, ph):
        if self.phases is None:
            return True
        if ph in ('p2a', 'p2b', 'p2c') and 'p2' in self.phases and not any(k in self.phases for k in ('p2a', 'p2b', 'p2c')):
            return True
        if ph == 'p2':
            return any(k in self.phases for k in ('p2', 'p2a', 'p2b', 'p2c'))
        return ph in self.phases

    def lam_init(self, l):
        return 0.8 - 0.6 * math.exp(-0.3 * l)

    def setup(self):
        P, I, L = self.P, self.I, self.DEPTH
        ident, rot, gains = self.ident, self.rot, self.gains
        P.dma('sp', lambda e: e.dma_start(out=ident.t[:], in_=I['c_ident'][:, :]), writes=[ident.r])
        P.dma('sp', lambda e: e.dma_start(out=rot.t[:], in_=I['c_rot'][:, :]), writes=[rot.r])
        P.dma('sp', lambda e: e.dma_start(out=self.flip.t[:], in_=I['c_flip'][:, :]), writes=[self.flip.r])
        for i, nm in enumerate(['norm_mix', 'norm_cross', 'norm_ffn']):
            src = I[nm].rearrange("l (c p o) -> p l c o", p=128, o=1)
            P.dma('sp', lambda e, i=i, src=src: e.dma_start(out=gains.t[:, i], in_=src, allow_slow_non_contiguous=True), writes=[gains.r])
        P.dma('sp', lambda e: e.dma_start(out=self.gmem.t[:], in_=I['mem_norm'].rearrange("(c p o) -> p c o", p=128, o=1), allow_slow_non_contiguous=True),
              writes=[self.gmem.r])
        P.dma('sp', lambda e: e.dma_start(out=self.subln.t[:], in_=I['subln_b'].rearrange("l (p o) -> p l o", o=1), allow_slow_non_contiguous=True),
              writes=[self.subln.r])
        P.op('dve', lambda e: e.memset(self.neghalf.t[:], -0.5), writes=[self.neghalf.r])
        with ExitStack() as st:
            lv = self.sbuf(st, 'lv', [128, L * 256], F32)
            tmp = self.sbuf(st, 'lvtmp', [128, L, 2, 64], F32)
            sm = self.sbuf(st, 'lvs', [128, L, 2], F32)
            ex = self.sbuf(st, 'lve', [128, L, 2], F32)
            src = I['lambda_vecs'].rearrange("l k -> (l k)").partition_broadcast(128)
            P.dma('sp', lambda e: e.dma_start(out=lv.t[:], in_=src), writes=[lv.r])
            lv4 = lv.t[:].rearrange("p (l a b k) -> p l a b k", l=L, a=2, b=2)
            P.op('dve', lambda e: e.tensor_tensor(out=tmp.t[:], in0=lv4[:, :, :, 0, :], in1=lv4[:, :, :, 1, :], op=ALU.mult),
                 reads=[lv.r], writes=[tmp.r])
            P.op('dve', lambda e: e.tensor_reduce(out=sm.t[:], in_=tmp.t[:], axis=AX.X, op=ALU.add),
                 reads=[tmp.r], writes=[sm.r])
            P.op('act', lambda e: e.activation(out=ex.t[:], in_=sm.t[:], func=AF.Exp), reads=[sm.r], writes=[ex.r])
            for l in range(L):
                P.op('dve', lambda e, l=l: e.tensor_scalar(out=self.neglam.t[:, l:l + 1], in0=ex.t[:, l, 1:2],
                                                          scalar1=ex.t[:, l, 0:1], scalar2=-self.lam_init(l),
                                                          op0=ALU.subtract, op1=ALU.add),
                     reads=[ex.r], writes=[self.neglam.r])
            mt = self.sbufs(st, 'memt', [128, D], F32, 2)
            junk = self.sbuf(st, 'memjunk', [128, D], F32)
            mb = self.sbufs(st, 'memb', [128, D], BF16, 2)
            ssq = self.sbufs(st, 'memss', [128, 2], F32, 2)
            tp = self.pss(st, 'memtp', [128, D], BF16, 2)
            for s in range(self.NSEQ):
                for j in range(2):
                    x_t, x_b, ss, pt = mt.next(), mb.next(), ssq.next(), tp.next()
                    P.dma('sp', lambda e, s=s, j=j, x_t=x_t: e.dma_start(out=x_t.t[:], in_=I['mem'][s, j * 128:(j + 1) * 128, :]),
                          writes=[x_t.r])
                    self.rms_to_bf16(x_t, junk, ss, x_b)
                    self.transpose_to(x_b, pt, self.memT[s], j * 128, 128, 'dve')
        P.barrier()

    def rms_to_bf16(self, x_t, junk, ss, x_b):
        P = self.P
        P.op('act', lambda e: e.activation(out=junk.t[:], in_=x_t.t[:], func=AF.Square, accum_out=ss.t[:, 0:1]),
             reads=[x_t.r], writes=[junk.r, ss.r])
        P.op('dve', lambda e: e.tensor_scalar(out=ss.t[:, 1:2], in0=ss.t[:, 0:1], scalar1=1.0 / D, scalar2=EPS,
                                              op0=ALU.mult, op1=ALU.add), reads=[ss.r], writes=[ss.r])
        P.op('pool', lambda e: e.tensor_tensor(out=ss.t[:, 0:1], in0=ss.t[:, 1:2], in1=self.neghalf.t[:, 0:1], op=ALU.pow),
             reads=[ss.r, self.neghalf.r], writes=[ss.r])
        P.op('dve', lambda e: e.tensor_scalar(out=x_b.t[:], in0=x_t.t[:], scalar1=ss.t[:, 0:1], scalar2=None, op0=ALU.mult),
             reads=[x_t.r, ss.r], writes=[x_b.r])

    def transpose_to(self, x_b, pt, dstT, col0, ncols, eng, dst_res=None):
        P = self.P
        for c in range(NCH):
            P.op('pe', lambda e, c=c: e.transpose(out=pt.t[:, c * 128:(c + 1) * 128], in_=x_b.t[:, c * 128:(c + 1) * 128],
                                                  identity=self.ident.t[:]),
                 reads=[x_b.r, self.ident.r], writes=[pt.r])
        src = pt.t[:].rearrange("p (c t) -> p c t", c=NCH)
        dres = dst_res if dst_res is not None else dstT.r
        if eng == 'act':
            P.op('act', lambda e: e.activation(out=dstT.t[:, :, col0:col0 + ncols], in_=src, func=AF.Copy),
                 reads=[pt.r], writes=[dres])
        else:
            P.op('dve', lambda e: e.tensor_copy(out=dstT.t[:, :, col0:col0 + ncols], in_=src), reads=[pt.r], writes=[dres])

    def load_weight(self, st_stage, dst, dst_sl, src_ap, nrows_chunks, ncols, scale_fn, const=1.0, rowchunk0=0):
        P = self.P
        stage = st_stage
        CW = stage.bufs[0].t.shape[1]
        for c in range(nrows_chunks):
            for c0 in range(0, ncols, CW):
                c1 = min(ncols, c0 + CW)
                sg = stage.next()
                P.dma('sp', lambda e, c=c, c0=c0, c1=c1, sg=sg: e.dma_start(
                    out=sg.t[:, 0:c1 - c0], in_=src_ap[(rowchunk0 + c) * 128:(rowchunk0 + c + 1) * 128, c0:c1]), writes=[sg.r])
                sc = scale_fn(c)
                rd = [sg.r] + ([sc[1]] if sc is not None else [])
                if sc is not None:
                    P.op('pool', lambda e, c=c, c0=c0, c1=c1, sg=sg, sc=sc: e.tensor_scalar(
                        out=dst_sl(c, c0, c1), in0=sg.t[:, 0:c1 - c0], scalar1=sc[0], scalar2=const,
                        op0=ALU.mult, op1=ALU.mult), reads=rd, writes=[dst.r])
                else:
                    P.op('pool', lambda e, c=c, c0=c0, c1=c1, sg=sg: e.tensor_scalar(
                        out=dst_sl(c, c0, c1), in0=sg.t[:, 0:c1 - c0], scalar1=const, scalar2=1.0,
                        op0=ALU.mult, op1=ALU.mult), reads=rd, writes=[dst.r])

    def x_src(self, l):
        return self.I['x'] if l == 0 else self.X['xres']

    def phase1(self, l):
        P, I, X, S = self.P, self.I, self.X, self.S
        with ExitStack() as st:
            w1 = self.sbuf(st, 'w1', [128, NCH, 4932], BF16)
            wki = self.sbuf(st, 'wki', [128, NCH, 128], BF16)
            stage = self.sbufs(st, 'stage', [128, 1644], F32, 2)
            cosT = self.sbuf(st, 'cosT', [128, S], BF16)
            sinT = self.sbuf(st, 'sinT', [128, S], BF16)
            P.dma('sp', lambda e: e.dma_start(out=cosT.t[:], in_=I['c_cos'][:, :]), writes=[cosT.r])
            P.dma('sp', lambda e: e.dma_start(out=sinT.t[:], in_=I['c_sin'][:, :]), writes=[sinT.r])
            gm = self.gains
            self.load_weight(stage, w1, lambda c, c0, c1: w1.t[:, c, c0:c1], I['w_in'][l], NCH, 4932,
                             lambda c: (gm.t[:, 0, l, c, :], gm.r))
            for c in range(NCH):
                for h in range(2):
                    P.op('pool', lambda e, c=c, h=h: e.tensor_copy(out=wki.t[:, c, h * 64:(h + 1) * 64], in_=w1.t[:, c, 4864:4928]),
                         reads=[w1.r], writes=[wki.r])
            xt = self.sbufs(st, 'xt', [128, D], F32, 3)
            junk = self.sbuf(st, 'junk', [128, D], F32)
            xb = self.sbufs(st, 'xb', [128, D], BF16, 2)
            ssq = self.sbufs(st, 'ss', [128, 2], F32, 4)
            xnT = [self.sbuf(st, f'xnT{i}', [128, NCH, 512], BF16) for i in range(2)]
            xnT_res = [[Res() for _ in range(4)] for _ in range(2)]
            tp = self.pss(st, 'tp', [128, D], BF16, 2)
            pm = self.pss(st, 'pm', [128, 512], F32, 4)
            pr = self.pss(st, 'pr', [128, 512], F32, 2)
            qsb = self.sbufs(st, 'qsb', [128, 512], BF16, 3)
            t1 = self.sbufs(st, 't1', [128, 512], F32, 2)
            t2 = self.sbufs(st, 't2', [128, 512], F32, 2)
            osb = self.sbufs(st, 'osb', [128, 512], BF16, 4)
            v65 = self.sbufs(st, 'v65', [128, 8, 65], BF16, 4)
            v129 = self.sbufs(st, 'v129', [128, 4, 129], BF16, 2)
            wsb = self.sbufs(st, 'wsb', [128, 4], F32, 2)
            for b_ in v65.bufs + v129.bufs:
                P.op('dve', lambda e, b_=b_: e.memset(b_.t[:], 1.0), writes=[b_.r])
            ftiles = []
            for i in range(4):
                ftiles.append(('qaT', i * 128, (w1, 0 + i * 128), False))
                ftiles.append(('kaT', i * 128, (w1, 512 + i * 128), False))
            for i in range(4):
                ftiles.append(('qbT', i * 128, (w1, 1536 + i * 128), True))
                ftiles.append(('kbT', i * 128, (w1, 2048 + i * 128), True))
                ftiles.append(('qcT', i * 128, (w1, 3072 + i * 128), True))
                ftiles.append(('kcT', i * 128, (w1, 3584 + i * 128), True))
            for i in range(2):
                ftiles.append(('qiT', i * 128, (w1, 4608 + i * 128), True))
            ftiles.append(('kiT', 0, (wki, 0), True))
            blk = 0
            for s in range(self.NSEQ):
                for tb in range(self.NB):
                    T0 = tb * 512
                    xn = xnT[blk % 2]
                    xr = xnT_res[blk % 2]
                    blk += 1
                    for j in range(4):
                        x_t, x_b, ss, pt = xt.next(), xb.next(), ssq.next(), tp.next()
                        src = self.x_src(l)[s, T0 + j * 128:T0 + (j + 1) * 128, :]
                        P.dma('sp', lambda e, x_t=x_t, src=src: e.dma_start(out=x_t.t[:], in_=src), writes=[x_t.r])
                        self.rms_to_bf16(x_t, junk, ss, x_b)
                        self.transpose_to(x_b, pt, xn, j * 128, 128, 'dve', dst_res=xr[j])
                    for (dst, row0, (wt, col0), rope) in ftiles:
                        pmm = pm.next()
                        for c in range(NCH):
                            P.op('pe', lambda e, c=c, wt=wt, col0=col0, pmm=pmm, xn=xn: e.matmul(
                                pmm.t[:], lhsT=wt.t[:, c, col0:col0 + 128], rhs=xn.t[:, c, :], start=(c == 0), stop=(c == NCH - 1)),
                                reads=[wt.r] + xr, writes=[pmm.r])
                        dview = X[dst][s, row0:row0 + 128, T0:T0 + 512]
                        if not rope:
                            o_ = osb.next()
                            P.op('act', lambda e, o_=o_, pmm=pmm: e.activation(out=o_.t[:], in_=pmm.t[:], func=AF.Copy),
                                 reads=[pmm.r], writes=[o_.r])
                        else:
                            q_ = qsb.next()
                            prr = pr.next()
                            a1, a2, o_ = t1.next(), t2.next(), osb.next()
                            P.op('act', lambda e, q_=q_, pmm=pmm: e.activation(out=q_.t[:], in_=pmm.t[:], func=AF.Copy),
                                 reads=[pmm.r], writes=[q_.r])
                            P.op('pe', lambda e, q_=q_, prr=prr: e.matmul(prr.t[:], lhsT=self.rot.t[:], rhs=q_.t[:], start=True, stop=True),
                                 reads=[self.rot.r, q_.r], writes=[prr.r])
                            P.op('pool', lambda e, q_=q_, a1=a1, T0=T0: e.tensor_tensor(out=a1.t[:], in0=q_.t[:], in1=cosT.t[:, T0:T0 + 512], op=ALU.mult),
                                 reads=[q_.r, cosT.r], writes=[a1.r])
                            P.op('dve', lambda e, prr=prr, a2=a2, T0=T0: e.tensor_tensor(out=a2.t[:], in0=prr.t[:], in1=sinT.t[:, T0:T0 + 512], op=ALU.mult),
                                 reads=[prr.r, sinT.r], writes=[a2.r])
                            P.op('dve', lambda e, a1=a1, a2=a2, o_=o_: e.tensor_tensor(out=o_.t[:], in0=a1.t[:], in1=a2.t[:], op=ALU.add),
                                 reads=[a1.r, a2.r], writes=[o_.r])
                        P.dma('pool', lambda e, o_=o_, dview=dview: e.dma_start(out=dview, in_=o_.t[:]), reads=[o_.r])
                    for j in range(4):
                        tok0 = T0 + j * 128
                        for (dst, col0, nh, hd, vpool) in (('va1', 1024, 8, 64, v65), ('vb1', 2560, 4, 128, v129), ('vc1', 4096, 8, 64, v65)):
                            pmm = pm.next()
                            for c in range(NCH):
                                P.op('pe', lambda e, c=c, j=j, col0=col0, pmm=pmm, xn=xn: e.matmul(
                                    pmm.t[:], lhsT=xn.t[:, c, j * 128:(j + 1) * 128], rhs=w1.t[:, c, col0:col0 + 512],
                                    start=(c == 0), stop=(c == NCH - 1)), reads=[w1.r, xr[j]], writes=[pmm.r])
                            v_ = vpool.next()
                            P.op('act', lambda e, v_=v_, pmm=pmm, nh=nh, hd=hd: e.activation(
                                out=v_.t[:, :, 0:hd], in_=pmm.t[:].rearrange("p (h d) -> p h d", h=nh), func=AF.Copy),
                                reads=[pmm.r], writes=[v_.r])
                            dview = X[dst][s, tok0:tok0 + 128, :]
                            P.dma('pool', lambda e, v_=v_, dview=dview: e.dma_start(out=dview, in_=v_.t[:].rearrange("p h d -> p (h d)")), reads=[v_.r])
                        pmm = pm.next()
                        for c in range(NCH):
                            P.op('pe', lambda e, c=c, j=j, pmm=pmm, xn=xn: e.matmul(
                                pmm.t[:, 0:4], lhsT=xn.t[:, c, j * 128:(j + 1) * 128], rhs=w1.t[:, c, 4928:4932],
                                start=(c == 0), stop=(c == NCH - 1)), reads=[w1.r, xr[j]], writes=[pmm.r])
                        w_ = wsb.next()
                        P.op('dve', lambda e, w_=w_, pmm=pmm: e.tensor_scalar(out=w_.t[:], in0=pmm.t[:, 0:4], scalar1=1.0 / 16.0, scalar2=None, op0=ALU.mult),
                             reads=[pmm.r], writes=[w_.r])
                        dview = X['wi'][s, tok0:tok0 + 128, :]
                        P.dma('pool', lambda e, w_=w_, dview=dview: e.dma_start(out=dview, in_=w_.t[:]), reads=[w_.r])
            P.barrier()

    def phase2(self, l, s):
        if self.want('p2a'):
            self.mixerA(l, s)
        if self.want('p2b'):
            self.mixerB(l, s)
        if self.want('p2c'):
            self.mixerC(l, s)

    def _p2_common(self, st):
        P = self.P
        c = {}
        c['sc'] = self.pss(st, 'sc', [128, 512], F32, 3)
        c['acc'] = [self.ps(st, f'acc{i}', [128, 512], F32) for i in range(4)]
        c['tp'] = self.ps(st, 'tpo', [128, 1024], BF16)
        c['E'] = self.sbufs(st, 'E', [128, 512], BF16, 4)
        c['ob'] = self.sbufs(st, 'oblk', [128, 4, 512], BF16, 2)
        c['oT'] = self.sbufs(st, 'oTsb', [128, 4, 512], BF16, 1)
        c['negone'] = self.sbuf(st, 'negone', [128, 32], F32)
        P.op('pool', lambda e: e.memset(c['negone'].t[:], -1.0), writes=[c['negone'].r])
        c['zt'] = self.sbuf(st, 'zt', [1, 512], BF16)
        P.op('pool', lambda e: e.memset(c['zt'].t[:], 0.0), writes=[c['zt'].r])
        c['hc'] = 0
        return c

    def _open_acc(self, c, acc, W=512):
        zt = c['zt']
        self.P.op('pe', lambda e: e.matmul(acc.t[:, 0:W], lhsT=zt.t[0:1, 0:128], rhs=zt.t[0:1, 0:W], start=True, stop=False),
                  reads=[zt.r], writes=[acc.r])

    def run_units(self, c, units, LA=2, late_delay=5):
        P = self.P
        n = len(units)
        pend = []
        late = []
        for i in range(n + LA):
            if i < n:
                u = units[i]
                if u.get('pre'):
                    u['pre']()
                sc, E = c['sc'].next(), c['E'].next()
                u['s1'](sc)
                P.op('act', lambda e, sc=sc, E=E: e.activation(out=E.t[:], in_=sc.t[:], func=AF.Exp, scale=0.125),
                     reads=[sc.r], writes=[E.r])
                pend.append(E)
            if i >= LA:
                k = i - LA
                u = units[k]
                u['s3'](pend[k])
                if u.get('post'):
                    u['post']()
                if u.get('late'):
                    late.append((k + late_delay, u['late']))
                while late and late[0][0] <= k:
                    late.pop(0)[1]()
        for _, fn in late:
            fn()

    def _load_kv(self, st, s, kname, vname, vw):
        P, X, S, NT = self.P, self.X, self.S, self.NT
        kT = self.sbuf(st, 'kT', [128, 4, S], BF16)
        v1 = self.sbuf(st, 'v1', [128, NT, vw], BF16)
        P.dma('sp', lambda e: e.dma_start(out=kT.t[:], in_=X[kname][s].rearrange("(c p) t -> p c t", p=128)), writes=[kT.r])
        half = NT // 2
        for hh in range(2):
            P.dma('sp', lambda e, hh=hh: e.dma_start(
                out=v1.t[:, hh * half:(hh + 1) * half, :],
                in_=X[vname][s, hh * half * 128:(hh + 1) * half * 128, :].rearrange("(t p) f -> p t f", p=128)), writes=[v1.r])
        return kT, v1

    def _store_o(self, c, ob, s, row0, Q0):
        P, X = self.P, self.X
        if 'store' in KSKIP:
            return
        oT = c['oT'].next()
        tp = c['tp']
        for half in range(2):
            for cc in range(2):
                ch = half * 2 + cc
                for j in range(4):
                    P.op('pe', lambda e, ch=ch, j=j, cc=cc: e.transpose(
                        out=tp.t[:, cc * 512 + j * 128: cc * 512 + (j + 1) * 128],
                        in_=ob.t[:, j, ch * 128:(ch + 1) * 128], identity=self.ident.t[:]),
                        reads=[ob.r, self.ident.r], writes=[tp.r])
            P.op('act', lambda e, half=half: e.activation(
                out=oT.t[:, half * 2:half * 2 + 2, :], in_=tp.t[:].rearrange("p (c q) -> p c q", c=2), func=AF.Copy),
                reads=[tp.r], writes=[oT.r])
        dview = X['oT'][s, row0:row0 + 512, Q0:Q0 + 512].rearrange("(c p) q -> p c q", p=128)
        P.dma('pool', lambda e: e.dma_start(out=dview, in_=oT.t[:]), reads=[oT.r])

    def _evac65(self, c, acc, stg, h):
        self.P.op('act', lambda e: e.activation(out=stg.t[:, :, h, :], in_=acc.t[:, 0:260].rearrange("p (j d) -> p j d", j=4), func=AF.Copy),
                  reads=[acc.r], writes=[stg.r])

    def _norm65(self, c, stg, rec, ob):
        P = self.P
        if 'norm' in KSKIP:
            return
        P.op('pool', lambda e: e.tensor_tensor(out=rec.t[:], in0=stg.t[:, :, :, 64],
                                               in1=c['negone'].t[:, 0:32].rearrange("p (j h) -> p j h", j=4), op=ALU.pow),
             reads=[stg.r, c['negone'].r], writes=[rec.r])
        P.op('pool', lambda e: e.tensor_tensor(out=ob.t[:].rearrange("p j (h d) -> p j h d", h=8), in0=stg.t[:, :, :, 0:64],
                                               in1=rec.t[:].unsqueeze(3).to_broadcast([128, 4, 8, 64]), op=ALU.mult),
             reads=[stg.r, rec.r], writes=[ob.r])

    def mixerA(self, l, s):
        P, I, X, S, NB = self.P, self.I, self.X, self.S, self.NB
        with ExitStack() as st:
            c = self._p2_common(st)
            kT, v1 = self._load_kv(st, s, 'kaT', 'va1', 520)
            bm = self.sbuf(st, 'bm', [128, 8, 8, 512], BF16)
            qb = self.sbufs(st, 'qblk', [128, 4, 512], BF16, 2)
            stgp = self.sbufs(st, 'stg', [128, 4, 8, 65], F32, 2)
            recp = self.sbufs(st, 'rec', [128, 4, 8], F32, 2)
            e_f = self.sbuf(st, 'e_f', [8, 1536], F32)
            e_b = self.sbuf(st, 'e_b', [8, 1536], BF16)
            r_eb = Res()
            P.dma('sp', lambda e: e.dma_start(out=e_f.t[:, 449:769], in_=I['rel_bias_a'][l]), writes=[e_f.r])
            P.op('dve', lambda e: e.tensor_copy(out=e_f.t[:, 0:449], in_=e_f.t[:, 449:450].to_broadcast([8, 449])),
                 reads=[e_f.r], writes=[e_f.r])
            P.op('dve', lambda e: e.tensor_copy(out=e_f.t[:, 769:1536], in_=e_f.t[:, 768:769].to_broadcast([8, 767])),
                 reads=[e_f.r], writes=[e_f.r])
            P.op('dve', lambda e: e.tensor_scalar(out=e_b.t[:], in0=e_f.t[:], scalar1=8.0, scalar2=None, op0=ALU.mult),
                 reads=[e_f.r], writes=[e_b.r])
            P.dma('sp', lambda e: e.dma_start(out=X['ebias'][:, :], in_=e_b.t[:]), reads=[e_b.r], writes=[r_eb])
            for h in range(8):
                src = bass.AP(tensor=X['ebias'].tensor, offset=h * 1536 + 1, ap=[[1, 128], [128, 8], [1, 512]])
                P.dma('sp', lambda e, h=h, src=src: e.dma_start(out=bm.t[:, h], in_=src), reads=[r_eb], writes=[bm.r])
            for t in range(8):
                for ph in range(2):
                    lo_fb = max(0, 2 * t + ph - 8)
                    hi_fb = min(7, 2 * t + ph)
                    if lo_fb > 0:
                        P.op('pool', lambda e, t=t, ph=ph, lo_fb=lo_fb: e.memset(bm.t[(1 - ph) * 64:(2 - ph) * 64, :, 7 - t, 0:64 * lo_fb], NEG),
                             writes=[bm.r])
                    if hi_fb < 7:
                        P.op('pool', lambda e, t=t, ph=ph, hi_fb=hi_fb: e.memset(bm.t[(1 - ph) * 64:(2 - ph) * 64, :, 7 - t, 64 * (hi_fb + 1):512], NEG),
                             writes=[bm.r])
            units = []
            for b in range(NB):
                Q0 = 512 * b
                q_ = qb.next()
                ob, stg, rec = c['ob'].next(), stgp.next(), recp.next()
                tmin = 4 if b == 0 else 0
                for h in range(8):
                    pr = slice((h % 2) * 64, (h % 2) * 64 + 64)
                    acc = c['acc'][c['hc'] % 4]
                    c['hc'] += 1
                    for t in range(tmin, 8):
                        K0 = Q0 - 512 + 128 * t
                        u = {}
                        if h == 0 and t == tmin:
                            u['pre'] = lambda q_=q_, Q0=Q0: P.dma('sp', lambda e: e.dma_start(
                                out=q_.t[:], in_=X['qaT'][s, :, Q0:Q0 + 512].rearrange("(c p) t -> p c t", p=128)), writes=[q_.r])

                        def s1(sc, pr=pr, h=h, K0=K0, q_=q_, t=t):
                            P.op('pe', lambda e: e.matmul(sc.t[:], lhsT=kT.t[pr, h // 2, K0:K0 + 128], rhs=q_.t[pr, h // 2, :], start=True, stop=False),
                                 reads=[kT.r, q_.r], writes=[sc.r])
                            P.op('pe', lambda e: e.matmul(sc.t[:], lhsT=self.flip.t[:], rhs=bm.t[:, h, 7 - t, :], start=False, stop=True),
                                 reads=[bm.r, self.flip.r], writes=[sc.r])

                        def s3(E, h=h, K0=K0, t=t, tmin=tmin, acc=acc):
                            if t == tmin:
                                self._open_acc(c, acc, 260)
                            for j in range(4):
                                if j <= t <= j + 4:
                                    P.op('pe', lambda e, j=j: e.matmul(
                                        acc.t[:, j * 65:(j + 1) * 65], lhsT=E.t[:, j * 128:(j + 1) * 128], rhs=v1.t[:, K0 // 128, h * 65:(h + 1) * 65],
                                        start=False, stop=(t == j + 4)), reads=[E.r, v1.r], writes=[acc.r])
                        u['s1'], u['s3'] = s1, s3
                        if t == 7:
                            def post(h=h, acc=acc, stg=stg, rec=rec, ob=ob):
                                self._evac65(c, acc, stg, h)
                                if h == 7:
                                    self._norm65(c, stg, rec, ob)
                            u['post'] = post
                            if h == 7:
                                u['late'] = lambda ob=ob, Q0=Q0: self._store_o(c, ob, s, 0, Q0)
                        units.append(u)
            self.run_units(c, units)
            P.barrier()

    def mixerB(self, l, s):
        P, I, X, S, NB = self.P, self.I, self.X, self.S, self.NB
        with ExitStack() as st:
            c = self._p2_common(st)
            kT, v1 = self._load_kv(st, s, 'kbT', 'vb1', 516)
            cm = self.sbuf(st, 'cm', [128, 4, 512], BF16)
            P.dma('sp', lambda e: e.dma_start(out=cm.t[:], in_=I['c_cmask'][:, :, :]), writes=[cm.r])
            qb = self.sbufs(st, 'qblk', [128, 4, 512], BF16, 2)
            stgp = self.sbufs(st, 'stgB', [128, 4, 2, 129], F32, 2)
            recp = self.sbufs(st, 'recB', [128, 4, 2], F32, 2)
            o2p = self.sbufs(st, 'o2', [128, 4, 2, 128], F32, 1)
            dfp = self.sbufs(st, 'df', [128, 4, 128], F32, 2)
            tnp = self.sbufs(st, 'tneg', [128, 4, 128], F32, 1)
            sqj = self.sbuf(st, 'sqj', [128, 128], BF16)
            st4 = self.sbufs(st, 'st4', [128, 3, 4], F32, 2)
            units = []
            for b in range(NB):
                Q0 = 512 * b
                q_ = qb.next()
                ob = c['ob'].next()
                nkt = 4 * b + 4
                for h in range(4):
                    stg, rec = stgp.next(), recp.next()
                    for mp in range(2):
                        pr = slice(mp * 64, mp * 64 + 64)
                        accs = (c['acc'][2 * (c['hc'] % 2)], c['acc'][2 * (c['hc'] % 2) + 1])
                        c['hc'] += 1
                        for t in range(nkt):
                            K0 = 128 * t
                            rel = t - 4 * b
                            u = {}
                            if h == 0 and mp == 0 and t == 0:
                                u['pre'] = lambda q_=q_, Q0=Q0: P.dma('sp', lambda e: e.dma_start(
                                    out=q_.t[:], in_=X['qbT'][s, :, Q0:Q0 + 512].rearrange("(c p) t -> p c t", p=128)), writes=[q_.r])

                            def s1(sc, pr=pr, h=h, K0=K0, q_=q_, rel=rel):
                                P.op('pe', lambda e: e.matmul(sc.t[:], lhsT=kT.t[pr, h, K0:K0 + 128], rhs=q_.t[pr, h, :], start=True, stop=(rel < 0)),
                                     reads=[kT.r, q_.r], writes=[sc.r])
                                if rel >= 0:
                                    P.op('pe', lambda e: e.matmul(sc.t[:], lhsT=self.ident.t[:], rhs=cm.t[:, rel, :], start=False, stop=True),
                                         reads=[cm.r, self.ident.r], writes=[sc.r])

                            def s3(E, h=h, t=t, b=b, rel=rel, accs=accs):
                                if t == 0:
                                    self._open_acc(c, accs[0], 258)
                                    self._open_acc(c, accs[1], 258)
                                for j in range(4):
                                    if rel <= j:
                                        acc = accs[j // 2]
                                        P.op('pe', lambda e, acc=acc, j=j: e.matmul(
                                            acc.t[:, (j % 2) * 129:(j % 2) * 129 + 129], lhsT=E.t[:, j * 128:(j + 1) * 128], rhs=v1.t[:, t, h * 129:(h + 1) * 129],
                                            start=False, stop=(t == 4 * b + j)), reads=[E.r, v1.r], writes=[acc.r])
                            u['s1'], u['s3'] = s1, s3
                            if t == nkt - 1:
                                def post(h=h, mp=mp, accs=accs, stg=stg, rec=rec, ob=ob):
                                    for jb in range(2):
                                        P.op('act', lambda e, jb=jb: e.activation(
                                            out=stg.t[:, 2 * jb:2 * jb + 2, mp, :], in_=accs[jb].t[:, 0:258].rearrange("p (j d) -> p j d", j=2), func=AF.Copy),
                                            reads=[accs[jb].r], writes=[stg.r])
                                    if mp == 1:
                                        o2, d_, tn, s4 = o2p.next(), dfp.next(), tnp.next(), st4.next()
                                        P.op('pool', lambda e: e.tensor_tensor(out=rec.t[:], in0=stg.t[:, :, :, 128],
                                                                               in1=c['negone'].t[:, 0:8].rearrange("p (j m) -> p j m", j=4), op=ALU.pow),
                                             reads=[stg.r, c['negone'].r], writes=[rec.r])
                                        P.op('pool', lambda e: e.tensor_tensor(out=o2.t[:], in0=stg.t[:, :, :, 0:128],
                                                                               in1=rec.t[:].unsqueeze(3).to_broadcast([128, 4, 2, 128]), op=ALU.mult),
                                             reads=[stg.r, rec.r], writes=[o2.r])
                                        P.op('pool', lambda e: e.tensor_scalar(out=tn.t[:], in0=o2.t[:, :, 1, :], scalar1=self.neglam.t[:, l:l + 1], scalar2=1.0,
                                                                               op0=ALU.mult, op1=ALU.mult), reads=[o2.r, self.neglam.r], writes=[tn.r])
                                        P.op('pool', lambda e: e.tensor_tensor(out=d_.t[:], in0=tn.t[:], in1=o2.t[:, :, 0, :], op=ALU.add),
                                             reads=[tn.r, o2.r], writes=[d_.r])
                                        for j in range(4):
                                            P.op('act', lambda e, j=j: e.activation(out=sqj.t[:], in_=d_.t[:, j, :], func=AF.Square, accum_out=s4.t[:, 0, j:j + 1]),
                                                 reads=[d_.r], writes=[sqj.r, s4.r])
                                        P.op('pool', lambda e: e.tensor_scalar(out=s4.t[:, 1, :], in0=s4.t[:, 0, :], scalar1=1.0 / 128.0, scalar2=EPS,
                                                                               op0=ALU.mult, op1=ALU.add), reads=[s4.r], writes=[s4.r])
                                        P.op('pool', lambda e: e.tensor_tensor(out=s4.t[:, 2, :], in0=s4.t[:, 1, :], in1=self.neghalf.t[:, 0:4], op=ALU.pow),
                                             reads=[s4.r, self.neghalf.r], writes=[s4.r])
                                        P.op('pool', lambda e: e.tensor_tensor(
                                            out=ob.t[:, :, h * 128:(h + 1) * 128], in0=d_.t[:], in1=s4.t[:, 2, :].unsqueeze(2).to_broadcast([128, 4, 128]), op=ALU.mult),
                                            reads=[d_.r, s4.r], writes=[ob.r])
                                u['post'] = post
                                if h == 3 and mp == 1:
                                    u['late'] = lambda ob=ob, Q0=Q0: self._store_o(c, ob, s, 512, Q0)
                            units.append(u)
            self.run_units(c, units)
            P.barrier()

    def mixerC(self, l, s):
        P, I, X, S, NB, NT = self.P, self.I, self.X, self.S, self.NB, self.NT
        FP8 = mybir.dt.float8e4
        with ExitStack() as st:
            c = self._p2_common(st)
            kT, v1 = self._load_kv(st, s, 'kcT', 'vc1', 520)
            kiT = self.sbuf(st, 'kiT', [128, S], BF16)
            P.dma('sp', lambda e: e.dma_start(out=kiT.t[:], in_=X['kiT'][s]), writes=[kiT.r])
            id128 = self.sbuf(st, 'id128', [128, 128], BF16)
            c240 = self.sbuf(st, 'c240', [128, 3, 128], BF16)
            P.op('pool', lambda e: e.memset(c240.t[:], -240.0), writes=[c240.r])
            P.op('pool', lambda e: e.tensor_scalar(out=id128.t[:], in0=self.ident.t[:], scalar1=128.0, scalar2=1.0, op0=ALU.mult, op1=ALU.mult),
                 reads=[self.ident.r], writes=[id128.r])
            qb = self.sbufs(st, 'qblk', [128, 4, 512], BF16, 2)
            qib = self.sbufs(st, 'qiblk', [128, 2, 512], BF16, 2)
            wib = self.sbufs(st, 'wiblk', [128, 4, 4], F32, 2)
            I_sbs = [self.sbuf(st, f'I_sb{i}', [128, S], F32) for i in range(2)]
            Mqs = [self.sbuf(st, f'Mq{i}', [128, S], BF16) for i in range(2)]
            negms = [self.sbuf(st, f'negm{i}', [128, NT, 512], FP8) for i in range(2)]
            rl = self.sbufs(st, 'rl', [128, 512], BF16, 4)
            dg = self.sbufs(st, 'dg', [128, 128], BF16, 8)
            stgp = self.sbufs(st, 'stg', [128, 4, 8, 65], F32, 1)
            recp = self.sbufs(st, 'rec', [128, 4, 8], F32, 1)
            pw = self.sbuf(st, 'pw', [128, NIT + 1], F32)
            thr0 = self.sbuf(st, 'thr0', [128, 1], F32)
            for i in range(NIT + 1):
                P.op('pool', lambda e, i=i: e.memset(pw.t[:, i:i + 1], 2.0 ** -(i + 1)), writes=[pw.r])
            P.op('pool', lambda e: e.memset(thr0.t[:], -1e29), writes=[thr0.r])
            sm = self.sbufs(st, 'bsm', [128, 4], F32, 3)
            halfs = self.sbufs(st, 'halfs', [128, NIT + 1], F32, 2)
            mid = self.sbufs(st, 'mid', [128, 1], F32, 4)
            cnt = self.sbufs(st, 'cnt', [128, 1], F32, 4)
            tsel = self.sbufs(st, 'tsel', [128, 1], F32, 4)
            blkbuf = {}

            def blk_bufs(b):
                if b not in blkbuf:
                    q_, qi_, wi_ = qb.next(), qib.next(), wib.next()
                    Q0 = 512 * b
                    P.dma('sp', lambda e: e.dma_start(
                        out=q_.t[:], in_=X['qcT'][s, :, Q0:Q0 + 512].rearrange("(c p) t -> p c t", p=128)), writes=[q_.r])
                    P.dma('sp', lambda e: e.dma_start(
                        out=qi_.t[:], in_=X['qiT'][s, :, Q0:Q0 + 512].rearrange("(c p) t -> p c t", p=128)), writes=[qi_.r])
                    P.dma('sp', lambda e: e.dma_start(
                        out=wi_.t[:], in_=X['wi'][s, Q0:Q0 + 512, :].rearrange("(j p) h -> p j h", p=128)), writes=[wi_.r])
                    blkbuf[b] = (q_, qi_, wi_)
                return blkbuf[b]

            def IDX(m):
                b, jj = m // 4, m % 4
                q_, qi_, wi_ = blk_bufs(b)
                n_k = 128 * (m + 1)
                I_sb = I_sbs[m % 2]
                dgs = []
                for h in range(4):
                    d_ = dg.next()
                    P.op('pool', lambda e, d_=d_, h=h: e.tensor_scalar(
                        out=d_.t[:], in0=self.ident.t[:], scalar1=wi_.t[:, jj, h:h + 1], scalar2=1.0, op0=ALU.mult, op1=ALU.mult),
                        reads=[self.ident.r, wi_.r], writes=[d_.r])
                    dgs.append(d_)
                for k0 in range(0, n_k, 512):
                    w = min(512, n_k - k0)
                    rls = []
                    for h in range(4):
                        pr = slice((h % 2) * 64, (h % 2) * 64 + 64)
                        sc = c['sc'].next()
                        P.op('pe', lambda e, sc=sc, pr=pr, h=h, k0=k0, w=w: e.matmul(
                            sc.t[:, 0:w], lhsT=qi_.t[pr, h // 2, jj * 128:(jj + 1) * 128], rhs=kiT.t[pr, k0:k0 + w], start=True, stop=True),
                            reads=[qi_.r, kiT.r], writes=[sc.r])
                        r_ = rl.next()
                        P.op('act', lambda e, sc=sc, r_=r_, w=w: e.activation(out=r_.t[:, 0:w], in_=sc.t[:, 0:w], func=AF.Relu),
                             reads=[sc.r], writes=[r_.r])
                        rls.append(r_)
                    accI = c['sc'].next()
                    for h in range(4):
                        P.op('pe', lambda e, accI=accI, h=h, w=w, rls=rls: e.matmul(
                            accI.t[:, 0:w], lhsT=dgs[h].t[:], rhs=rls[h].t[:, 0:w], start=(h == 0), stop=(h == 3)),
                            reads=[dgs[h].r, rls[h].r], writes=[accI.r])
                    P.op('act', lambda e, accI=accI, k0=k0, w=w: e.activation(out=I_sb.t[:, k0:k0 + w], in_=accI.t[:, 0:w], func=AF.Copy),
                         reads=[accI.r], writes=[I_sb.r])

            def BIS(m):
                n_k = 128 * (m + 1)
                I_sb, Mq = I_sbs[m % 2], Mqs[m % 2]
                if m >= 2:
                    sm_ = sm.next()
                    hf = halfs.next()
                    P.op('dve', lambda e: e.tensor_reduce(out=sm_.t[:, 0:1], in_=I_sb.t[:, 0:n_k], axis=AX.X, op=ALU.max),
                         reads=[I_sb.r], writes=[sm_.r])
                    P.op('dve', lambda e: e.tensor_reduce(out=sm_.t[:, 1:2], in_=I_sb.t[:, 0:n_k], axis=AX.X, op=ALU.min),
                         reads=[I_sb.r], writes=[sm_.r])
                P.op('dve', lambda e: e.memset(I_sb.t[0:64, n_k - 64:n_k], -1e30), writes=[I_sb.r])
                if m >= 2:
                    P.op('dve', lambda e: e.tensor_tensor(out=sm_.t[:, 2:3], in0=sm_.t[:, 0:1], in1=sm_.t[:, 1:2], op=ALU.subtract),
                         reads=[sm_.r], writes=[sm_.r])
                    P.op('dve', lambda e: e.tensor_scalar(out=hf.t[:], in0=pw.t[:], scalar1=sm_.t[:, 2:3], scalar2=None, op0=ALU.mult),
                         reads=[sm_.r, pw.r], writes=[hf.r])
                    md = mid.next()
                    P.op('dve', lambda e, md=md: e.tensor_tensor(out=md.t[:], in0=sm_.t[:, 1:2], in1=hf.t[:, 0:1], op=ALU.add),
                         reads=[sm_.r, hf.r], writes=[md.r])
                    for it in range(NIT):
                        cn, ts, md2 = cnt.next(), tsel.next(), mid.next()
                        P.op('dve', lambda e, cn=cn, md=md: e.tensor_scalar(
                            out=Mq.t[:, 0:n_k], in0=I_sb.t[:, 0:n_k], scalar1=md.t[:, 0:1], scalar2=None,
                            op0=ALU.is_ge, op1=ALU.add, accum_out=cn.t[:, 0:1]),
                            reads=[I_sb.r, md.r], writes=[Mq.r, cn.r])
                        P.op('dve', lambda e, cn=cn, ts=ts, it=it: e.tensor_scalar(
                            out=ts.t[:], in0=cn.t[:], scalar1=float(TOPK), scalar2=hf.t[:, it:it + 1], op0=ALU.is_ge, op1=ALU.mult),
                            reads=[cn.r, hf.r], writes=[ts.r])
                        P.op('dve', lambda e, md=md, md2=md2, ts=ts, it=it: e.scalar_tensor_tensor(
                            out=md2.t[:], in0=md.t[:], scalar=hf.t[:, it + 1:it + 2], in1=ts.t[:], op0=ALU.subtract, op1=ALU.add),
                            reads=[md.r, ts.r, hf.r], writes=[md2.r])
                        md = md2
                    P.op('dve', lambda e, md=md: e.tensor_tensor(out=sm_.t[:, 3:4], in0=md.t[:], in1=hf.t[:, NIT:NIT + 1], op=ALU.subtract),
                         reads=[md.r, hf.r], writes=[sm_.r])
                    thr_ap, thr_res = sm_.t[:, 3:4], sm_.r
                else:
                    thr_ap, thr_res = thr0.t[:, 0:1], thr0.r
                P.op('dve', lambda e: e.tensor_scalar(
                    out=Mq.t[:, 0:n_k], in0=I_sb.t[:, 0:n_k], scalar1=thr_ap, scalar2=None, op0=ALU.is_ge),
                    reads=[I_sb.r, thr_res], writes=[Mq.r])

            def TR(m):
                b, jj = m // 4, m % 4
                nkt = 4 * b + 4
                Mq, negm = Mqs[m % 2], negms[b % 2]
                tp = c['tp']
                for t0 in range(0, m + 1, 8):
                    nt_ = min(8, m + 1 - t0)
                    for tt in range(nt_):
                        P.op('pe', lambda e, tt=tt, t0=t0: e.transpose(
                            out=tp.t[:, tt * 128:(tt + 1) * 128], in_=Mq.t[:, (t0 + tt) * 128:(t0 + tt + 1) * 128], identity=self.ident.t[:]),
                            reads=[Mq.r, self.ident.r], writes=[tp.r])
                    P.op('act', lambda e, t0=t0, nt_=nt_: e.activation(
                        out=negm.t[:, t0:t0 + nt_, jj * 128:(jj + 1) * 128],
                        in_=tp.t[:, 0:nt_ * 128].rearrange("p (t q) -> p t q", t=nt_),
                        func=AF.Identity, scale=240.0, bias=-240.0, saturate=False), reads=[tp.r], writes=[negm.r])
                if m + 1 < nkt:
                    nf = nkt - m - 1
                    P.op('pool', lambda e: e.tensor_copy(out=negm.t[:, m + 1:nkt, jj * 128:(jj + 1) * 128], in_=c240.t[:, 0:nf, :], saturate=False),
                         reads=[c240.r], writes=[negm.r])

            def MAIN(b):
                Q0 = 512 * b
                nkt = 4 * b + 4
                q_, qi_, wi_ = blk_bufs(b)
                negm = negms[b % 2]
                ob, stg, rec = c['ob'].next(), stgp.next(), recp.next()
                units = []
                for h in range(8):
                    pr = slice((h % 2) * 64, (h % 2) * 64 + 64)
                    acc = c['acc'][c['hc'] % 4]
                    c['hc'] += 1
                    for t in range(nkt):
                        K0 = 128 * t
                        u = {}

                        def s1(sc, pr=pr, h=h, K0=K0, t=t):
                            P.op('pe', lambda e: e.matmul(sc.t[:], lhsT=kT.t[pr, h // 2, K0:K0 + 128], rhs=q_.t[pr, h // 2, :], start=True, stop=False),
                                 reads=[kT.r, q_.r], writes=[sc.r])
                            P.op('pe', lambda e: e.matmul(sc.t[:], lhsT=id128.t[:], rhs=negm.t[:, t, :], start=False, stop=True),
                                 reads=[negm.r, id128.r], writes=[sc.r])

                        def s3(E, h=h, t=t, acc=acc):
                            if t == 0:
                                self._open_acc(c, acc, 260)
                            for j in range(4):
                                if t <= 4 * b + j:
                                    P.op('pe', lambda e, j=j: e.matmul(
                                        acc.t[:, j * 65:(j + 1) * 65], lhsT=E.t[:, j * 128:(j + 1) * 128], rhs=v1.t[:, t, h * 65:(h + 1) * 65],
                                        start=False, stop=(t == 4 * b + j)), reads=[E.r, v1.r], writes=[acc.r])
                        u['s1'], u['s3'] = s1, s3
                        if t == nkt - 1:
                            def post(h=h, acc=acc):
                                self._evac65(c, acc, stg, h)
                                if h == 7:
                                    self._norm65(c, stg, rec, ob)
                            u['post'] = post
                        units.append(u)
                self.run_units(c, units)
                return lambda: self._store_o(c, ob, s, 1024, Q0)

            IDX(0)
            if NT > 1:
                IDX(1)
            pending_store = None
            for m in range(NT):
                BIS(m)
                TR(m)
                if m + 2 < NT:
                    IDX(m + 2)
                if m % 4 == 3:
                    st_fn = MAIN(m // 4)
                    if pending_store is not None:
                        pending_store()
                    pending_store = st_fn
            if pending_store is not None:
                pending_store()
            P.barrier()

    def load_x_block(self, l, s, T0, ntile, xts, xbp, ssp, junk, tpp, dstT, dst_res, load=True):
        P = self.P
        for j in range(ntile):
            x_t = xts[j]
            if load:
                src = self.x_src(l)[s, T0 + j * 128:T0 + (j + 1) * 128, :]
                P.dma('sp', lambda e, x_t=x_t, src=src: e.dma_start(out=x_t.t[:], in_=src), writes=[x_t.r])
            x_b, ss, pt = xbp.next(), ssp.next(), tpp.next()
            self.rms_to_bf16(x_t, junk, ss, x_b)
            self.transpose_to(x_b, pt, dstT, j * 128, 128, 'dve', dst_res=dst_res[j])

    def phase3a(self, l):
        P, I, X, S = self.P, self.I, self.X, self.S
        with ExitStack() as st:
            wg = self.sbuf(st, 'wg', [128, NCH, 3072], BF16)
            wu = self.sbuf(st, 'wu', [128, 12, D], BF16)
            wo = self.sbuf(st, 'wo', [128, NCH, D], BF16)
            wq = self.sbuf(st, 'wq', [128, NCH, 256], BF16)
            wox = self.sbuf(st, 'wox', [128, 2, D], BF16)
            wkv = self.sbuf(st, 'wkv', [128, NCH, 512], BF16)
            gn = self.gains
            with ExitStack() as st2:
                stage = self.sbufs(st2, 'stage', [128, 1024], F32, 2)
                self.load_weight(stage, wg, lambda c, c0, c1: wg.t[:, c, c0:c1], I['w_in'][l][:, 4932:8004], NCH, 3072,
                                 lambda c: (gn.t[:, 0, l, c, :], gn.r))
                self.load_weight(stage, wu, lambda c, c0, c1: wu.t[:, c, c0:c1], I['w_up_a'][l], 4, D, lambda c: None)
                self.load_weight(stage, wu, lambda c, c0, c1: wu.t[:, 4 + c, c0:c1], I['w_up_b'][l], 4, D,
                                 lambda c: (self.subln.t[:, l, :], self.subln.r), const=1.0 - self.lam_init(l))
                self.load_weight(stage, wu, lambda c, c0, c1: wu.t[:, 8 + c, c0:c1], I['w_up_c'][l], 4, D, lambda c: None)
                self.load_weight(stage, wo, lambda c, c0, c1: wo.t[:, c, c0:c1], I['w_out'][l], NCH, D, lambda c: None, const=0.5)
                self.load_weight(stage, wq, lambda c, c0, c1: wq.t[:, c, c0:c1], I['w_q_x'][l], NCH, 256,
                                 lambda c: (gn.t[:, 1, l, c, :], gn.r))
                self.load_weight(stage, wox, lambda c, c0, c1: wox.t[:, c, c0:c1], I['w_o_x'][l], 2, D, lambda c: None)
                self.load_weight(stage, wkv, lambda c, c0, c1: wkv.t[:, c, c0:c1], I['w_kv_x'][l], NCH, 512,
                                 lambda c: (self.gmem.t[:, c, :], self.gmem.r))
                P.barrier()
            tp = self.pss(st, 'tp', [128, D], BF16, 1)
            pm = self.pss(st, 'pm', [128, 512], F32, 3)
            acc = [self.ps(st, f'acc{i}', [128, 512], F32) for i in range(4)]
            kmT = [self.sbuf(st, f'kmT{s}', [128, 2, MEM], BF16) for s in range(self.NSEQ)]
            vm1 = [self.sbuf(st, f'vm1{s}', [128, 2, 4, 65], BF16) for s in range(self.NSEQ)]
            for s in range(self.NSEQ):
                P.op('dve', lambda e, s=s: e.memset(vm1[s].t[:], 1.0), writes=[vm1[s].r])
                for c2 in range(2):
                    pk = pm.next()
                    for c in range(NCH):
                        P.op('pe', lambda e, c=c, c2=c2, pk=pk, s=s: e.matmul(
                            pk.t[:, 0:MEM], lhsT=wkv.t[:, c, c2 * 128:(c2 + 1) * 128], rhs=self.memT[s].t[:, c, :],
                            start=(c == 0), stop=(c == NCH - 1)), reads=[wkv.r, self.memT[s].r], writes=[pk.r])
                    P.op('act', lambda e, c2=c2, pk=pk, s=s: e.activation(out=kmT[s].t[:, c2, :], in_=pk.t[:, 0:MEM], func=AF.Copy),
                         reads=[pk.r], writes=[kmT[s].r])
                for t in range(2):
                    pv = pm.next()
                    for c in range(NCH):
                        P.op('pe', lambda e, c=c, t=t, pv=pv, s=s: e.matmul(
                            pv.t[:, 0:256], lhsT=self.memT[s].t[:, c, t * 128:(t + 1) * 128], rhs=wkv.t[:, c, 256:512],
                            start=(c == 0), stop=(c == NCH - 1)), reads=[wkv.r, self.memT[s].r], writes=[pv.r])
                    P.op('act', lambda e, t=t, pv=pv, s=s: e.activation(
                        out=vm1[s].t[:, t, :, 0:64], in_=pv.t[:, 0:256].rearrange("p (h d) -> p h d", h=4), func=AF.Copy),
                        reads=[pv.r], writes=[vm1[s].r])
            xts = [self.sbuf(st, f'xt{j}', [128, D], F32) for j in range(4)]
            junk = self.sbuf(st, 'junk', [128, D], BF16)
            xbp = self.sbufs(st, 'xb', [128, D], BF16, 2)
            ssp = self.sbufs(st, 'ss', [128, 2], F32, 4)
            xnT = self.sbuf(st, 'xnT', [128, NCH, 512], BF16)
            xnT_res = [Res() for _ in range(4)]
            oTb = self.sbuf(st, 'oTb', [128, 12, 512], BF16)
            sgp = self.sbufs(st, 'sg', [128, 512], F32, 3)
            ttp = self.sbufs(st, 'tt', [128, 512], F32, 3)
            ysum = self.sbufs(st, 'ysum', [128, 512], F32, 2)
            yT = self.sbuf(st, 'yT', [128, NCH, 512], BF16)
            yT_res = [Res() for _ in range(NCH)]
            qxT = self.sbuf(st, 'qxT', [128, 2, 512], BF16)
            Ep = self.sbufs(st, 'E', [128, 512], BF16, 3)
            recp = self.sbufs(st, 'rec', [128, 1], F32, 8)
            ox = self.sbuf(st, 'ox', [128, 4, 256], BF16)
            oxT = self.sbuf(st, 'oxT', [128, 2, 512], BF16)
            for s in range(self.NSEQ):
                for tb in range(self.NB):
                    T0 = tb * 512
                    self.load_x_block(l, s, T0, 4, xts, xbp, ssp, junk, tp, xnT, xnT_res)
                    P.dma('sp', lambda e, s=s, T0=T0: e.dma_start(
                        out=oTb.t[:], in_=X['oT'][s, :, T0:T0 + 512].rearrange("(c p) q -> p c q", p=128)), writes=[oTb.r])
                    for f in range(NCH):
                        tts = []
                        for i in range(3):
                            pg = pm.next()
                            for c in range(NCH):
                                P.op('pe', lambda e, c=c, i=i, f=f, pg=pg: e.matmul(
                                    pg.t[:], lhsT=wg.t[:, c, i * 1024 + f * 128: i * 1024 + (f + 1) * 128], rhs=xnT.t[:, c, :],
                                    start=(c == 0), stop=(c == NCH - 1)), reads=[wg.r] + xnT_res, writes=[pg.r])
                            sg = sgp.next()
                            P.op('act', lambda e, sg=sg, pg=pg: e.activation(out=sg.t[:], in_=pg.t[:], func=AF.Tanh, scale=0.5),
                                 reads=[pg.r], writes=[sg.r])
                            pu = pm.next()
                            for c in range(4):
                                P.op('pe', lambda e, c=c, i=i, f=f, pu=pu: e.matmul(
                                    pu.t[:], lhsT=wu.t[:, 4 * i + c, f * 128:(f + 1) * 128], rhs=oTb.t[:, 4 * i + c, :],
                                    start=(c == 0), stop=(c == 3)), reads=[wu.r, oTb.r], writes=[pu.r])
                            tt = ttp.next()
                            P.op('dve', lambda e, sg=sg, pu=pu, tt=tt: e.scalar_tensor_tensor(
                                out=tt.t[:], in0=sg.t[:], scalar=1.0, in1=pu.t[:], op0=ALU.add, op1=ALU.mult),
                                reads=[sg.r, pu.r], writes=[tt.r])
                            tts.append(tt)
                        ys = ysum.next()
                        P.op('pool', lambda e, ys=ys, tts=tts: e.tensor_tensor(out=ys.t[:], in0=tts[0].t[:], in1=tts[1].t[:], op=ALU.add),
                             reads=[tts[0].r, tts[1].r], writes=[ys.r])
                        P.op('pool', lambda e, ys=ys, tts=tts, f=f: e.tensor_tensor(out=yT.t[:, f, :], in0=ys.t[:], in1=tts[2].t[:], op=ALU.add),
                             reads=[ys.r, tts[2].r], writes=[yT_res[f]])
                    for j in range(4):
                        for hf in range(2):
                            po = pm.next()
                            for f in range(NCH):
                                P.op('pe', lambda e, f=f, j=j, hf=hf, po=po: e.matmul(
                                    po.t[:], lhsT=yT.t[:, f, j * 128:(j + 1) * 128], rhs=wo.t[:, f, hf * 512:(hf + 1) * 512],
                                    start=(f == 0), stop=(f == NCH - 1)), reads=[wo.r] + yT_res, writes=[po.r])
                            P.op('dve', lambda e, j=j, hf=hf, po=po: e.tensor_tensor(
                                out=xts[j].t[:, hf * 512:(hf + 1) * 512], in0=po.t[:], in1=xts[j].t[:, hf * 512:(hf + 1) * 512], op=ALU.add),
                                reads=[po.r, xts[j].r], writes=[xts[j].r])
                    self.load_x_block(l, s, T0, 4, xts, xbp, ssp, junk, tp, xnT, xnT_res, load=False)
                    for c2 in range(2):
                        pq = pm.next()
                        for c in range(NCH):
                            P.op('pe', lambda e, c=c, c2=c2, pq=pq: e.matmul(
                                pq.t[:], lhsT=wq.t[:, c, c2 * 128:(c2 + 1) * 128], rhs=xnT.t[:, c, :],
                                start=(c == 0), stop=(c == NCH - 1)), reads=[wq.r] + xnT_res, writes=[pq.r])
                        P.op('act', lambda e, c2=c2, pq=pq: e.activation(out=qxT.t[:, c2, :], in_=pq.t[:], func=AF.Copy),
                             reads=[pq.r], writes=[qxT.r])
                    for h in range(4):
                        pr = slice((h % 2) * 64, (h % 2) * 64 + 64)
                        for t in range(2):
                            sc = pm.next()
                            P.op('pe', lambda e, sc=sc, pr=pr, h=h, t=t, s=s: e.matmul(
                                sc.t[:], lhsT=kmT[s].t[pr, h // 2, t * 128:(t + 1) * 128], rhs=qxT.t[pr, h // 2, :], start=True, stop=True),
                                reads=[kmT[s].r, qxT.r], writes=[sc.r])
                            E = Ep.next()
                            P.op('act', lambda e, sc=sc, E=E: e.activation(out=E.t[:], in_=sc.t[:], func=AF.Exp, scale=0.125),
                                 reads=[sc.r], writes=[E.r])
                            for j in range(4):
                                P.op('pe', lambda e, E=E, j=j, t=t, h=h, s=s: e.matmul(
                                    acc[j].t[:, 0:65], lhsT=E.t[:, j * 128:(j + 1) * 128], rhs=vm1[s].t[:, t, h, :],
                                    start=(t == 0), stop=(t == 1)), reads=[E.r, vm1[s].r], writes=[acc[j].r])
                        for j in range(4):
                            rec = recp.next()
                            P.op('dve', lambda e, rec=rec, j=j: e.reciprocal(out=rec.t[:], in_=acc[j].t[:, 64:65]), reads=[acc[j].r], writes=[rec.r])
                            P.op('dve', lambda e, rec=rec, j=j, h=h: e.tensor_scalar(
                                out=ox.t[:, j, h * 64:(h + 1) * 64], in0=acc[j].t[:, 0:64], scalar1=rec.t[:, 0:1], scalar2=None, op0=ALU.mult),
                                reads=[acc[j].r, rec.r], writes=[ox.r])
                    pt = tp.next()
                    for c2 in range(2):
                        for j in range(4):
                            P.op('pe', lambda e, c2=c2, j=j, pt=pt: e.transpose(
                                out=pt.t[:, (c2 * 4 + j) * 128:(c2 * 4 + j + 1) * 128], in_=ox.t[:, j, c2 * 128:(c2 + 1) * 128], identity=self.ident.t[:]),
                                reads=[ox.r, self.ident.r], writes=[pt.r])
                    P.op('dve', lambda e, pt=pt: e.tensor_copy(out=oxT.t[:], in_=pt.t[:].rearrange("p (c q) -> p c q", c=2)),
                         reads=[pt.r], writes=[oxT.r])
                    for j in range(4):
                        for hf in range(2):
                            po = pm.next()
                            for c2 in range(2):
                                P.op('pe', lambda e, c2=c2, j=j, hf=hf, po=po: e.matmul(
                                    po.t[:], lhsT=oxT.t[:, c2, j * 128:(j + 1) * 128], rhs=wox.t[:, c2, hf * 512:(hf + 1) * 512],
                                    start=(c2 == 0), stop=(c2 == 1)), reads=[wox.r, oxT.r], writes=[po.r])
                            P.op('dve', lambda e, j=j, hf=hf, po=po: e.tensor_tensor(
                                out=xts[j].t[:, hf * 512:(hf + 1) * 512], in0=po.t[:], in1=xts[j].t[:, hf * 512:(hf + 1) * 512], op=ALU.add),
                                reads=[po.r, xts[j].r], writes=[xts[j].r])
                        dview = X['xres'][s, T0 + j * 128:T0 + (j + 1) * 128, :]
                        P.dma('pool', lambda e, j=j, dview=dview: e.dma_start(out=dview, in_=xts[j].t[:]), reads=[xts[j].r])
            P.barrier()

    def phase3b(self, l):
        P, I, X, S = self.P, self.I, self.X, self.S
        last = (l == self.DEPTH - 1)
        TB = 256
        with ExitStack() as st:
            wgu = self.sbuf(st, 'wgu', [128, NCH, 2 * DFF], BF16)
            wd = self.sbuf(st, 'wd', [128, NFF, D], BF16)
            gn = self.gains
            with ExitStack() as st2:
                stage = self.sbufs(st2, 'stage', [128, 1408], F32, 2)
                self.load_weight(stage, wgu, lambda c, c0, c1: wgu.t[:, c, c0:c1], I['w_gu'][l], NCH, 2 * DFF,
                                 lambda c: (gn.t[:, 2, l, c, :], gn.r))
                self.load_weight(stage, wd, lambda c, c0, c1: wd.t[:, c, c0:c1], I['w_down'][l], NFF, D, lambda c: None)
                P.barrier()
            tp = self.pss(st, 'tp', [128, D], BF16, 2)
            pm = self.pss(st, 'pm', [128, 512], F32, 6)
            xts = [self.sbuf(st, f'xt{j}', [128, D], F32) for j in range(4)]
            junk = self.sbuf(st, 'junk', [128, D], BF16)
            xbp = self.sbufs(st, 'xb', [128, D], BF16, 2)
            ssp = self.sbufs(st, 'ss', [128, 2], F32, 4)
            hnT = [self.sbuf(st, f'hnT{i}', [128, NCH, TB], BF16) for i in range(2)]
            hnT_res = [[Res() for _ in range(2)] for _ in range(2)]
            hT = self.sbuf(st, 'hT', [128, NFF, TB], BF16)
            hT_res = [Res() for _ in range(NFF)]
            slp = self.sbufs(st, 'sl', [128, TB], F32, 3)
            if last:
                fin_g = self.sbuf(st, 'fin_g', [128, D], F32)
                P.dma('sp', lambda e: e.dma_start(out=fin_g.t[:], in_=I['final_norm'].partition_broadcast(128)), writes=[fin_g.r])
                outp = self.sbufs(st, 'outp', [128, D], F32, 2)
            blk = 0
            for s in range(self.NSEQ):
                for tb in range(S // TB):
                    T0 = tb * TB
                    xs = xts[(blk % 2) * 2:(blk % 2) * 2 + 2]
                    hn, hr = hnT[blk % 2], hnT_res[blk % 2]
                    blk += 1
                    self.load_x_block(l + 1, s, T0, 2, xs, xbp, ssp, junk, tp, hn, hr)
                    for f in range(NFF):
                        pg, pu = pm.next(), pm.next()
                        for (pp, off) in ((pg, 0), (pu, DFF)):
                            for c in range(NCH):
                                P.op('pe', lambda e, c=c, f=f, pp=pp, off=off, hn=hn: e.matmul(
                                    pp.t[:, 0:TB], lhsT=wgu.t[:, c, off + f * 128: off + (f + 1) * 128], rhs=hn.t[:, c, :],
                                    start=(c == 0), stop=(c == NCH - 1)), reads=[wgu.r] + hr, writes=[pp.r])
                        sl = slp.next()
                        P.op('act', lambda e, sl=sl, pg=pg: e.activation(out=sl.t[:], in_=pg.t[:, 0:TB], func=AF.Silu),
                             reads=[pg.r], writes=[sl.r])
                        P.op('dve', lambda e, sl=sl, pu=pu, f=f: e.tensor_tensor(out=hT.t[:, f, :], in0=pu.t[:, 0:TB], in1=sl.t[:], op=ALU.mult),
                             reads=[pu.r, sl.r], writes=[hT_res[f]])
                    for j in range(2):
                        for hf in range(2):
                            po = pm.next()
                            for f in range(NFF):
                                P.op('pe', lambda e, f=f, j=j, hf=hf, po=po: e.matmul(
                                    po.t[:], lhsT=hT.t[:, f, j * 128:(j + 1) * 128], rhs=wd.t[:, f, hf * 512:(hf + 1) * 512],
                                    start=(f == 0), stop=(f == NFF - 1)), reads=[wd.r] + hT_res, writes=[po.r])
                            P.op('dve', lambda e, j=j, hf=hf, po=po, xs=xs: e.tensor_tensor(
                                out=xs[j].t[:, hf * 512:(hf + 1) * 512], in0=po.t[:], in1=xs[j].t[:, hf * 512:(hf + 1) * 512], op=ALU.add),
                                reads=[po.r, xs[j].r], writes=[xs[j].r])
                        row0 = T0 + j * 128
                        if not last:
                            dview = X['xres'][s, row0:row0 + 128, :]
                            P.dma('pool', lambda e, j=j, dview=dview, xs=xs: e.dma_start(out=dview, in_=xs[j].t[:]), reads=[xs[j].r])
                        else:
                            ss = ssp.next()
                            o_ = outp.next()
                            x_t = xs[j]
                            P.op('act', lambda e, x_t=x_t, ss=ss: e.activation(out=junk.t[:], in_=x_t.t[:], func=AF.Square, accum_out=ss.t[:, 0:1]),
                                 reads=[x_t.r], writes=[junk.r, ss.r])
                            P.op('dve', lambda e, ss=ss: e.tensor_scalar(out=ss.t[:, 1:2], in0=ss.t[:, 0:1], scalar1=1.0 / D, scalar2=EPS,
                                                                        op0=ALU.mult, op1=ALU.add), reads=[ss.r], writes=[ss.r])
                            P.op('pool', lambda e, ss=ss: e.tensor_tensor(out=ss.t[:, 0:1], in0=ss.t[:, 1:2], in1=self.neghalf.t[:, 0:1], op=ALU.pow),
                                 reads=[ss.r, self.neghalf.r], writes=[ss.r])
                            P.op('dve', lambda e, x_t=x_t, ss=ss, o_=o_: e.scalar_tensor_tensor(
                                out=o_.t[:], in0=x_t.t[:], scalar=ss.t[:, 0:1], in1=fin_g.t[:], op0=ALU.mult, op1=ALU.mult),
                                reads=[x_t.r, ss.r, fin_g.r], writes=[o_.r])
                            dview = self.out[s, row0:row0 + 128, :]
                            P.dma('pool', lambda e, dview=dview, o_=o_: e.dma_start(out=dview, in_=o_.t[:]), reads=[o_.r])
            P.barrier()


def make_consts(S):
    bf = ml_dtypes.bfloat16
    ident = np.eye(128, dtype=np.float32).astype(bf)
    rot = np.zeros((128, 128), np.float32)
    for blk in range(2):
        for j in range(32):
            rot[blk * 64 + j + 32, blk * 64 + j] = -1.0
            rot[blk * 64 + j, blk * 64 + j + 32] = 1.0
    pos = np.arange(S, dtype=np.float32)
    inv = (10000.0 ** (-np.arange(0, 64, 2, dtype=np.float32) / 64)).astype(np.float32)
    ang = pos[None, :] * inv[:, None]
    cos = np.tile(np.cos(ang), (4, 1)).astype(bf)
    sin = np.tile(np.sin(ang), (4, 1)).astype(bf)
    cm = np.zeros((128, 4, 512), np.float32)
    for t in range(4):
        for p in range(128):
            kc = 2 * t + p // 64
            for fb in range(8):
                if kc > fb:
                    cm[p, t, fb * 64:(fb + 1) * 64] = NEG
    flip = np.eye(128, dtype=np.float32)[::-1].copy().astype(bf)
    return {'c_ident': ident, 'c_flip': flip, 'c_rot': rot.astype(bf), 'c_cos': cos, 'c_sin': sin, 'c_cmask': cm.astype(bf)}


_CACHE = {}


def kernel(**inputs):
    x = np.asarray(inputs['x'], np.float32)
    B, S, _ = x.shape
    NCORES = 8
    NSEQ = B // NCORES
    DEPTH = inputs['w_in'].shape[0]
    key = (S, NSEQ, DEPTH)
    if key not in _CACHE:
        _CACHE[key] = Builder(S, NSEQ, DEPTH).build()
    nc = _CACHE[key]
    consts = make_consts(S)
    shared = {k: np.ascontiguousarray(np.asarray(v, np.float32)) for k, v in inputs.items() if k not in ('x', 'mem')}
    shared['lambda_vecs'] = shared['lambda_vecs'].reshape(DEPTH, 256)
    shared.update(consts)
    mem = np.asarray(inputs['mem'], np.float32)
    in_maps = []
    for c in range(NCORES):
        m = dict(shared)
        m['x'] = np.ascontiguousarray(x[c * NSEQ:(c + 1) * NSEQ])
        m['mem'] = np.ascontiguousarray(mem[c * NSEQ:(c + 1) * NSEQ])
        in_maps.append(m)
    res = run_bass_kernel_spmd(nc, in_maps, core_ids=list(range(NCORES)))
    return np.concatenate([r['out'] for r in res.results], axis=0).astype(np.float32)
```

```python
import math
import os
KSKIP = os.environ.get('KSKIP', '')
from contextlib import ExitStack

import numpy as np
import ml_dtypes

import concourse.bass as bass
import concourse.mybir as mybir
from concourse.bass_utils import run_bass_kernel_spmd

F32 = mybir.dt.float32
BF16 = mybir.dt.bfloat16
AF = mybir.ActivationFunctionType
ALU = mybir.AluOpType
AX = mybir.AxisListType

LIMIT = 30000
ENGS = ['pe', 'act', 'dve', 'pool', 'sp']

D = 1024
NCH = 8
MEM = 256
DFF = 2816
NFF = 22
N_IN = 8004
EPS = 1e-6
NEG = -240000.0
NIT = 16
TOPK = 256


class Res:
    __slots__ = ('name', 'w', 'r')

    def __init__(self, name=''):
        self.name = name
        self.w = None
        self.r = []


class Buf:
    __slots__ = ('t', 'r')

    def __init__(self, t, name=''):
        self.t = t
        self.r = Res(name)


class Prog:
    def __init__(self, nc, stack, dma_pool=None):
        self.nc = nc
        self.stack = stack
        self.streams = {e: [] for e in ENGS}
        self.idx = {e: 0 for e in ENGS}
        self.clock = {e: {} for e in ENGS}
        self.esems = {e: [] for e in ENGS}
        dma_pool = dma_pool or {'sp': 40, 'act': 4, 'pool': 24}
        self.dpool = {}
        self.dnext = {q: 0 for q in dma_pool}
        self.dcount = {}
        self.dsem = {}
        for q, n in dma_pool.items():
            self.dpool[q] = []
            for i in range(n):
                s = stack.enter_context(nc.semaphore(f"dq_{q}_{i}"))
                self.dpool[q].append(s)
                self.dcount[(q, i)] = 0
                self.dsem[(q, i)] = s
        self.nwaits = 0

    def _esem(self, eng, epoch):
        while len(self.esems[eng]) <= epoch:
            s = self.stack.enter_context(self.nc.semaphore(f"e_{eng}_{len(self.esems[eng])}"))
            self.esems[eng].append(s)
        return self.esems[eng][epoch]

    def _need(self, eng, tok, kind):
        key, val, snap = tok
        if key == eng:
            if eng == 'pe' or kind != 'raw':
                return
        ck = self.clock[eng]
        if ck.get(key, 0) >= val:
            return
        self.streams[eng].append(('wait', key, val))
        self.nwaits += 1
        new = dict(ck)
        for k, v in snap.items():
            if new.get(k, 0) < v:
                new[k] = v
        if new.get(key, 0) < val:
            new[key] = val
        self.clock[eng] = new

    def _deps(self, eng, reads, writes):
        for res in reads:
            if res.w is not None:
                self._need(eng, res.w, 'raw')
        for res in writes:
            if res.w is not None:
                self._need(eng, res.w, 'waw')
            for t in res.r:
                self._need(eng, t, 'war')

    def _commit(self, tok, reads, writes):
        for res in writes:
            res.w = tok
            res.r = []
        key = tok[0]
        for res in reads:
            if res in writes:
                continue
            if isinstance(key, str):
                res.r = [t for t in res.r if t[0] != key]
            res.r.append(tok)

    def op(self, eng, fn, reads=(), writes=()):
        self._deps(eng, reads, writes)
        self.idx[eng] += 1
        i = self.idx[eng]
        self.streams[eng].append(('op', fn, i))
        tok = (eng, i, self.clock[eng])
        self._commit(tok, reads, writes)
        return tok

    def dma(self, q, fn, reads=(), writes=()):
        self._deps(q, reads, writes)
        slot = self.dnext[q]
        self.dnext[q] = (slot + 1) % len(self.dpool[q])
        key = (q, slot)
        prev = self.dcount[key]
        if prev > 0:
            self._need(q, (key, prev, {}), 'raw')
        val = prev + 16
        assert val < 2 * LIMIT, "dma sem overflow"
        self.dcount[key] = val
        self.streams[q].append(('dma', fn, key))
        tok = (key, val, self.clock[q])
        self._commit(tok, reads, writes)
        return tok

    def barrier(self):
        for key, val in self.dcount.items():
            if val > 0:
                self._need('sp', (key, val, {}), 'raw')
        for e in ENGS:
            if e != 'sp' and self.idx[e] > 0:
                self._need('sp', (e, self.idx[e], self.clock[e]), 'raw')
        tok = self.op('sp', lambda e: e.nop())
        for e in ENGS:
            if e != 'sp':
                self._need(e, tok, 'raw')

    def finish(self):
        self.barrier()

    def emit(self):
        nc = self.nc
        handles = {'pe': 'tensor', 'act': 'scalar', 'dve': 'vector', 'pool': 'gpsimd', 'sp': 'sync'}
        for eng in ENGS:
            for ep in range((self.idx[eng] + LIMIT - 1) // LIMIT + 1):
                self._esem(eng, ep)
        with nc.Block() as block:
            for eng in ENGS:
                stream = self.streams[eng]

                def body(e, eng=eng, stream=stream):
                    for item in stream:
                        if item[0] == 'wait':
                            _, key, val = item
                            if isinstance(key, str):
                                e.wait_ge(self.esems[key][(val - 1) // LIMIT], (val - 1) % LIMIT + 1)
                            else:
                                e.wait_ge(self.dsem[key], val)
                        elif item[0] == 'op':
                            _, fn, i = item
                            fn(e).then_inc(self.esems[eng][(i - 1) // LIMIT], 1)
                        else:
                            _, fn, key = item
                            fn(e).then_inc(self.dsem[key], 16)

                getattr(block, handles[eng])(body)


class Rot:
    def __init__(self, bufs):
        self.bufs = bufs
        self.i = 0

    def next(self):
        b = self.bufs[self.i]
        self.i = (self.i + 1) % len(self.bufs)
        return b


class Builder:
    def __init__(self, S, NSEQ, DEPTH, debug=False, phases=None):
        self.S, self.NSEQ, self.DEPTH, self.debug = S, NSEQ, DEPTH, debug
        self.phases = phases
        self.NT = S // 128
        self.NB = S // 512
        self.nc = bass.Bass("TRN2", target_bir_lowering=False)
        self.uid = 0

    def dram_in(self, name, shape, dt=F32):
        return self.nc.dram_tensor(name, list(shape), dt, kind="ExternalInput").ap()

    def dram_scr(self, name, shape, dt):
        kind = "ExternalOutput" if self.debug else "Internal"
        return self.nc.dram_tensor(name, list(shape), dt, kind=kind).ap()

    def sb(self, st, name, shape, dt):
        self.uid += 1
        return st.enter_context(self.nc.sbuf_tensor(f"{name}_{self.uid}", list(shape), dt))

    def sbuf(self, st, name, shape, dt):
        return Buf(self.sb(st, name, shape, dt), name)

    def sbufs(self, st, name, shape, dt, n):
        return Rot([self.sbuf(st, f"{name}{i}", shape, dt) for i in range(n)])

    def ps(self, st, name, shape, dt):
        self.uid += 1
        return Buf(st.enter_context(self.nc.psum_tensor(f"{name}_{self.uid}", list(shape), dt)), name)

    def pss(self, st, name, shape, dt, n):
        return Rot([self.ps(st, f"{name}{i}", shape, dt) for i in range(n)])

    def build(self):
        nc = self.nc
        S, NSEQ, DEPTH = self.S, self.NSEQ, self.DEPTH
        L = DEPTH
        I = {}
        I['x'] = self.dram_in('x', [NSEQ, S, D])
        I['mem'] = self.dram_in('mem', [NSEQ, MEM, D])
        I['norm_mix'] = self.dram_in('norm_mix', [L, D])
        I['w_in'] = self.dram_in('w_in', [L, D, N_IN])
        I['rel_bias_a'] = self.dram_in('rel_bias_a', [L, 8, 320])
        I['lambda_vecs'] = self.dram_in('lambda_vecs', [L, 256])
        I['subln_b'] = self.dram_in('subln_b', [L, 128])
        I['w_up_a'] = self.dram_in('w_up_a', [L, 512, D])
        I['w_up_b'] = self.dram_in('w_up_b', [L, 512, D])
        I['w_up_c'] = self.dram_in('w_up_c', [L, 512, D])
        I['w_out'] = self.dram_in('w_out', [L, D, D])
        I['norm_cross'] = self.dram_in('norm_cross', [L, D])
        I['w_q_x'] = self.dram_in('w_q_x', [L, D, 256])
        I['w_kv_x'] = self.dram_in('w_kv_x', [L, D, 512])
        I['w_o_x'] = self.dram_in('w_o_x', [L, 256, D])
        I['norm_ffn'] = self.dram_in('norm_ffn', [L, D])
        I['w_gu'] = self.dram_in('w_gu', [L, D, 2 * DFF])
        I['w_down'] = self.dram_in('w_down', [L, DFF, D])
        I['mem_norm'] = self.dram_in('mem_norm', [D])
        I['final_norm'] = self.dram_in('final_norm', [D])
        I['c_ident'] = self.dram_in('c_ident', [128, 128], BF16)
        I['c_rot'] = self.dram_in('c_rot', [128, 128], BF16)
        I['c_flip'] = self.dram_in('c_flip', [128, 128], BF16)
        I['c_cos'] = self.dram_in('c_cos', [128, S], BF16)
        I['c_sin'] = self.dram_in('c_sin', [128, S], BF16)
        I['c_cmask'] = self.dram_in('c_cmask', [128, 4, 512], BF16)
        self.I = I
        self.out = nc.dram_tensor('out', [NSEQ, S, D], F32, kind="ExternalOutput").ap()
        X = {}
        X['xres'] = self.dram_scr('xres', [NSEQ, S, D], F32)
        for nm in ['qaT', 'kaT', 'qbT', 'kbT', 'qcT', 'kcT']:
            X[nm] = self.dram_scr(nm, [NSEQ, 512, S], BF16)
        X['qiT'] = self.dram_scr('qiT', [NSEQ, 256, S], BF16)
        X['kiT'] = self.dram_scr('kiT', [NSEQ, 128, S], BF16)
        X['va1'] = self.dram_scr('va1', [NSEQ, S, 8 * 65], BF16)
        X['vb1'] = self.dram_scr('vb1', [NSEQ, S, 4 * 129], BF16)
        X['vc1'] = self.dram_scr('vc1', [NSEQ, S, 8 * 65], BF16)
        X['wi'] = self.dram_scr('wi', [NSEQ, S, 4], F32)
        X['oT'] = self.dram_scr('oT', [NSEQ, 1536, S], BF16)
        X['ebias'] = self.dram_scr('ebias', [8, 1536], BF16)
        self.X = X

        with ExitStack() as st:
            self.P = P = Prog(nc, st)
            self.ident = self.sbuf(st, 'ident', [128, 128], BF16)
            self.rot = self.sbuf(st, 'rot', [128, 128], BF16)
            self.flip = self.sbuf(st, 'flip', [128, 128], BF16)
            self.gains = self.sbuf(st, 'gains', [128, 3, L, 8, 1], F32)
            self.gmem = self.sbuf(st, 'gmem', [128, 8, 1], F32)
            self.subln = self.sbuf(st, 'subln', [128, L, 1], F32)
            self.neglam = self.sbuf(st, 'neglam', [128, L], F32)
            self.neghalf = self.sbuf(st, 'neghalf', [128, 4], F32)
            self.memT = [self.sbuf(st, f'memT{s}', [128, NCH, MEM], BF16) for s in range(NSEQ)]
            self.setup()
            for l in range(DEPTH):
                if self.want('p1'):
                    self.phase1(l)
                if self.want('p2'):
                    for s in range(NSEQ):
                        self.phase2(l, s)
                if self.want('p3a'):
                    self.phase3a(l)
                if self.want('p3b'):
                    self.phase3b(l)
            P.finish()
            P.emit()
        return nc

    def want(self, ph):
        if self.phases is None:
            return True
        if ph in ('p2a', 'p2b', 'p2c') and 'p2' in self.phases and not any(k in self.phases for k in ('p2a', 'p2b', 'p2c')):
            return True
        if ph == 'p2':
            return any(k in self.phases for k in ('p2', 'p2a', 'p2b', 'p2c'))
        return ph in self.phases

    def lam_init(self, l):
        return 0.8 - 0.6 * math.exp(-0.3 * l)

    def setup(self):
        P, I, L = self.P, self.I, self.DEPTH
        ident, rot, gains = self.ident, self.rot, self.gains
        P.dma('sp', lambda e: e.dma_start(out=ident.t[:], in_=I['c_ident'][:, :]), writes=[ident.r])
        P.dma('sp', lambda e: e.dma_start(out=rot.t[:], in_=I['c_rot'][:, :]), writes=[rot.r])
        P.dma('sp', lambda e: e.dma_start(out=self.flip.t[:], in_=I['c_flip'][:, :]), writes=[self.flip.r])
        for i, nm in enumerate(['norm_mix', 'norm_cross', 'norm_ffn']):
            src = I[nm].rearrange("l (c p o) -> p l c o", p=128, o=1)
            P.dma('sp', lambda e, i=i, src=src: e.dma_start(out=gains.t[:, i], in_=src, allow_slow_non_contiguous=True), writes=[gains.r])
        P.dma('sp', lambda e: e.dma_start(out=self.gmem.t[:], in_=I['mem_norm'].rearrange("(c p o) -> p c o", p=128, o=1), allow_slow_non_contiguous=True),
              writes=[self.gmem.r])
        P.dma('sp', lambda e: e.dma_start(out=self.subln.t[:], in_=I['subln_b'].rearrange("l (p o) -> p l o", o=1), allow_slow_non_contiguous=True),
              writes=[self.subln.r])
        P.op('dve', lambda e: e.memset(self.neghalf.t[:], -0.5), writes=[self.neghalf.r])
        with ExitStack() as st:
            lv = self.sbuf(st, 'lv', [128, L * 256], F32)
            tmp = self.sbuf(st, 'lvtmp', [128, L, 2, 64], F32)
            sm = self.sbuf(st, 'lvs', [128, L, 2], F32)
            ex = self.sbuf(st, 'lve', [128, L, 2], F32)
            src = I['lambda_vecs'].rearrange("l k -> (l k)").partition_broadcast(128)
            P.dma('sp', lambda e: e.dma_start(out=lv.t[:], in_=src), writes=[lv.r])
            lv4 = lv.t[:].rearrange("p (l a b k) -> p l a b k", l=L, a=2, b=2)
            P.op('dve', lambda e: e.tensor_tensor(out=tmp.t[:], in0=lv4[:, :, :, 0, :], in1=lv4[:, :, :, 1, :], op=ALU.mult),
                 reads=[lv.r], writes=[tmp.r])
            P.op('dve', lambda e: e.tensor_reduce(out=sm.t[:], in_=tmp.t[:], axis=AX.X, op=ALU.add),
                 reads=[tmp.r], writes=[sm.r])
            P.op('act', lambda e: e.activation(out=ex.t[:], in_=sm.t[:], func=AF.Exp), reads=[sm.r], writes=[ex.r])
            for l in range(L):
                P.op('dve', lambda e, l=l: e.tensor_scalar(out=self.neglam.t[:, l:l + 1], in0=ex.t[:, l, 1:2],
                                                          scalar1=ex.t[:, l, 0:1], scalar2=-self.lam_init(l),
                                                          op0=ALU.subtract, op1=ALU.add),
                     reads=[ex.r], writes=[self.neglam.r])
            mt = self.sbufs(st, 'memt', [128, D], F32, 2)
            junk = self.sbuf(st, 'memjunk', [128, D], F32)
            mb = self.sbufs(st, 'memb', [128, D], BF16, 2)
            ssq = self.sbufs(st, 'memss', [128, 2], F32, 2)
            tp = self.pss(st, 'memtp', [128, D], BF16, 2)
            for s in range(self.NSEQ):
                for j in range(2):
                    x_t, x_b, ss, pt = mt.next(), mb.next(), ssq.next(), tp.next()
                    P.dma('sp', lambda e, s=s, j=j, x_t=x_t: e.dma_start(out=x_t.t[:], in_=I['mem'][s, j * 128:(j + 1) * 128, :]),
                          writes=[x_t.r])
                    self.rms_to_bf16(x_t, junk, ss, x_b)
                    self.transpose_to(x_b, pt, self.memT[s], j * 128, 128, 'dve')
        P.barrier()

    def rms_to_bf16(self, x_t, junk, ss, x_b):
        P = self.P
        P.op('act', lambda e: e.activation(out=junk.t[:], in_=x_t.t[:], func=AF.Square, accum_out=ss.t[:, 0:1]),
             reads=[x_t.r], writes=[junk.r, ss.r])
        P.op('dve', lambda e: e.tensor_scalar(out=ss.t[:, 1:2], in0=ss.t[:, 0:1], scalar1=1.0 / D, scalar2=EPS,
                                              op0=ALU.mult, op1=ALU.add), reads=[ss.r], writes=[ss.r])
        P.op('pool', lambda e: e.tensor_tensor(out=ss.t[:, 0:1], in0=ss.t[:, 1:2], in1=self.neghalf.t[:, 0:1], op=ALU.pow),
             reads=[ss.r, self.neghalf.r], writes=[ss.r])
        P.op('dve', lambda e: e.tensor_scalar(out=x_b.t[:], in0=x_t.t[:], scalar1=ss.t[:, 0:1], scalar2=None, op0=ALU.mult),
             reads=[x_t.r, ss.r], writes=[x_b.r])

    def transpose_to(self, x_b, pt, dstT, col0, ncols, eng, dst_res=None):
        P = self.P
        for c in range(NCH):
            P.op('pe', lambda e, c=c: e.transpose(out=pt.t[:, c * 128:(c + 1) * 128], in_=x_b.t[:, c * 128:(c + 1) * 128],
                                                  identity=self.ident.t[:]),
                 reads=[x_b.r, self.ident.r], writes=[pt.r])
        src = pt.t[:].rearrange("p (c t) -> p c t", c=NCH)
        dres = dst_res if dst_res is not None else dstT.r
        if eng == 'act':
            P.op('act', lambda e: e.activation(out=dstT.t[:, :, col0:col0 + ncols], in_=src, func=AF.Copy),
                 reads=[pt.r], writes=[dres])
        else:
            P.op('dve', lambda e: e.tensor_copy(out=dstT.t[:, :, col0:col0 + ncols], in_=src), reads=[pt.r], writes=[dres])

    def load_weight(self, st_stage, dst, dst_sl, src_ap, nrows_chunks, ncols, scale_fn, const=1.0, rowchunk0=0):
        P = self.P
        stage = st_stage
        CW = stage.bufs[0].t.shape[1]
        for c in range(nrows_chunks):
            for c0 in range(0, ncols, CW):
                c1 = min(ncols, c0 + CW)
                sg = stage.next()
                P.dma('sp', lambda e, c=c, c0=c0, c1=c1, sg=sg: e.dma_start(
                    out=sg.t[:, 0:c1 - c0], in_=src_ap[(rowchunk0 + c) * 128:(rowchunk0 + c + 1) * 128, c0:c1]), writes=[sg.r])
                sc = scale_fn(c)
                rd = [sg.r] + ([sc[1]] if sc is not None else [])
                self.cast_rr = getattr(self, 'cast_rr', 0) + 1
                ceng = 'pool' if self.cast_rr % 2 else 'dve'
                if sc is not None:
                    P.op(ceng, lambda e, c=c, c0=c0, c1=c1, sg=sg, sc=sc: e.tensor_scalar(
                        out=dst_sl(c, c0, c1), in0=sg.t[:, 0:c1 - c0], scalar1=sc[0], scalar2=const,
                        op0=ALU.mult, op1=ALU.mult), reads=rd, writes=[dst.r])
                else:
                    P.op(ceng, lambda e, c=c, c0=c0, c1=c1, sg=sg: e.tensor_scalar(
                        out=dst_sl(c, c0, c1), in0=sg.t[:, 0:c1 - c0], scalar1=const, scalar2=1.0,
                        op0=ALU.mult, op1=ALU.mult), reads=rd, writes=[dst.r])

    def x_src(self, l):
        return self.I['x'] if l == 0 else self.X['xres']

    def phase1(self, l):
        P, I, X, S = self.P, self.I, self.X, self.S
        with ExitStack() as st:
            w1 = self.sbuf(st, 'w1', [128, NCH, 4932], BF16)
            wki = self.sbuf(st, 'wki', [128, NCH, 128], BF16)
            stage = self.sbufs(st, 'stage', [128, 1644], F32, 2)
            cosT = self.sbuf(st, 'cosT', [128, S], BF16)
            sinT = self.sbuf(st, 'sinT', [128, S], BF16)
            P.dma('sp', lambda e: e.dma_start(out=cosT.t[:], in_=I['c_cos'][:, :]), writes=[cosT.r])
            P.dma('sp', lambda e: e.dma_start(out=sinT.t[:], in_=I['c_sin'][:, :]), writes=[sinT.r])
            gm = self.gains
            self.load_weight(stage, w1, lambda c, c0, c1: w1.t[:, c, c0:c1], I['w_in'][l], NCH, 4932,
                             lambda c: (gm.t[:, 0, l, c, :], gm.r))
            for c in range(NCH):
                for h in range(2):
                    P.op('pool', lambda e, c=c, h=h: e.tensor_copy(out=wki.t[:, c, h * 64:(h + 1) * 64], in_=w1.t[:, c, 4864:4928]),
                         reads=[w1.r], writes=[wki.r])
            xt2 = [[self.sbuf(st, f'xt{i}_{j}', [128, D], F32) for j in range(4)] for i in range(2)]
            junk = self.sbuf(st, 'junk', [128, D], BF16)
            xb = self.sbufs(st, 'xb', [128, D], BF16, 2)
            ssq = self.sbufs(st, 'ss', [128, 2], F32, 4)
            xnT = [self.sbuf(st, f'xnT{i}', [128, NCH, 512], BF16) for i in range(2)]
            xnT_res = [[Res() for _ in range(4)] for _ in range(2)]
            tp = self.pss(st, 'tp', [128, D], BF16, 2)
            pm = self.pss(st, 'pm', [128, 512], F32, 4)
            pr = self.pss(st, 'pr', [128, 512], F32, 2)
            qsb = self.sbufs(st, 'qsb', [128, 512], BF16, 3)
            t1 = self.sbufs(st, 't1', [128, 512], F32, 2)
            t2 = self.sbufs(st, 't2', [128, 512], F32, 2)
            osb = self.sbufs(st, 'osb', [128, 512], BF16, 4)
            v65 = self.sbufs(st, 'v65', [128, 8, 65], BF16, 4)
            v129 = self.sbufs(st, 'v129', [128, 4, 129], BF16, 2)
            wsb = self.sbufs(st, 'wsb', [128, 4], F32, 2)
            for b_ in v65.bufs + v129.bufs:
                P.op('dve', lambda e, b_=b_: e.memset(b_.t[:], 1.0), writes=[b_.r])
            ftiles = []
            for i in range(4):
                ftiles.append(('qaT', i * 128, (w1, 0 + i * 128), False))
                ftiles.append(('kaT', i * 128, (w1, 512 + i * 128), False))
            for i in range(4):
                ftiles.append(('qbT', i * 128, (w1, 1536 + i * 128), True))
                ftiles.append(('kbT', i * 128, (w1, 2048 + i * 128), True))
                ftiles.append(('qcT', i * 128, (w1, 3072 + i * 128), True))
                ftiles.append(('kcT', i * 128, (w1, 3584 + i * 128), True))
            for i in range(2):
                ftiles.append(('qiT', i * 128, (w1, 4608 + i * 128), True))
            ftiles.append(('kiT', 0, (wki, 0), True))
            blk = 0
            blocks = [(s_, tb_) for s_ in range(self.NSEQ) for tb_ in range(self.NB)]

            def load_blk(i):
                s_, tb_ = blocks[i]
                for j in range(4):
                    x_t = xt2[i % 2][j]
                    src = self.x_src(l)[s_, tb_ * 512 + j * 128:tb_ * 512 + (j + 1) * 128, :]
                    P.dma('sp', lambda e, x_t=x_t, src=src: e.dma_start(out=x_t.t[:], in_=src), writes=[x_t.r])
            load_blk(0)
            for s in range(self.NSEQ):
                for tb in range(self.NB):
                    T0 = tb * 512
                    xn = xnT[blk % 2]
                    xr = xnT_res[blk % 2]
                    if blk + 1 < len(blocks):
                        load_blk(blk + 1)
                    xcur = xt2[blk % 2]
                    blk += 1
                    for j in range(4):
                        x_t, x_b, ss, pt = xcur[j], xb.next(), ssq.next(), tp.next()
                        self.rms_to_bf16(x_t, junk, ss, x_b)
                        self.transpose_to(x_b, pt, xn, j * 128, 128, 'dve', dst_res=xr[j])
                    for (dst, row0, (wt, col0), rope) in ftiles:
                        pmm = pm.next()
                        for c in range(NCH):
                            P.op('pe', lambda e, c=c, wt=wt, col0=col0, pmm=pmm, xn=xn: e.matmul(
                                pmm.t[:], lhsT=wt.t[:, c, col0:col0 + 128], rhs=xn.t[:, c, :], start=(c == 0), stop=(c == NCH - 1)),
                                reads=[wt.r] + xr, writes=[pmm.r])
                        dview = X[dst][s, row0:row0 + 128, T0:T0 + 512]
                        if not rope:
                            o_ = osb.next()
                            P.op('act', lambda e, o_=o_, pmm=pmm: e.activation(out=o_.t[:], in_=pmm.t[:], func=AF.Copy),
                                 reads=[pmm.r], writes=[o_.r])
                        else:
                            q_ = qsb.next()
                            prr = pr.next()
                            a1, a2, o_ = t1.next(), t2.next(), osb.next()
                            P.op('act', lambda e, q_=q_, pmm=pmm: e.activation(out=q_.t[:], in_=pmm.t[:], func=AF.Copy),
                                 reads=[pmm.r], writes=[q_.r])
                            P.op('pe', lambda e, q_=q_, prr=prr: e.matmul(prr.t[:], lhsT=self.rot.t[:], rhs=q_.t[:], start=True, stop=True),
                                 reads=[self.rot.r, q_.r], writes=[prr.r])
                            P.op('pool', lambda e, q_=q_, a1=a1, T0=T0: e.tensor_tensor(out=a1.t[:], in0=q_.t[:], in1=cosT.t[:, T0:T0 + 512], op=ALU.mult),
                                 reads=[q_.r, cosT.r], writes=[a1.r])
                            P.op('dve', lambda e, prr=prr, a2=a2, T0=T0: e.tensor_tensor(out=a2.t[:], in0=prr.t[:], in1=sinT.t[:, T0:T0 + 512], op=ALU.mult),
                                 reads=[prr.r, sinT.r], writes=[a2.r])
                            P.op('dve', lambda e, a1=a1, a2=a2, o_=o_: e.tensor_tensor(out=o_.t[:], in0=a1.t[:], in1=a2.t[:], op=ALU.add),
                                 reads=[a1.r, a2.r], writes=[o_.r])
                        P.dma('sp', lambda e, o_=o_, dview=dview: e.dma_start(out=dview, in_=o_.t[:]), reads=[o_.r])
                    for j in range(4):
                        tok0 = T0 + j * 128
                        for (dst, col0, nh, hd, vpool) in (('va1', 1024, 8, 64, v65), ('vb1', 2560, 4, 128, v129), ('vc1', 4096, 8, 64, v65)):
                            pmm = pm.next()
                            for c in range(NCH):
                                P.op('pe', lambda e, c=c, j=j, col0=col0, pmm=pmm, xn=xn: e.matmul(
                                    pmm.t[:], lhsT=xn.t[:, c, j * 128:(j + 1) * 128], rhs=w1.t[:, c, col0:col0 + 512],
                                    start=(c == 0), stop=(c == NCH - 1)), reads=[w1.r, xr[j]], writes=[pmm.r])
                            v_ = vpool.next()
                            P.op('act', lambda e, v_=v_, pmm=pmm, nh=nh, hd=hd: e.activation(
                                out=v_.t[:, :, 0:hd], in_=pmm.t[:].rearrange("p (h d) -> p h d", h=nh), func=AF.Copy),
                                reads=[pmm.r], writes=[v_.r])
                            dview = X[dst][s, tok0:tok0 + 128, :]
                            P.dma('sp', lambda e, v_=v_, dview=dview: e.dma_start(out=dview, in_=v_.t[:].rearrange("p h d -> p (h d)")), reads=[v_.r])
                        pmm = pm.next()
                        for c in range(NCH):
                            P.op('pe', lambda e, c=c, j=j, pmm=pmm, xn=xn: e.matmul(
                                pmm.t[:, 0:4], lhsT=xn.t[:, c, j * 128:(j + 1) * 128], rhs=w1.t[:, c, 4928:4932],
                                start=(c == 0), stop=(c == NCH - 1)), reads=[w1.r, xr[j]], writes=[pmm.r])
                        w_ = wsb.next()
                        P.op('dve', lambda e, w_=w_, pmm=pmm: e.tensor_scalar(out=w_.t[:], in0=pmm.t[:, 0:4], scalar1=1.0 / 16.0, scalar2=None, op0=ALU.mult),
                             reads=[pmm.r], writes=[w_.r])
                        dview = X['wi'][s, tok0:tok0 + 128, :]
                        P.dma('sp', lambda e, w_=w_, dview=dview: e.dma_start(out=dview, in_=w_.t[:]), reads=[w_.r])
            P.barrier()

    def phase2(self, l, s):
        if self.want('p2a'):
            self.mixerA(l, s)
        if self.want('p2b'):
            self.mixerB(l, s)
        if self.want('p2c'):
            self.mixerC(l, s)

    def _p2_common(self, st):
        P = self.P
        c = {}
        c['sc'] = self.pss(st, 'sc', [128, 512], F32, 3)
        c['acc'] = [self.ps(st, f'acc{i}', [128, 512], F32) for i in range(4)]
        c['tp'] = self.ps(st, 'tpo', [128, 1024], BF16)
        c['E'] = self.sbufs(st, 'E', [128, 512], BF16, 4)
        c['ob'] = self.sbufs(st, 'oblk', [128, 4, 512], BF16, 2)
        c['oT'] = self.sbufs(st, 'oTsb', [128, 4, 512], BF16, 1)
        c['negone'] = self.sbuf(st, 'negone', [128, 32], F32)
        P.op('pool', lambda e: e.memset(c['negone'].t[:], -1.0), writes=[c['negone'].r])
        c['zt'] = self.sbuf(st, 'zt', [1, 512], BF16)
        P.op('pool', lambda e: e.memset(c['zt'].t[:], 0.0), writes=[c['zt'].r])
        c['hc'] = 0
        return c

    def _open_acc(self, c, acc, W=512):
        zt = c['zt']
        self.P.op('pe', lambda e: e.matmul(acc.t[:, 0:W], lhsT=zt.t[0:1, 0:128], rhs=zt.t[0:1, 0:W], start=True, stop=False),
                  reads=[zt.r], writes=[acc.r])

    def run_units(self, c, units, LA=2, late_delay=5):
        P = self.P
        n = len(units)
        pend = []
        late = []
        for i in range(n + LA):
            if i < n:
                u = units[i]
                if u.get('pre'):
                    u['pre']()
                sc, E = c['sc'].next(), c['E'].next()
                u['s1'](sc)
                P.op('act', lambda e, sc=sc, E=E: e.activation(out=E.t[:], in_=sc.t[:], func=AF.Exp, scale=0.125),
                     reads=[sc.r], writes=[E.r])
                pend.append(E)
            if i >= LA:
                k = i - LA
                u = units[k]
                u['s3'](pend[k])
                if u.get('post'):
                    u['post']()
                if u.get('late'):
                    late.append((k + late_delay, u['late']))
                while late and late[0][0] <= k:
                    late.pop(0)[1]()
        for _, fn in late:
            fn()

    def _qz_bufs(self, st, n=2):
        P = self.P
        sets = []
        for i in range(n):
            pair = []
            for par in range(2):
                b_ = self.sbuf(st, f'qz{i}_{par}', [128, 4, 512], BF16)
                P.op('pool', lambda e, b_=b_: e.memset(b_.t[:], 0.0), writes=[b_.r])
                pair.append(b_)
            sets.append(pair)
        return sets

    def _qz_load(self, qz, src3):
        P = self.P
        for par in range(2):
            b_ = qz[par]
            P.dma('sp', lambda e, b_=b_, par=par: e.dma_start(out=b_.t[par * 64:(par + 1) * 64], in_=src3[par * 64:(par + 1) * 64]), writes=[b_.r])

    def _load_kv(self, st, s, kname, vname, vw):
        P, X, S, NT = self.P, self.X, self.S, self.NT
        kT = self.sbuf(st, 'kT', [128, 4, S], BF16)
        v1 = self.sbuf(st, 'v1', [128, NT, vw], BF16)
        P.dma('sp', lambda e: e.dma_start(out=kT.t[:], in_=X[kname][s].rearrange("(c p) t -> p c t", p=128)), writes=[kT.r])
        half = NT // 2
        for hh in range(2):
            P.dma('sp', lambda e, hh=hh: e.dma_start(
                out=v1.t[:, hh * half:(hh + 1) * half, :],
                in_=X[vname][s, hh * half * 128:(hh + 1) * half * 128, :].rearrange("(t p) f -> p t f", p=128)), writes=[v1.r])
        return kT, v1

    def _store_o(self, c, ob, s, row0, Q0):
        P, X = self.P, self.X
        if 'store' in KSKIP:
            return
        oT = c['oT'].next()
        tp = c['tp']
        for half in range(2):
            for cc in range(2):
                ch = half * 2 + cc
                for j in range(4):
                    P.op('pe', lambda e, ch=ch, j=j, cc=cc: e.transpose(
                        out=tp.t[:, cc * 512 + j * 128: cc * 512 + (j + 1) * 128],
                        in_=ob.t[:, j, ch * 128:(ch + 1) * 128], identity=self.ident.t[:]),
                        reads=[ob.r, self.ident.r], writes=[tp.r])
            P.op('act', lambda e, half=half: e.activation(
                out=oT.t[:, half * 2:half * 2 + 2, :], in_=tp.t[:].rearrange("p (c q) -> p c q", c=2), func=AF.Copy),
                reads=[tp.r], writes=[oT.r])
        dview = X['oT'][s, row0:row0 + 512, Q0:Q0 + 512].rearrange("(c p) q -> p c q", p=128)
        P.dma('pool', lambda e: e.dma_start(out=dview, in_=oT.t[:]), reads=[oT.r])

    def _evac65(self, c, acc, stg, h):
        self.P.op('act', lambda e: e.activation(out=stg.t[:, :, h, :], in_=acc.t[:, 0:260].rearrange("p (j d) -> p j d", j=4), func=AF.Copy),
                  reads=[acc.r], writes=[stg.r])

    def _norm65(self, c, stg, rec, ob):
        P = self.P
        if 'norm' in KSKIP:
            return
        P.op('pool', lambda e: e.tensor_tensor(out=rec.t[:], in0=stg.t[:, :, :, 64],
                                               in1=c['negone'].t[:, 0:32].rearrange("p (j h) -> p j h", j=4), op=ALU.pow),
             reads=[stg.r, c['negone'].r], writes=[rec.r])
        P.op('pool', lambda e: e.tensor_tensor(out=ob.t[:].rearrange("p j (h d) -> p j h d", h=8), in0=stg.t[:, :, :, 0:64],
                                               in1=rec.t[:].unsqueeze(3).to_broadcast([128, 4, 8, 64]), op=ALU.mult),
             reads=[stg.r, rec.r], writes=[ob.r])

    def mixerA(self, l, s):
        P, I, X, S, NB = self.P, self.I, self.X, self.S, self.NB
        with ExitStack() as st:
            c = self._p2_common(st)
            kT, v1 = self._load_kv(st, s, 'kaT', 'va1', 520)
            bm = self.sbuf(st, 'bm', [128, 8, 8, 512], BF16)
            qzs = self._qz_bufs(st)
            stgp = self.sbufs(st, 'stg', [128, 4, 8, 65], F32, 2)
            recp = self.sbufs(st, 'rec', [128, 4, 8], F32, 2)
            e_f = self.sbuf(st, 'e_f', [8, 1536], F32)
            e_b = self.sbuf(st, 'e_b', [8, 1536], BF16)
            r_eb = Res()
            P.dma('sp', lambda e: e.dma_start(out=e_f.t[:, 449:769], in_=I['rel_bias_a'][l]), writes=[e_f.r])
            P.op('dve', lambda e: e.tensor_copy(out=e_f.t[:, 0:449], in_=e_f.t[:, 449:450].to_broadcast([8, 449])),
                 reads=[e_f.r], writes=[e_f.r])
            P.op('dve', lambda e: e.tensor_copy(out=e_f.t[:, 769:1536], in_=e_f.t[:, 768:769].to_broadcast([8, 767])),
                 reads=[e_f.r], writes=[e_f.r])
            P.op('dve', lambda e: e.tensor_scalar(out=e_b.t[:], in0=e_f.t[:], scalar1=8.0, scalar2=None, op0=ALU.mult),
                 reads=[e_f.r], writes=[e_b.r])
            P.dma('sp', lambda e: e.dma_start(out=X['ebias'][:, :], in_=e_b.t[:]), reads=[e_b.r], writes=[r_eb])
            for h in range(8):
                src = bass.AP(tensor=X['ebias'].tensor, offset=h * 1536 + 1, ap=[[1, 128], [128, 8], [1, 512]])
                P.dma('sp', lambda e, h=h, src=src: e.dma_start(out=bm.t[:, h], in_=src), reads=[r_eb], writes=[bm.r])
            for t in range(8):
                for ph in range(2):
                    lo_fb = max(0, 2 * t + ph - 8)
                    hi_fb = min(7, 2 * t + ph)
                    if lo_fb > 0:
                        P.op('pool', lambda e, t=t, ph=ph, lo_fb=lo_fb: e.memset(bm.t[(1 - ph) * 64:(2 - ph) * 64, :, 7 - t, 0:64 * lo_fb], NEG),
                             writes=[bm.r])
                    if hi_fb < 7:
                        P.op('pool', lambda e, t=t, ph=ph, hi_fb=hi_fb: e.memset(bm.t[(1 - ph) * 64:(2 - ph) * 64, :, 7 - t, 64 * (hi_fb + 1):512], NEG),
                             writes=[bm.r])
            units = []
            for b in range(NB):
                Q0 = 512 * b
                q_ = qzs[b % 2]
                ob, stg, rec = c['ob'].next(), stgp.next(), recp.next()
                tmin = 4 if b == 0 else 0
                for h in range(8):
                    pr = slice((h % 2) * 64, (h % 2) * 64 + 64)
                    acc = c['acc'][c['hc'] % 4]
                    c['hc'] += 1
                    for t in range(tmin, 8):
                        K0 = Q0 - 512 + 128 * t
                        u = {}
                        if h == 0 and t == tmin:
                            u['pre'] = lambda q_=q_, Q0=Q0: self._qz_load(q_, X['qaT'][s, :, Q0:Q0 + 512].rearrange("(c p) t -> p c t", p=128))

                        def s1(sc, pr=pr, h=h, K0=K0, q_=q_, t=t):
                            qz = q_[h % 2]
                            P.op('pe', lambda e: e.matmul(sc.t[:], lhsT=kT.t[:, h // 2, K0:K0 + 128], rhs=qz.t[:, h // 2, :], start=True, stop=False),
                                 reads=[kT.r, qz.r], writes=[sc.r])
                            P.op('pe', lambda e: e.matmul(sc.t[:], lhsT=self.flip.t[:], rhs=bm.t[:, h, 7 - t, :], start=False, stop=True),
                                 reads=[bm.r, self.flip.r], writes=[sc.r])

                        def s3(E, h=h, K0=K0, t=t, tmin=tmin, acc=acc):
                            if t == tmin:
                                self._open_acc(c, acc, 260)
                            for j in range(4):
                                if j <= t <= j + 4:
                                    P.op('pe', lambda e, j=j: e.matmul(
                                        acc.t[:, j * 65:(j + 1) * 65], lhsT=E.t[:, j * 128:(j + 1) * 128], rhs=v1.t[:, K0 // 128, h * 65:(h + 1) * 65],
                                        start=False, stop=(t == j + 4)), reads=[E.r, v1.r], writes=[acc.r])
                        u['s1'], u['s3'] = s1, s3
                        if t == 7:
                            def post(h=h, acc=acc, stg=stg, rec=rec, ob=ob):
                                self._evac65(c, acc, stg, h)
                                if h == 7:
                                    self._norm65(c, stg, rec, ob)
                            u['post'] = post
                            if h == 7:
                                u['late'] = lambda ob=ob, Q0=Q0: self._store_o(c, ob, s, 0, Q0)
                        units.append(u)
            self.run_units(c, units)
            P.barrier()

    def mixerB(self, l, s):
        P, I, X, S, NB = self.P, self.I, self.X, self.S, self.NB
        with ExitStack() as st:
            c = self._p2_common(st)
            kT, v1 = self._load_kv(st, s, 'kbT', 'vb1', 516)
            cm = self.sbuf(st, 'cm', [128, 4, 512], BF16)
            P.dma('sp', lambda e: e.dma_start(out=cm.t[:], in_=I['c_cmask'][:, :, :]), writes=[cm.r])
            qzs = self._qz_bufs(st)
            stgp = self.sbufs(st, 'stgB', [128, 4, 2, 129], F32, 2)
            recp = self.sbufs(st, 'recB', [128, 4, 2], F32, 2)
            o2p = self.sbufs(st, 'o2', [128, 4, 2, 128], F32, 1)
            dfp = self.sbufs(st, 'df', [128, 4, 128], F32, 2)
            tnp = self.sbufs(st, 'tneg', [128, 4, 128], F32, 1)
            sqj = self.sbuf(st, 'sqj', [128, 128], BF16)
            st4 = self.sbufs(st, 'st4', [128, 3, 4], F32, 2)
            units = []
            for b in range(NB):
                Q0 = 512 * b
                q_ = qzs[b % 2]
                ob = c['ob'].next()
                nkt = 4 * b + 4
                for h in range(4):
                    stg, rec = stgp.next(), recp.next()
                    for mp in range(2):
                        pr = slice(mp * 64, mp * 64 + 64)
                        accs = (c['acc'][2 * (c['hc'] % 2)], c['acc'][2 * (c['hc'] % 2) + 1])
                        c['hc'] += 1
                        for t in range(nkt):
                            K0 = 128 * t
                            rel = t - 4 * b
                            u = {}
                            if h == 0 and mp == 0 and t == 0:
                                u['pre'] = lambda q_=q_, Q0=Q0: self._qz_load(q_, X['qbT'][s, :, Q0:Q0 + 512].rearrange("(c p) t -> p c t", p=128))

                            def s1(sc, pr=pr, h=h, K0=K0, q_=q_, rel=rel, mp=mp):
                                qz = q_[mp]
                                P.op('pe', lambda e: e.matmul(sc.t[:], lhsT=kT.t[:, h, K0:K0 + 128], rhs=qz.t[:, h, :], start=True, stop=(rel < 0)),
                                     reads=[kT.r, qz.r], writes=[sc.r])
                                if rel >= 0:
                                    P.op('pe', lambda e: e.matmul(sc.t[:], lhsT=self.ident.t[:], rhs=cm.t[:, rel, :], start=False, stop=True),
                                         reads=[cm.r, self.ident.r], writes=[sc.r])

                            def s3(E, h=h, t=t, b=b, rel=rel, accs=accs):
                                if t == 0:
                                    self._open_acc(c, accs[0], 258)
                                    self._open_acc(c, accs[1], 258)
                                for j in range(4):
                                    if rel <= j:
                                        acc = accs[j // 2]
                                        P.op('pe', lambda e, acc=acc, j=j: e.matmul(
                                            acc.t[:, (j % 2) * 129:(j % 2) * 129 + 129], lhsT=E.t[:, j * 128:(j + 1) * 128], rhs=v1.t[:, t, h * 129:(h + 1) * 129],
                                            start=False, stop=(t == 4 * b + j)), reads=[E.r, v1.r], writes=[acc.r])
                            u['s1'], u['s3'] = s1, s3
                            if t == nkt - 1:
                                def post(h=h, mp=mp, accs=accs, stg=stg, rec=rec, ob=ob):
                                    for jb in range(2):
                                        P.op('act', lambda e, jb=jb: e.activation(
                                            out=stg.t[:, 2 * jb:2 * jb + 2, mp, :], in_=accs[jb].t[:, 0:258].rearrange("p (j d) -> p j d", j=2), func=AF.Copy),
                                            reads=[accs[jb].r], writes=[stg.r])
                                    if mp == 1:
                                        o2, d_, tn, s4 = o2p.next(), dfp.next(), tnp.next(), st4.next()
                                        P.op('pool', lambda e: e.tensor_tensor(out=rec.t[:], in0=stg.t[:, :, :, 128],
                                                                               in1=c['negone'].t[:, 0:8].rearrange("p (j m) -> p j m", j=4), op=ALU.pow),
                                             reads=[stg.r, c['negone'].r], writes=[rec.r])
                                        P.op('pool', lambda e: e.tensor_tensor(out=o2.t[:], in0=stg.t[:, :, :, 0:128],
                                                                               in1=rec.t[:].unsqueeze(3).to_broadcast([128, 4, 2, 128]), op=ALU.mult),
                                             reads=[stg.r, rec.r], writes=[o2.r])
                                        P.op('pool', lambda e: e.tensor_scalar(out=tn.t[:], in0=o2.t[:, :, 1, :], scalar1=self.neglam.t[:, l:l + 1], scalar2=1.0,
                                                                               op0=ALU.mult, op1=ALU.mult), reads=[o2.r, self.neglam.r], writes=[tn.r])
                                        P.op('pool', lambda e: e.tensor_tensor(out=d_.t[:], in0=tn.t[:], in1=o2.t[:, :, 0, :], op=ALU.add),
                                             reads=[tn.r, o2.r], writes=[d_.r])
                                        for j in range(4):
                                            P.op('act', lambda e, j=j: e.activation(out=sqj.t[:], in_=d_.t[:, j, :], func=AF.Square, accum_out=s4.t[:, 0, j:j + 1]),
                                                 reads=[d_.r], writes=[sqj.r, s4.r])
                                        P.op('pool', lambda e: e.tensor_scalar(out=s4.t[:, 1, :], in0=s4.t[:, 0, :], scalar1=1.0 / 128.0, scalar2=EPS,
                                                                               op0=ALU.mult, op1=ALU.add), reads=[s4.r], writes=[s4.r])
                                        P.op('pool', lambda e: e.tensor_tensor(out=s4.t[:, 2, :], in0=s4.t[:, 1, :], in1=self.neghalf.t[:, 0:4], op=ALU.pow),
                                             reads=[s4.r, self.neghalf.r], writes=[s4.r])
                                        P.op('pool', lambda e: e.tensor_tensor(
                                            out=ob.t[:, :, h * 128:(h + 1) * 128], in0=d_.t[:], in1=s4.t[:, 2, :].unsqueeze(2).to_broadcast([128, 4, 128]), op=ALU.mult),
                                            reads=[d_.r, s4.r], writes=[ob.r])
                                u['post'] = post
                                if h == 3 and mp == 1:
                                    u['late'] = lambda ob=ob, Q0=Q0: self._store_o(c, ob, s, 512, Q0)
                            units.append(u)
            self.run_units(c, units)
            P.barrier()

    def mixerC(self, l, s):
        P, I, X, S, NB, NT = self.P, self.I, self.X, self.S, self.NB, self.NT
        FP8 = mybir.dt.float8e4
        with ExitStack() as st:
            c = self._p2_common(st)
            kT, v1 = self._load_kv(st, s, 'kcT', 'vc1', 520)
            kiT = self.sbuf(st, 'kiT', [128, S], BF16)
            P.dma('sp', lambda e: e.dma_start(out=kiT.t[:], in_=X['kiT'][s]), writes=[kiT.r])
            id128 = self.sbuf(st, 'id128', [128, 128], BF16)
            c240 = self.sbuf(st, 'c240', [128, 3, 128], BF16)
            P.op('pool', lambda e: e.memset(c240.t[:], -240.0), writes=[c240.r])
            P.op('pool', lambda e: e.tensor_scalar(out=id128.t[:], in0=self.ident.t[:], scalar1=128.0, scalar2=1.0, op0=ALU.mult, op1=ALU.mult),
                 reads=[self.ident.r], writes=[id128.r])
            qzs = self._qz_bufs(st, n=1)
            qib = self.sbufs(st, 'qiblk', [128, 2, 512], BF16, 2)
            wib = self.sbufs(st, 'wiblk', [128, 4, 4], F32, 2)
            I_sbs = [self.sbuf(st, f'I_sb{i}', [128, S], F32) for i in range(2)]
            Mqs = [self.sbuf(st, f'Mq{i}', [128, S], BF16) for i in range(2)]
            negms = [self.sbuf(st, f'negm{i}', [128, NT, 512], FP8) for i in range(2)]
            rl = self.sbufs(st, 'rl', [128, 512], BF16, 4)
            dg = self.sbufs(st, 'dg', [128, 128], BF16, 8)
            stgp = self.sbufs(st, 'stg', [128, 4, 8, 65], F32, 1)
            recp = self.sbufs(st, 'rec', [128, 4, 8], F32, 1)
            pw = self.sbuf(st, 'pw', [128, NIT + 1], F32)
            thr0 = self.sbuf(st, 'thr0', [128, 1], F32)
            for i in range(NIT + 1):
                P.op('pool', lambda e, i=i: e.memset(pw.t[:, i:i + 1], 2.0 ** -(i + 1)), writes=[pw.r])
            P.op('pool', lambda e: e.memset(thr0.t[:], -1e29), writes=[thr0.r])
            sm = self.sbufs(st, 'bsm', [128, 4], F32, 3)
            halfs = self.sbufs(st, 'halfs', [128, NIT + 1], F32, 2)
            mid = self.sbufs(st, 'mid', [128, 1], F32, 4)
            cnt = self.sbufs(st, 'cnt', [128, 1], F32, 4)
            tsel = self.sbufs(st, 'tsel', [128, 1], F32, 4)
            blkbuf = {}

            def blk_bufs(b):
                if b not in blkbuf:
                    q_, qi_, wi_ = qzs[0], qib.next(), wib.next()
                    Q0 = 512 * b
                    P.dma('sp', lambda e: e.dma_start(
                        out=qi_.t[:], in_=X['qiT'][s, :, Q0:Q0 + 512].rearrange("(c p) t -> p c t", p=128)), writes=[qi_.r])
                    P.dma('sp', lambda e: e.dma_start(
                        out=wi_.t[:], in_=X['wi'][s, Q0:Q0 + 512, :].rearrange("(j p) h -> p j h", p=128)), writes=[wi_.r])
                    blkbuf[b] = (q_, qi_, wi_)
                return blkbuf[b]

            def IDX(m):
                b, jj = m // 4, m % 4
                q_, qi_, wi_ = blk_bufs(b)
                n_k = 128 * (m + 1)
                I_sb = I_sbs[m % 2]
                dgs = []
                for h in range(4):
                    d_ = dg.next()
                    P.op('pool', lambda e, d_=d_, h=h: e.tensor_scalar(
                        out=d_.t[:], in0=self.ident.t[:], scalar1=wi_.t[:, jj, h:h + 1], scalar2=1.0, op0=ALU.mult, op1=ALU.mult),
                        reads=[self.ident.r, wi_.r], writes=[d_.r])
                    dgs.append(d_)
                for k0 in range(0, n_k, 512):
                    w = min(512, n_k - k0)
                    rls = []
                    for h in range(4):
                        pr = slice((h % 2) * 64, (h % 2) * 64 + 64)
                        sc = c['sc'].next()
                        P.op('pe', lambda e, sc=sc, pr=pr, h=h, k0=k0, w=w: e.matmul(
                            sc.t[:, 0:w], lhsT=qi_.t[pr, h // 2, jj * 128:(jj + 1) * 128], rhs=kiT.t[pr, k0:k0 + w], start=True, stop=True),
                            reads=[qi_.r, kiT.r], writes=[sc.r])
                        r_ = rl.next()
                        P.op('act', lambda e, sc=sc, r_=r_, w=w: e.activation(out=r_.t[:, 0:w], in_=sc.t[:, 0:w], func=AF.Relu),
                             reads=[sc.r], writes=[r_.r])
                        rls.append(r_)
                    accI = c['sc'].next()
                    for h in range(4):
                        P.op('pe', lambda e, accI=accI, h=h, w=w, rls=rls: e.matmul(
                            accI.t[:, 0:w], lhsT=dgs[h].t[:], rhs=rls[h].t[:, 0:w], start=(h == 0), stop=(h == 3)),
                            reads=[dgs[h].r, rls[h].r], writes=[accI.r])
                    P.op('act', lambda e, accI=accI, k0=k0, w=w: e.activation(out=I_sb.t[:, k0:k0 + w], in_=accI.t[:, 0:w], func=AF.Copy),
                         reads=[accI.r], writes=[I_sb.r])

            def BIS(m):
                n_k = 128 * (m + 1)
                I_sb, Mq = I_sbs[m % 2], Mqs[m % 2]
                if m >= 2:
                    sm_ = sm.next()
                    hf = halfs.next()
                    P.op('dve', lambda e: e.tensor_reduce(out=sm_.t[:, 0:1], in_=I_sb.t[:, 0:n_k], axis=AX.X, op=ALU.max),
                         reads=[I_sb.r], writes=[sm_.r])
                    P.op('dve', lambda e: e.tensor_reduce(out=sm_.t[:, 1:2], in_=I_sb.t[:, 0:n_k], axis=AX.X, op=ALU.min),
                         reads=[I_sb.r], writes=[sm_.r])
                P.op('dve', lambda e: e.memset(I_sb.t[0:64, n_k - 64:n_k], -1e30), writes=[I_sb.r])
                if m >= 2:
                    P.op('dve', lambda e: e.tensor_tensor(out=sm_.t[:, 2:3], in0=sm_.t[:, 0:1], in1=sm_.t[:, 1:2], op=ALU.subtract),
                         reads=[sm_.r], writes=[sm_.r])
                    P.op('dve', lambda e: e.tensor_scalar(out=hf.t[:], in0=pw.t[:], scalar1=sm_.t[:, 2:3], scalar2=None, op0=ALU.mult),
                         reads=[sm_.r, pw.r], writes=[hf.r])
                    md = mid.next()
                    P.op('dve', lambda e, md=md: e.tensor_tensor(out=md.t[:], in0=sm_.t[:, 1:2], in1=hf.t[:, 0:1], op=ALU.add),
                         reads=[sm_.r, hf.r], writes=[md.r])
                    for it in range(NIT):
                        cn, ts, md2 = cnt.next(), tsel.next(), mid.next()
                        P.op('dve', lambda e, cn=cn, md=md: e.tensor_scalar(
                            out=Mq.t[:, 0:n_k], in0=I_sb.t[:, 0:n_k], scalar1=md.t[:, 0:1], scalar2=None,
                            op0=ALU.is_ge, op1=ALU.add, accum_out=cn.t[:, 0:1]),
                            reads=[I_sb.r, md.r], writes=[Mq.r, cn.r])
                        P.op('dve', lambda e, cn=cn, ts=ts, it=it: e.tensor_scalar(
                            out=ts.t[:], in0=cn.t[:], scalar1=float(TOPK), scalar2=hf.t[:, it:it + 1], op0=ALU.is_ge, op1=ALU.mult),
                            reads=[cn.r, hf.r], writes=[ts.r])
                        P.op('dve', lambda e, md=md, md2=md2, ts=ts, it=it: e.scalar_tensor_tensor(
                            out=md2.t[:], in0=md.t[:], scalar=hf.t[:, it + 1:it + 2], in1=ts.t[:], op0=ALU.subtract, op1=ALU.add),
                            reads=[md.r, ts.r, hf.r], writes=[md2.r])
                        md = md2
                    P.op('dve', lambda e, md=md: e.tensor_tensor(out=sm_.t[:, 3:4], in0=md.t[:], in1=hf.t[:, NIT:NIT + 1], op=ALU.subtract),
                         reads=[md.r, hf.r], writes=[sm_.r])
                    thr_ap, thr_res = sm_.t[:, 3:4], sm_.r
                else:
                    thr_ap, thr_res = thr0.t[:, 0:1], thr0.r
                P.op('dve', lambda e: e.tensor_scalar(
                    out=Mq.t[:, 0:n_k], in0=I_sb.t[:, 0:n_k], scalar1=thr_ap, scalar2=None, op0=ALU.is_ge),
                    reads=[I_sb.r, thr_res], writes=[Mq.r])

            def TR(m):
                b, jj = m // 4, m % 4
                nkt = 4 * b + 4
                Mq, negm = Mqs[m % 2], negms[b % 2]
                tp = c['tp']
                for t0 in range(0, m + 1, 8):
                    nt_ = min(8, m + 1 - t0)
                    for tt in range(nt_):
                        P.op('pe', lambda e, tt=tt, t0=t0: e.transpose(
                            out=tp.t[:, tt * 128:(tt + 1) * 128], in_=Mq.t[:, (t0 + tt) * 128:(t0 + tt + 1) * 128], identity=self.ident.t[:]),
                            reads=[Mq.r, self.ident.r], writes=[tp.r])
                    P.op('act', lambda e, t0=t0, nt_=nt_: e.activation(
                        out=negm.t[:, t0:t0 + nt_, jj * 128:(jj + 1) * 128],
                        in_=tp.t[:, 0:nt_ * 128].rearrange("p (t q) -> p t q", t=nt_),
                        func=AF.Identity, scale=240.0, bias=-240.0, saturate=False), reads=[tp.r], writes=[negm.r])
                if m + 1 < nkt:
                    nf = nkt - m - 1
                    P.op('pool', lambda e: e.tensor_copy(out=negm.t[:, m + 1:nkt, jj * 128:(jj + 1) * 128], in_=c240.t[:, 0:nf, :], saturate=False),
                         reads=[c240.r], writes=[negm.r])

            def MAIN(b):
                Q0 = 512 * b
                nkt = 4 * b + 4
                q_, qi_, wi_ = blk_bufs(b)
                self._qz_load(q_, X['qcT'][s, :, Q0:Q0 + 512].rearrange("(c p) t -> p c t", p=128))
                negm = negms[b % 2]
                ob, stg, rec = c['ob'].next(), stgp.next(), recp.next()
                units = []
                for h in range(8):
                    pr = slice((h % 2) * 64, (h % 2) * 64 + 64)
                    acc = c['acc'][c['hc'] % 4]
                    c['hc'] += 1
                    for t in range(nkt):
                        K0 = 128 * t
                        u = {}

                        def s1(sc, pr=pr, h=h, K0=K0, t=t):
                            qz = q_[h % 2]
                            P.op('pe', lambda e: e.matmul(sc.t[:], lhsT=kT.t[:, h // 2, K0:K0 + 128], rhs=qz.t[:, h // 2, :], start=True, stop=False),
                                 reads=[kT.r, qz.r], writes=[sc.r])
                            P.op('pe', lambda e: e.matmul(sc.t[:], lhsT=id128.t[:], rhs=negm.t[:, t, :], start=False, stop=True),
                                 reads=[negm.r, id128.r], writes=[sc.r])

                        def s3(E, h=h, t=t, acc=acc):
                            if t == 0:
                                self._open_acc(c, acc, 260)
                            for j in range(4):
                                if t <= 4 * b + j:
                                    P.op('pe', lambda e, j=j: e.matmul(
                                        acc.t[:, j * 65:(j + 1) * 65], lhsT=E.t[:, j * 128:(j + 1) * 128], rhs=v1.t[:, t, h * 65:(h + 1) * 65],
                                        start=False, stop=(t == 4 * b + j)), reads=[E.r, v1.r], writes=[acc.r])
                        u['s1'], u['s3'] = s1, s3
                        if t == nkt - 1:
                            def post(h=h, acc=acc):
                                self._evac65(c, acc, stg, h)
                                if h == 7:
                                    self._norm65(c, stg, rec, ob)
                            u['post'] = post
                        units.append(u)
                self.run_units(c, units)
                return lambda: self._store_o(c, ob, s, 1024, Q0)

            IDX(0)
            if NT > 1:
                IDX(1)
            pending_store = None
            for m in range(NT):
                BIS(m)
                TR(m)
                if m + 2 < NT:
                    IDX(m + 2)
                if m % 4 == 3:
                    st_fn = MAIN(m // 4)
                    if pending_store is not None:
                        pending_store()
                    pending_store = st_fn
            if pending_store is not None:
                pending_store()
            P.barrier()

    def load_x_block(self, l, s, T0, ntile, xts, xbp, ssp, junk, tpp, dstT, dst_res, load=True):
        P = self.P
        for j in range(ntile):
            x_t = xts[j]
            if load:
                src = self.x_src(l)[s, T0 + j * 128:T0 + (j + 1) * 128, :]
                P.dma('sp', lambda e, x_t=x_t, src=src: e.dma_start(out=x_t.t[:], in_=src), writes=[x_t.r])
            x_b, ss, pt = xbp.next(), ssp.next(), tpp.next()
            self.rms_to_bf16(x_t, junk, ss, x_b)
            self.transpose_to(x_b, pt, dstT, j * 128, 128, 'dve', dst_res=dst_res[j])

    def phase3a(self, l):
        P, I, X, S = self.P, self.I, self.X, self.S
        with ExitStack() as st:
            wg = self.sbuf(st, 'wg', [128, NCH, 3072], BF16)
            wu = self.sbuf(st, 'wu', [128, 12, D], BF16)
            wo = self.sbuf(st, 'wo', [128, NCH, D], BF16)
            wq = self.sbuf(st, 'wq', [128, NCH, 256], BF16)
            wox = self.sbuf(st, 'wox', [128, 2, D], BF16)
            wkv = self.sbuf(st, 'wkv', [128, NCH, 512], BF16)
            gn = self.gains
            with ExitStack() as st2:
                stage = self.sbufs(st2, 'stage', [128, 1024], F32, 2)
                self.load_weight(stage, wg, lambda c, c0, c1: wg.t[:, c, c0:c1], I['w_in'][l][:, 4932:8004], NCH, 3072,
                                 lambda c: (gn.t[:, 0, l, c, :], gn.r))
                self.load_weight(stage, wu, lambda c, c0, c1: wu.t[:, c, c0:c1], I['w_up_a'][l], 4, D, lambda c: None)
                self.load_weight(stage, wu, lambda c, c0, c1: wu.t[:, 4 + c, c0:c1], I['w_up_b'][l], 4, D,
                                 lambda c: (self.subln.t[:, l, :], self.subln.r), const=1.0 - self.lam_init(l))
                self.load_weight(stage, wu, lambda c, c0, c1: wu.t[:, 8 + c, c0:c1], I['w_up_c'][l], 4, D, lambda c: None)
                self.load_weight(stage, wo, lambda c, c0, c1: wo.t[:, c, c0:c1], I['w_out'][l], NCH, D, lambda c: None, const=0.5)
                self.load_weight(stage, wq, lambda c, c0, c1: wq.t[:, c, c0:c1], I['w_q_x'][l], NCH, 256,
                                 lambda c: (gn.t[:, 1, l, c, :], gn.r))
                self.load_weight(stage, wox, lambda c, c0, c1: wox.t[:, c, c0:c1], I['w_o_x'][l], 2, D, lambda c: None)
                self.load_weight(stage, wkv, lambda c, c0, c1: wkv.t[:, c, c0:c1], I['w_kv_x'][l], NCH, 512,
                                 lambda c: (self.gmem.t[:, c, :], self.gmem.r))
                P.barrier()
            tp = self.pss(st, 'tp', [128, D], BF16, 1)
            pm = self.pss(st, 'pm', [128, 512], F32, 3)
            acc = [self.ps(st, f'acc{i}', [128, 512], F32) for i in range(4)]
            kmT = [self.sbuf(st, f'kmT{s}', [128, 2, MEM], BF16) for s in range(self.NSEQ)]
            vm1 = [self.sbuf(st, f'vm1{s}', [128, 2, 4, 65], BF16) for s in range(self.NSEQ)]
            for s in range(self.NSEQ):
                P.op('dve', lambda e, s=s: e.memset(vm1[s].t[:], 1.0), writes=[vm1[s].r])
                for c2 in range(2):
                    pk = pm.next()
                    for c in range(NCH):
                        P.op('pe', lambda e, c=c, c2=c2, pk=pk, s=s: e.matmul(
                            pk.t[:, 0:MEM], lhsT=wkv.t[:, c, c2 * 128:(c2 + 1) * 128], rhs=self.memT[s].t[:, c, :],
                            start=(c == 0), stop=(c == NCH - 1)), reads=[wkv.r, self.memT[s].r], writes=[pk.r])
                    P.op('act', lambda e, c2=c2, pk=pk, s=s: e.activation(out=kmT[s].t[:, c2, :], in_=pk.t[:, 0:MEM], func=AF.Copy),
                         reads=[pk.r], writes=[kmT[s].r])
                for t in range(2):
                    pv = pm.next()
                    for c in range(NCH):
                        P.op('pe', lambda e, c=c, t=t, pv=pv, s=s: e.matmul(
                            pv.t[:, 0:256], lhsT=self.memT[s].t[:, c, t * 128:(t + 1) * 128], rhs=wkv.t[:, c, 256:512],
                            start=(c == 0), stop=(c == NCH - 1)), reads=[wkv.r, self.memT[s].r], writes=[pv.r])
                    P.op('act', lambda e, t=t, pv=pv, s=s: e.activation(
                        out=vm1[s].t[:, t, :, 0:64], in_=pv.t[:, 0:256].rearrange("p (h d) -> p h d", h=4), func=AF.Copy),
                        reads=[pv.r], writes=[vm1[s].r])
            xts = [self.sbuf(st, f'xt{j}', [128, D], F32) for j in range(4)]
            junk = self.sbuf(st, 'junk', [128, D], BF16)
            xbp = self.sbufs(st, 'xb', [128, D], BF16, 2)
            ssp = self.sbufs(st, 'ss', [128, 2], F32, 4)
            xnT = self.sbuf(st, 'xnT', [128, NCH, 512], BF16)
            xnT_res = [Res() for _ in range(4)]
            oTb = self.sbuf(st, 'oTb', [128, 12, 512], BF16)
            sgp = self.sbufs(st, 'sg', [128, 512], F32, 3)
            ttp = self.sbufs(st, 'tt', [128, 512], F32, 3)
            ysum = self.sbufs(st, 'ysum', [128, 512], F32, 2)
            yT = self.sbuf(st, 'yT', [128, NCH, 512], BF16)
            yT_res = [Res() for _ in range(NCH)]
            qxT = self.sbuf(st, 'qxT', [128, 2, 512], BF16)
            Ep = self.sbufs(st, 'E', [128, 512], BF16, 3)
            recp = self.sbufs(st, 'rec', [128, 1], F32, 8)
            ox = self.sbuf(st, 'ox', [128, 4, 256], BF16)
            oxT = self.sbuf(st, 'oxT', [128, 2, 512], BF16)
            for s in range(self.NSEQ):
                for tb in range(self.NB):
                    T0 = tb * 512
                    self.load_x_block(l, s, T0, 4, xts, xbp, ssp, junk, tp, xnT, xnT_res)
                    P.dma('sp', lambda e, s=s, T0=T0: e.dma_start(
                        out=oTb.t[:], in_=X['oT'][s, :, T0:T0 + 512].rearrange("(c p) q -> p c q", p=128)), writes=[oTb.r])
                    for f in range(NCH):
                        tts = []
                        for i in range(3):
                            pg = pm.next()
                            for c in range(NCH):
                                P.op('pe', lambda e, c=c, i=i, f=f, pg=pg: e.matmul(
                                    pg.t[:], lhsT=wg.t[:, c, i * 1024 + f * 128: i * 1024 + (f + 1) * 128], rhs=xnT.t[:, c, :],
                                    start=(c == 0), stop=(c == NCH - 1)), reads=[wg.r] + xnT_res, writes=[pg.r])
                            sg = sgp.next()
                            P.op('act', lambda e, sg=sg, pg=pg: e.activation(out=sg.t[:], in_=pg.t[:], func=AF.Tanh, scale=0.5),
                                 reads=[pg.r], writes=[sg.r])
                            pu = pm.next()
                            for c in range(4):
                                P.op('pe', lambda e, c=c, i=i, f=f, pu=pu: e.matmul(
                                    pu.t[:], lhsT=wu.t[:, 4 * i + c, f * 128:(f + 1) * 128], rhs=oTb.t[:, 4 * i + c, :],
                                    start=(c == 0), stop=(c == 3)), reads=[wu.r, oTb.r], writes=[pu.r])
                            tt = ttp.next()
                            P.op('dve', lambda e, sg=sg, pu=pu, tt=tt: e.scalar_tensor_tensor(
                                out=tt.t[:], in0=sg.t[:], scalar=1.0, in1=pu.t[:], op0=ALU.add, op1=ALU.mult),
                                reads=[sg.r, pu.r], writes=[tt.r])
                            tts.append(tt)
                        ys = ysum.next()
                        P.op('pool', lambda e, ys=ys, tts=tts: e.tensor_tensor(out=ys.t[:], in0=tts[0].t[:], in1=tts[1].t[:], op=ALU.add),
                             reads=[tts[0].r, tts[1].r], writes=[ys.r])
                        P.op('pool', lambda e, ys=ys, tts=tts, f=f: e.tensor_tensor(out=yT.t[:, f, :], in0=ys.t[:], in1=tts[2].t[:], op=ALU.add),
                             reads=[ys.r, tts[2].r], writes=[yT_res[f]])
                    for j in range(4):
                        for hf in range(2):
                            po = pm.next()
                            for f in range(NCH):
                                P.op('pe', lambda e, f=f, j=j, hf=hf, po=po: e.matmul(
                                    po.t[:], lhsT=yT.t[:, f, j * 128:(j + 1) * 128], rhs=wo.t[:, f, hf * 512:(hf + 1) * 512],
                                    start=(f == 0), stop=(f == NCH - 1)), reads=[wo.r] + yT_res, writes=[po.r])
                            P.op('dve', lambda e, j=j, hf=hf, po=po: e.tensor_tensor(
                                out=xts[j].t[:, hf * 512:(hf + 1) * 512], in0=po.t[:], in1=xts[j].t[:, hf * 512:(hf + 1) * 512], op=ALU.add),
                                reads=[po.r, xts[j].r], writes=[xts[j].r])
                    self.load_x_block(l, s, T0, 4, xts, xbp, ssp, junk, tp, xnT, xnT_res, load=False)
                    for c2 in range(2):
                        pq = pm.next()
                        for c in range(NCH):
                            P.op('pe', lambda e, c=c, c2=c2, pq=pq: e.matmul(
                                pq.t[:], lhsT=wq.t[:, c, c2 * 128:(c2 + 1) * 128], rhs=xnT.t[:, c, :],
                                start=(c == 0), stop=(c == NCH - 1)), reads=[wq.r] + xnT_res, writes=[pq.r])
                        P.op('act', lambda e, c2=c2, pq=pq: e.activation(out=qxT.t[:, c2, :], in_=pq.t[:], func=AF.Copy),
                             reads=[pq.r], writes=[qxT.r])
                    for h in range(4):
                        pr = slice((h % 2) * 64, (h % 2) * 64 + 64)
                        for t in range(2):
                            sc = pm.next()
                            P.op('pe', lambda e, sc=sc, pr=pr, h=h, t=t, s=s: e.matmul(
                                sc.t[:], lhsT=kmT[s].t[pr, h // 2, t * 128:(t + 1) * 128], rhs=qxT.t[pr, h // 2, :], start=True, stop=True),
                                reads=[kmT[s].r, qxT.r], writes=[sc.r])
                            E = Ep.next()
                            P.op('act', lambda e, sc=sc, E=E: e.activation(out=E.t[:], in_=sc.t[:], func=AF.Exp, scale=0.125),
                                 reads=[sc.r], writes=[E.r])
                            for j in range(4):
                                P.op('pe', lambda e, E=E, j=j, t=t, h=h, s=s: e.matmul(
                                    acc[j].t[:, 0:65], lhsT=E.t[:, j * 128:(j + 1) * 128], rhs=vm1[s].t[:, t, h, :],
                                    start=(t == 0), stop=(t == 1)), reads=[E.r, vm1[s].r], writes=[acc[j].r])
                        for j in range(4):
                            rec = recp.next()
                            P.op('dve', lambda e, rec=rec, j=j: e.reciprocal(out=rec.t[:], in_=acc[j].t[:, 64:65]), reads=[acc[j].r], writes=[rec.r])
                            P.op('dve', lambda e, rec=rec, j=j, h=h: e.tensor_scalar(
                                out=ox.t[:, j, h * 64:(h + 1) * 64], in0=acc[j].t[:, 0:64], scalar1=rec.t[:, 0:1], scalar2=None, op0=ALU.mult),
                                reads=[acc[j].r, rec.r], writes=[ox.r])
                    pt = tp.next()
                    for c2 in range(2):
                        for j in range(4):
                            P.op('pe', lambda e, c2=c2, j=j, pt=pt: e.transpose(
                                out=pt.t[:, (c2 * 4 + j) * 128:(c2 * 4 + j + 1) * 128], in_=ox.t[:, j, c2 * 128:(c2 + 1) * 128], identity=self.ident.t[:]),
                                reads=[ox.r, self.ident.r], writes=[pt.r])
                    P.op('dve', lambda e, pt=pt: e.tensor_copy(out=oxT.t[:], in_=pt.t[:].rearrange("p (c q) -> p c q", c=2)),
                         reads=[pt.r], writes=[oxT.r])
                    for j in range(4):
                        for hf in range(2):
                            po = pm.next()
                            for c2 in range(2):
                                P.op('pe', lambda e, c2=c2, j=j, hf=hf, po=po: e.matmul(
                                    po.t[:], lhsT=oxT.t[:, c2, j * 128:(j + 1) * 128], rhs=wox.t[:, c2, hf * 512:(hf + 1) * 512],
                                    start=(c2 == 0), stop=(c2 == 1)), reads=[wox.r, oxT.r], writes=[po.r])
                            P.op('dve', lambda e, j=j, hf=hf, po=po: e.tensor_tensor(
                                out=xts[j].t[:, hf * 512:(hf + 1) * 512], in0=po.t[:], in1=xts[j].t[:, hf * 512:(hf + 1) * 512], op=ALU.add),
                                reads=[po.r, xts[j].r], writes=[xts[j].r])
                        dview = X['xres'][s, T0 + j * 128:T0 + (j + 1) * 128, :]
                        P.dma('pool', lambda e, j=j, dview=dview: e.dma_start(out=dview, in_=xts[j].t[:]), reads=[xts[j].r])
            P.barrier()

    def phase3b(self, l):
        P, I, X, S = self.P, self.I, self.X, self.S
        last = (l == self.DEPTH - 1)
        TB = 256
        with ExitStack() as st:
            wgu = self.sbuf(st, 'wgu', [128, NCH, 2 * DFF], BF16)
            wd = self.sbuf(st, 'wd', [128, NFF, D], BF16)
            gn = self.gains
            with ExitStack() as st2:
                stage = self.sbufs(st2, 'stage', [128, 1408], F32, 2)
                self.load_weight(stage, wgu, lambda c, c0, c1: wgu.t[:, c, c0:c1], I['w_gu'][l], NCH, 2 * DFF,
                                 lambda c: (gn.t[:, 2, l, c, :], gn.r))
                self.load_weight(stage, wd, lambda c, c0, c1: wd.t[:, c, c0:c1], I['w_down'][l], NFF, D, lambda c: None)
                P.barrier()
            tp = self.pss(st, 'tp', [128, D], BF16, 2)
            pm = self.pss(st, 'pm', [128, 512], F32, 6)
            xts = [self.sbuf(st, f'xt{j}', [128, D], F32) for j in range(4)]
            junk = self.sbuf(st, 'junk', [128, D], BF16)
            xbp = self.sbufs(st, 'xb', [128, D], BF16, 2)
            ssp = self.sbufs(st, 'ss', [128, 2], F32, 4)
            hnT = [self.sbuf(st, f'hnT{i}', [128, NCH, TB], BF16) for i in range(2)]
            hnT_res = [[Res() for _ in range(2)] for _ in range(2)]
            hT = self.sbuf(st, 'hT', [128, NFF, TB], BF16)
            hT_res = [Res() for _ in range(NFF)]
            slp = self.sbufs(st, 'sl', [128, TB], F32, 3)
            if last:
                fin_g = self.sbuf(st, 'fin_g', [128, D], F32)
                P.dma('sp', lambda e: e.dma_start(out=fin_g.t[:], in_=I['final_norm'].partition_broadcast(128)), writes=[fin_g.r])
                outp = self.sbufs(st, 'outp', [128, D], F32, 2)
            blk = 0
            for s in range(self.NSEQ):
                for tb in range(S // TB):
                    T0 = tb * TB
                    xs = xts[(blk % 2) * 2:(blk % 2) * 2 + 2]
                    hn, hr = hnT[blk % 2], hnT_res[blk % 2]
                    blk += 1
                    self.load_x_block(l + 1, s, T0, 2, xs, xbp, ssp, junk, tp, hn, hr)
                    for f in range(NFF):
                        pg, pu = pm.next(), pm.next()
                        for (pp, off) in ((pg, 0), (pu, DFF)):
                            for c in range(NCH):
                                P.op('pe', lambda e, c=c, f=f, pp=pp, off=off, hn=hn: e.matmul(
                                    pp.t[:, 0:TB], lhsT=wgu.t[:, c, off + f * 128: off + (f + 1) * 128], rhs=hn.t[:, c, :],
                                    start=(c == 0), stop=(c == NCH - 1)), reads=[wgu.r] + hr, writes=[pp.r])
                        sl = slp.next()
                        P.op('act', lambda e, sl=sl, pg=pg: e.activation(out=sl.t[:], in_=pg.t[:, 0:TB], func=AF.Silu),
                             reads=[pg.r], writes=[sl.r])
                        P.op('dve', lambda e, sl=sl, pu=pu, f=f: e.tensor_tensor(out=hT.t[:, f, :], in0=pu.t[:, 0:TB], in1=sl.t[:], op=ALU.mult),
                             reads=[pu.r, sl.r], writes=[hT_res[f]])
                    for j in range(2):
                        for hf in range(2):
                            po = pm.next()
                            for f in range(NFF):
                                P.op('pe', lambda e, f=f, j=j, hf=hf, po=po: e.matmul(
                                    po.t[:], lhsT=hT.t[:, f, j * 128:(j + 1) * 128], rhs=wd.t[:, f, hf * 512:(hf + 1) * 512],
                                    start=(f == 0), stop=(f == NFF - 1)), reads=[wd.r] + hT_res, writes=[po.r])
                            P.op('dve', lambda e, j=j, hf=hf, po=po, xs=xs: e.tensor_tensor(
                                out=xs[j].t[:, hf * 512:(hf + 1) * 512], in0=po.t[:], in1=xs[j].t[:, hf * 512:(hf + 1) * 512], op=ALU.add),
                                reads=[po.r, xs[j].r], writes=[xs[j].r])
                        row0 = T0 + j * 128
                        if not last:
                            dview = X['xres'][s, row0:row0 + 128, :]
                            P.dma('pool', lambda e, j=j, dview=dview, xs=xs: e.dma_start(out=dview, in_=xs[j].t[:]), reads=[xs[j].r])
                        else:
                            ss = ssp.next()
                            o_ = outp.next()
                            x_t = xs[j]
                            P.op('act', lambda e, x_t=x_t, ss=ss: e.activation(out=junk.t[:], in_=x_t.t[:], func=AF.Square, accum_out=ss.t[:, 0:1]),
                                 reads=[x_t.r], writes=[junk.r, ss.r])
                            P.op('dve', lambda e, ss=ss: e.tensor_scalar(out=ss.t[:, 1:2], in0=ss.t[:, 0:1], scalar1=1.0 / D, scalar2=EPS,
                                                                        op0=ALU.mult, op1=ALU.add), reads=[ss.r], writes=[ss.r])
                            P.op('pool', lambda e, ss=ss: e.tensor_tensor(out=ss.t[:, 0:1], in0=ss.t[:, 1:2], in1=self.neghalf.t[:, 0:1], op=ALU.pow),
                                 reads=[ss.r, self.neghalf.r], writes=[ss.r])
                            P.op('dve', lambda e, x_t=x_t, ss=ss, o_=o_: e.scalar_tensor_tensor(
                                out=o_.t[:], in0=x_t.t[:], scalar=ss.t[:, 0:1], in1=fin_g.t[:], op0=ALU.mult, op1=ALU.mult),
                                reads=[x_t.r, ss.r, fin_g.r], writes=[o_.r])
                            dview = self.out[s, row0:row0 + 128, :]
                            P.dma('pool', lambda e, dview=dview, o_=o_: e.dma_start(out=dview, in_=o_.t[:]), reads=[o_.r])
            P.barrier()


def make_consts(S):
    bf = ml_dtypes.bfloat16
    ident = np.eye(128, dtype=np.float32).astype(bf)
    rot = np.zeros((128, 128), np.float32)
    for blk in range(2):
        for j in range(32):
            rot[blk * 64 + j + 32, blk * 64 + j] = -1.0
            rot[blk * 64 + j, blk * 64 + j + 32] = 1.0
    pos = np.arange(S, dtype=np.float32)
    inv = (10000.0 ** (-np.arange(0, 64, 2, dtype=np.float32) / 64)).astype(np.float32)
    ang = pos[None, :] * inv[:, None]
    cos = np.tile(np.cos(ang), (4, 1)).astype(bf)
    sin = np.tile(np.sin(ang), (4, 1)).astype(bf)
    cm = np.zeros((128, 4, 512), np.float32)
    for t in range(4):
        for p in range(128):
            kc = 2 * t + p // 64
            for fb in range(8):
                if kc > fb:
                    cm[p, t, fb * 64:(fb + 1) * 64] = NEG
    flip = np.eye(128, dtype=np.float32)[::-1].copy().astype(bf)
    return {'c_ident': ident, 'c_flip': flip, 'c_rot': rot.astype(bf), 'c_cos': cos, 'c_sin': sin, 'c_cmask': cm.astype(bf)}


_CACHE = {}


def kernel(**inputs):
    x = np.asarray(inputs['x'], np.float32)
    B, S, _ = x.shape
    NCORES = 8
    NSEQ = B // NCORES
    DEPTH = inputs['w_in'].shape[0]
    key = (S, NSEQ, DEPTH)
    if key not in _CACHE:
        _CACHE[key] = Builder(S, NSEQ, DEPTH).build()
    nc = _CACHE[key]
    consts = make_consts(S)
    shared = {k: np.ascontiguousarray(np.asarray(v, np.float32)) for k, v in inputs.items() if k not in ('x', 'mem')}
    shared['lambda_vecs'] = shared['lambda_vecs'].reshape(DEPTH, 256)
    shared.update(consts)
    mem = np.asarray(inputs['mem'], np.float32)
    in_maps = []
    for c in range(NCORES):
        m = dict(shared)
        m['x'] = np.ascontiguousarray(x[c * NSEQ:(c + 1) * NSEQ])
        m['mem'] = np.ascontiguousarray(mem[c * NSEQ:(c + 1) * NSEQ])
        in_maps.append(m)
    res = run_bass_kernel_spmd(nc, in_maps, core_ids=list(range(NCORES)))
    return np.concatenate([r['out'] for r in res.results], axis=0).astype(np.float32)
```
